# Optimizing a Trainium2 kernel written in Bass

```python
import math
import jax, jax.numpy as jnp
from jax import lax
import numpy as np

D_MODEL = 1024
BATCH = 8
SEQ = 4096
DEPTH = 1

H_A = 8
DK_A = 64
DV_A = 64
W_A = H_A * DV_A
CONV_K = 4
CHUNK = 64
H_B = 8
DH_B = 64
W_B = H_B * DH_B
HI = 8
DI = 64
TOPK_MAX = 256
Q_BLOCK = 128
ROT_DIM = DH_B // 4
ROPE_THETA = 500000.0
D_FF = 2816
PLE_DIM = 256
EPS = 1e-6

MIX_SIZES = (W_A, W_A, W_A, W_A, H_A, H_A,
             W_B, W_B, W_B, HI * DI, DI, HI,
             D_MODEL, D_MODEL)
MIX_COLS = sum(MIX_SIZES)

kernel_name = "hybrid_gdn_dsa_macaron_layer"


def rmsnorm(x, g):
    x32 = x.astype(jnp.float32)
    y = x32 * lax.rsqrt(jnp.mean(x32 * x32, axis=-1, keepdims=True) + EPS)
    return (y * g.astype(jnp.float32)).astype(x.dtype)


def l2norm(x):
    x32 = x.astype(jnp.float32)
    return x32 * lax.rsqrt(jnp.sum(x32 * x32, axis=-1, keepdims=True) + EPS)


def swiglu(u, w_in, w_out):
    gate, up = jnp.split(u @ w_in, 2, axis=-1)
    return (jax.nn.silu(gate) * up) @ w_out


def rope_tables(seq_len):
    pos = jnp.arange(seq_len, dtype=jnp.float32)
    inv = ROPE_THETA ** (-jnp.arange(0, ROT_DIM, 2, dtype=jnp.float32) / ROT_DIM)
    ang = pos[:, None] * inv[None, :]
    return jnp.cos(ang), jnp.sin(ang)


def partial_rope(x, cos, sin):
    c = cos[None, :, None, :].astype(x.dtype)
    s = sin[None, :, None, :].astype(x.dtype)
    half = ROT_DIM // 2
    x1, x2, xp = x[..., :half], x[..., half:ROT_DIM], x[..., ROT_DIM:]
    return jnp.concatenate([x1 * c - x2 * s, x2 * c + x1 * s, xp], axis=-1)


def causal_conv(x, w):
    k = w.shape[0]
    return lax.conv_general_dilated(
        x, w[:, None, :], window_strides=(1,), padding=[(k - 1, 0)],
        dimension_numbers=("NWC", "WIO", "NWC"), feature_group_count=x.shape[-1])


def gated_delta_rule(q, k, v, g, beta):
    B, S, H, DK = q.shape
    DV = v.shape[-1]
    N, C = S // CHUNK, CHUNK

    def chunks(t):
        return t.reshape(B, N, C, H, -1).transpose(0, 3, 1, 2, 4)

    q, k, v = chunks(q), chunks(k), chunks(v)
    g = g.reshape(B, N, C, H).transpose(0, 3, 1, 2)
    beta = beta.reshape(B, N, C, H).transpose(0, 3, 1, 2)
    gc = jnp.cumsum(g, axis=-1)
    idx = jnp.arange(C)
    incl = idx[:, None] >= idx[None, :]
    strict = idx[:, None] > idx[None, :]
    decay = jnp.exp(jnp.where(incl, gc[..., :, None] - gc[..., None, :], -jnp.inf))
    kk = jnp.einsum("bhnid,bhnjd->bhnij", k, k)
    a_mat = jnp.where(strict, beta[..., :, None] * kk * decay, 0.0) + jnp.eye(C, dtype=jnp.float32)
    rhs = jnp.concatenate([v * beta[..., None], k * (beta * jnp.exp(gc))[..., None]], axis=-1)
    sol = lax.linalg.triangular_solve(a_mat, rhs, left_side=True, lower=True, unit_diagonal=True)
    u_val, w_dec = sol[..., :DV], sol[..., DV:]
    qk = jnp.einsum("bhnid,bhnjd->bhnij", q, k) * decay
    q_dec = q * jnp.exp(gc)[..., None]
    k_dec = k * jnp.exp(gc[..., -1:] - gc)[..., None]
    g_tot = jnp.exp(gc[..., -1])

    def step(state, inp):
        u_c, w_c, qk_c, qd_c, kd_c, gt_c = inp
        v_new = u_c - jnp.einsum("bhcd,bhde->bhce", w_c, state)
        o = jnp.einsum("bhcd,bhde->bhce", qd_c, state) + jnp.einsum("bhij,bhje->bhie", qk_c, v_new)
        state = state * gt_c[..., None, None] + jnp.einsum("bhcd,bhce->bhde", kd_c, v_new)
        return state, o

    xs = tuple(jnp.moveaxis(t, 2, 0) for t in (u_val, w_dec, qk, q_dec, k_dec, g_tot))
    s0 = jnp.zeros((B, H, DK, DV), jnp.float32)
    _, o = lax.scan(step, s0, xs)
    return o.transpose(1, 0, 3, 2, 4).reshape(B, S, H, DV)


def dsa_attention(q, k, v, q_idx, k_idx, w_idx):
    B, S, H, Dh = q.shape
    n_blk = S // Q_BLOCK
    top = min(TOPK_MAX, S // 4)
    key_pos = jnp.arange(S)
    gather = jax.vmap(lambda t, i: t[i])

    def to_blocks(t):
        return jnp.moveaxis(t.reshape(B, n_blk, Q_BLOCK, *t.shape[2:]), 1, 0)

    def one_block(args):
        qb, qib, wb, start = args
        qpos = start + jnp.arange(Q_BLOCK)
        causal = key_pos[None, :] <= qpos[:, None]
        dots = jnp.einsum("bqhd,bsd->bqhs", qib, k_idx).astype(jnp.float32) * (DI ** -0.5)
        score = jnp.einsum("bqh,bqhs->bqs", wb.astype(jnp.float32) * (HI ** -0.5), jax.nn.relu(dots))
        score = jnp.where(causal[None], score, -jnp.inf)
        _, sel = lax.top_k(score, top)
        valid = sel <= qpos[None, :, None]
        k_sel = gather(k, sel)
        v_sel = gather(v, sel)
        att = jnp.einsum("bqhd,bqkhd->bhqk", qb, k_sel).astype(jnp.float32) * (Dh ** -0.5)
        att = jnp.where(valid[:, None], att, -jnp.inf)
        prob = jax.nn.softmax(att, axis=-1).astype(v.dtype)
        return jnp.einsum("bhqk,bqkhd->bqhd", prob, v_sel)

    starts = jnp.arange(n_blk) * Q_BLOCK
    out = lax.map(one_block, (to_blocks(q), to_blocks(q_idx), to_blocks(w_idx), starts))
    return jnp.moveaxis(out, 0, 1).reshape(B, S, H, Dh)


def token_mixer(u, w_in, conv_w, a_log, dt_bias, dn_norm_g, idx_k_norm_g,
                w_br_a, w_br_b, w_out, cos, sin):
    B, S, _ = u.shape
    offsets = np.cumsum(np.array(MIX_SIZES))[:-1].tolist()
    (q_a, k_a, v_a, z_a, a_a, b_a, q_b, k_b, v_b, q_i, k_i, w_i, g_a, g_b) = jnp.split(
        u @ w_in, offsets, axis=-1)

    qkv = jax.nn.silu(causal_conv(jnp.concatenate([q_a, k_a, v_a], axis=-1), conv_w))
    q_a, k_a, v_a = jnp.split(qkv, 3, axis=-1)
    qa = l2norm(q_a.reshape(B, S, H_A, DK_A)) * (DK_A ** -0.5)
    ka = l2norm(k_a.reshape(B, S, H_A, DK_A))
    va = v_a.reshape(B, S, H_A, DV_A).astype(jnp.float32)
    g = -jnp.exp(a_log.astype(jnp.float32)) * jax.nn.softplus(
        a_a.astype(jnp.float32) + dt_bias.astype(jnp.float32))
    beta = jax.nn.sigmoid(b_a.astype(jnp.float32))
    o_a = gated_delta_rule(qa, ka, va, g, beta).astype(u.dtype)
    o_a = rmsnorm(o_a, dn_norm_g) * jax.nn.silu(z_a.reshape(B, S, H_A, DV_A))
    y_a = o_a.reshape(B, S, W_A) @ w_br_a

    qb = partial_rope(q_b.reshape(B, S, H_B, DH_B), cos, sin)
    kb = partial_rope(k_b.reshape(B, S, H_B, DH_B), cos, sin)
    vb = v_b.reshape(B, S, H_B, DH_B)
    qi = partial_rope(q_i.reshape(B, S, HI, DI), cos, sin)
    ki = partial_rope(rmsnorm(k_i, idx_k_norm_g)[:, :, None, :], cos, sin)[:, :, 0, :]
    o_b = dsa_attention(qb, kb, vb, qi, ki, w_i)
    y_b = o_b.reshape(B, S, W_B) @ w_br_b

    merged = jax.nn.sigmoid(g_a) * y_a + jax.nn.sigmoid(g_b) * y_b
    return merged @ w_out


def setup_inputs(seed: int = 0) -> dict:
    key = jax.random.key(seed)
    ks = iter(jax.random.split(key, 40))
    f32 = jnp.float32
    L, D = DEPTH, D_MODEL

    def nrm(shape, fan_in):
        return jax.random.normal(next(ks), shape, f32) * (fan_in ** -0.5)

    def gain(shape):
        return 1.0 + 0.02 * jax.random.normal(next(ks), shape, f32)

    x = jax.random.normal(next(ks), (BATCH, SEQ, D), f32)
    p = jax.random.normal(next(ks), (DEPTH, BATCH, SEQ, PLE_DIM), f32)
    a_log = jnp.log(jax.random.uniform(next(ks), (L, H_A), f32, 1.0, 16.0))
    dt = jnp.exp(jax.random.uniform(next(ks), (L, H_A), f32, math.log(0.001), math.log(0.1)))
    dt_bias = dt + jnp.log(-jnp.expm1(-dt))
    return {
        "x": x,
        "p": p,
        "ffn1_norm_pre": gain((L, D)),
        "ffn1_norm_post": gain((L, D)),
        "ffn1_w_in": nrm((L, D, 2 * D_FF), D),
        "ffn1_w_out": nrm((L, D_FF, D), D_FF),
        "mix_norm_pre": gain((L, D)),
        "mix_norm_post": gain((L, D)),
        "mix_w_in": nrm((L, D, MIX_COLS), D),
        "conv_w": nrm((L, CONV_K, 3 * W_A), CONV_K),
        "a_log": a_log,
        "dt_bias": dt_bias,
        "dn_norm_g": gain((L, DV_A)),
        "idx_k_norm_g": gain((L, DI)),
        "w_br_a": nrm((L, W_A, D), W_A),
        "w_br_b": nrm((L, W_B, D), W_B),
        "mix_w_out": nrm((L, D, D), D),
        "ffn2_norm_pre": gain((L, D)),
        "ffn2_norm_post": gain((L, D)),
        "ffn2_w_in": nrm((L, D, 2 * D_FF), D),
        "ffn2_w_out": nrm((L, D_FF, D), D_FF),
        "ple_norm_pre": gain((L, D)),
        "ple_norm_post": gain((L, D)),
        "ple_w_gate": nrm((L, D, D), D),
        "ple_w_proj": nrm((L, PLE_DIM, D), PLE_DIM),
    }


def reference(x, p, ffn1_norm_pre, ffn1_norm_post, ffn1_w_in, ffn1_w_out,
              mix_norm_pre, mix_norm_post, mix_w_in, conv_w, a_log, dt_bias,
              dn_norm_g, idx_k_norm_g, w_br_a, w_br_b, mix_w_out,
              ffn2_norm_pre, ffn2_norm_post, ffn2_w_in, ffn2_w_out,
              ple_norm_pre, ple_norm_post, ple_w_gate, ple_w_proj):
    cos, sin = rope_tables(x.shape[1])
    h = x
    for i in range(DEPTH):
        h = h + 0.5 * rmsnorm(swiglu(rmsnorm(h, ffn1_norm_pre[i]), ffn1_w_in[i], ffn1_w_out[i]),
                              ffn1_norm_post[i])
        mix = token_mixer(rmsnorm(h, mix_norm_pre[i]), mix_w_in[i], conv_w[i], a_log[i], dt_bias[i],
                          dn_norm_g[i], idx_k_norm_g[i], w_br_a[i], w_br_b[i], mix_w_out[i], cos, sin)
        h = h + rmsnorm(mix, mix_norm_post[i])
        h = h + 0.5 * rmsnorm(swiglu(rmsnorm(h, ffn2_norm_pre[i]), ffn2_w_in[i], ffn2_w_out[i]),
                              ffn2_norm_post[i])
        gate = jax.nn.sigmoid(rmsnorm(h, ple_norm_pre[i]) @ ple_w_gate[i])
        h = h + rmsnorm(gate * (p[i] @ ple_w_proj[i]), ple_norm_post[i])
    return h
```

```python
import os
import numpy as np
import concourse.bass as bass
import concourse.mybir as mybir
from concourse.bass_utils import run_bass_kernel_spmd
from contextlib import ExitStack

F32 = mybir.dt.float32
BF16 = mybir.dt.bfloat16
U8 = mybir.dt.uint8
ALU = mybir.AluOpType
AF = mybir.ActivationFunctionType
AX = mybir.AxisListType

ENGS = ("pe", "act", "dve", "pool", "sp")

S = 4096
D = 1024
DFF = 2816
NFC = DFF // 128
EPS = 1e-6
TF = 512
TM = 512


class Buf:
    __slots__ = ("name", "w", "r", "dkey", "excl")

    def __init__(self, name, dkey=None):
        self.name = name
        self.w = {}
        self.r = {}
        self.dkey = dkey
        self.excl = False


class Prog:
    def __init__(self, nc):
        self.nc = nc
        self.streams = {e: [] for e in ENGS}
        self.cnt = {e: 0 for e in ENGS}
        self.seen = {e: {} for e in ENGS}
        self.dma_keys = {}
        self.bufs = []
        self.pass_idx = 0
        self.persistent = True

    def buf(self, name, dma_dst=False):
        dkey = None
        if dma_dst:
            if self.persistent:
                dkey = ("d", name)
            else:
                dkey = ("p", self.pass_idx)
                self.pass_idx += 1
            self.dma_keys.setdefault(dkey, 0)
        b = Buf(name, dkey)
        self.bufs.append(b)
        return b

    def _emit(self, eng, fn, reads, writes, tok_key, tok_inc):
        waits = {}
        for b in reads:
            for k, v in b.w.items():
                if waits.get(k, 0) < v:
                    waits[k] = v
            if b.excl:
                for k, v in b.r.items():
                    if k != tok_key and waits.get(k, 0) < v:
                        waits[k] = v
        for b in writes:
            for k, v in b.w.items():
                if waits.get(k, 0) < v:
                    waits[k] = v
            for k, v in b.r.items():
                if waits.get(k, 0) < v:
                    waits[k] = v
        seen = self.seen[eng]
        wl = []
        for k, v in waits.items():
            if k == tok_key and (eng == "pe" or isinstance(k, tuple)):
                continue
            if seen.get(k, 0) >= v:
                continue
            seen[k] = v
            wl.append((k, v))
        if isinstance(tok_key, tuple):
            self.dma_keys[tok_key] += tok_inc
            val = self.dma_keys[tok_key]
        else:
            self.cnt[eng] += 1
            val = self.cnt[eng]
        for b in reads:
            if b.r.get(tok_key, 0) < val:
                b.r[tok_key] = val
        for b in writes:
            b.w = {tok_key: val}
            b.r = {}
        self.streams[eng].append((wl, fn, tok_key, tok_inc))

    def op(self, eng, fn, reads=(), writes=()):
        self._emit(eng, fn, list(reads), list(writes), eng, 1)

    def dma(self, eng, fn, reads, writes):
        dst = writes[0]
        assert dst.dkey is not None, dst.name
        self._emit(eng, fn, list(reads), list(writes), dst.dkey, 16)

    def barrier(self):
        allw = {e: self.cnt[e] for e in ENGS if self.cnt[e] > 0}
        for k, v in self.dma_keys.items():
            if v > 0:
                allw[k] = v
        for eng in ENGS:
            seen = self.seen[eng]
            wl = []
            for k, v in allw.items():
                if k == eng:
                    continue
                if seen.get(k, 0) >= v:
                    continue
                seen[k] = v
                wl.append((k, v))
            if wl:
                self.streams[eng].append((wl, None, None, 0))
        for b in self.bufs:
            b.w = {}
            b.r = {}
        self.pass_idx = 0

    def replay(self):
        nc = self.nc
        with ExitStack() as st:
            sems = {}
            for e in ENGS:
                if self.cnt[e] > 0:
                    sems[e] = st.enter_context(nc.semaphore("s_" + e))
            ndma = 0
            for i, k in enumerate(self.dma_keys):
                if self.dma_keys[k] > 0:
                    sems[k] = st.enter_context(nc.semaphore("sd%d" % i))
                    ndma += 1
            self.n_dma_sems = ndma
            block = st.enter_context(nc.Block())
            streams = self.streams

            def run(name, eng):
                for wl, fn, tk, inc in streams[name]:
                    for k, v in wl:
                        eng.wait_ge(sems[k], v)
                    if fn is not None:
                        fn(eng).then_inc(sems[tk], inc)

            @block.tensor
            def _(e):
                run("pe", e)

            @block.scalar
            def _(e):
                run("act", e)

            @block.vector
            def _(e):
                run("dve", e)

            @block.gpsimd
            def _(e):
                run("pool", e)

            @block.sync
            def _(e):
                run("sp", e)


class Carver:
    def __init__(self, big, start, limit):
        self.big = big
        self.off = start
        self.limit = limit

    def t(self, shape, dt, parts=128):
        esz = 4 if dt == F32 else 2
        n = int(np.prod(shape[1:])) * esz
        off = (self.off + 31) // 32 * 32
        assert off + n <= self.limit, ("SBUF overflow", off + n, self.limit)
        v = self.big[0:shape[0], off:off + n].bitcast(dt)
        if len(shape) == 3:
            v = v.rearrange("p (a b) -> p a b", b=shape[2])
        elif len(shape) == 4:
            v = v.rearrange("p (a b c) -> p a b c", b=shape[2], c=shape[3])
        self.off = off + n
        return v


C_IDENT = 0
C_ONESD = 128
C_BLK64 = 256
C_U64 = 384
C_ONES64 = 448
C_SL = 512
C_UI = 576
C_CAUS = 640
C_NEGC = 768
C_POW2 = 896
C_123 = 928
C_POW4 = 932
C_N = 948


def host_consts():
    c = np.zeros((128, C_N), np.float32)
    c[:, C_IDENT:C_IDENT + 128] = np.eye(128)
    c[:, C_ONESD:C_ONESD + 128] = 1.0 / D
    c[0:64, C_BLK64:C_BLK64 + 64] = 1.0
    c[64:128, C_BLK64 + 64:C_BLK64 + 128] = 1.0
    i = np.arange(64)
    c[0:64, C_U64:C_U64 + 64] = (i[:, None] <= i[None, :])
    c[0:64, C_ONES64:C_ONES64 + 64] = 1.0
    c[0:64, C_SL:C_SL + 64] = (i[:, None] > i[None, :])
    c[0:64, C_UI:C_UI + 64] = (i[None, :] >= i[:, None])
    t = np.arange(128)
    c[:, C_CAUS:C_CAUS + 128] = (t[None, :] <= t[:, None])
    c[:, C_NEGC:C_NEGC + 128] = np.where(t[None, :] <= t[:, None], 0.0, -1e30)
    c[:, C_POW2:C_POW2 + 32] = 2.0 ** -(np.arange(32) + 1.0)
    c[:, C_123:C_123 + 3] = np.array([1.0, 2.0, 3.0])
    c[:, C_POW4:C_POW4 + 16] = 4.0 ** -(np.arange(16) + 1.0)
    return c


class K:
    pass


def build(debug=None):
    nc = bass.Bass("TRN2", target_bir_lowering=False)
    k = K()
    build.k = k
    k.nc = nc
    k.debug = debug
    P = Prog(nc)
    k.P = P

    k.in_names = []
    skip_pre = bool(debug and "skip_pre" in debug)

    def din(name, shape, dt=F32):
        if skip_pre and int(np.prod(shape)) > 200000:
            return None
        k.in_names.append(name)
        return nc.dram_tensor(name, list(shape), dt, kind="ExternalInput").ap()

    def dscr(name, shape, dt=F32):
        kind = "ExternalOutput" if (debug and name in debug) else "Internal"
        return nc.dram_tensor(name, list(shape), dt, kind=kind).ap()

    k.xT = din("xT", [D, S])
    k.pT = din("pT", [256, S])
    k.consts = din("consts", [128, C_N])
    k.gains = din("gains", [128, 64])
    k.ffn_w_in = [din("ffn1_w_in", [128, 8, 2 * DFF]), din("ffn2_w_in", [128, 8, 2 * DFF])]
    k.ffn_w_out = [din("ffn1_w_out", [128, NFC, D]), din("ffn2_w_out", [128, NFC, D])]
    k.outT = nc.dram_tensor("outT", [D, S], F32, kind="ExternalOutput").ap()
    k.mix_w1 = din("mix_w1", [128, 8, NW1])
    k.mix_w2 = din("mix_w2", [128, 8, 2048])
    k.w_br_a = din("w_br_a", [128, 4, D])
    k.w_br_b = din("w_br_b", [128, 4, D])
    k.mix_w_out = din("mix_w_out", [128, 8, D])
    k.ple_wg = din("ple_wg", [128, 8, D])
    k.ple_wp = din("ple_wp", [128, 2, D])
    k.conv_w = din("conv_w", [128, 12, 4])
    k.rows_d = din("rows", [128, R_N])
    k.cs_d = din("cs", [128, 32, 16])
    k.h1T = dscr("h1T", [D, S])
    k.d = {}
    k.bd = {}
    for nm, shp, dt in (("qaT", [512, S], BF16), ("kaT", [512, S], BF16), ("ka_tok", [S, 512], BF16), ("va_tok", [S, 512], BF16),
                        ("z_tok", [S, 512], F32), ("gb_tok", [S, 16], F32), ("qbT", [8, 65, S], BF16), ("kbT", [8, 65, S], BF16),
                        ("vb_tok", [S, 8 * 65], BF16), ("qiT", [8, 64, S], BF16), ("kiT", [64, S], BF16), ("wi_tok", [S, 8], F32),
                        ("oaT", [512, S], BF16), ("obT", [512, S], BF16), ("h2T", [D, S], F32), ("h3T", [D, S], F32)):
        k.d[nm] = dscr(nm, shp, dt)
        k.bd[nm] = P.buf("d_" + nm, True)

    big = nc.alloc_sbuf_tensor("big", [128, 212480], U8)
    k.big = big
    k.ps = [nc.alloc_psum_tensor("ps%d" % i, [128, 512], F32) for i in range(8)]
    k.b_ps = [P.buf("ps%d" % i) for i in range(8)]
    for b_ in k.b_ps:
        b_.excl = True
    k.ps_i = 0
    k.ps_pool = list(range(8))
    k.ps_held = set()
    k.gdn_n = 64
    k.gdn_stages = None
    if debug:
        for f in debug:
            if f.startswith("gdn_n="):
                k.gdn_n = int(f.split("=")[1])
            if f.startswith("gdn_stages="):
                k.gdn_stages = int(f.split("=")[1])

    cv = Carver(big, 0, 9216)
    k.c32 = cv.t([128, C_N], F32)
    k.cbf = cv.t([128, C_N], BF16)
    k.gsb = cv.t([128, 64], F32)
    k.epsb = cv.t([128, 1], F32)
    k.rows = cv.t([128, R_N], F32)
    k.cs = cv.t([128, 32, 16], F32)
    k.kmax = cv.t([128, 8], F32)
    k.oneb = cv.t([128, 1], F32)
    k.b_rows = P.buf("rows", True)
    k.b_cs = P.buf("cs", True)
    k.b_kmax = P.buf("kmax")
    P.dma("sp", lambda e: e.dma_start(out=k.rows, in_=k.rows_d), [], [k.b_rows])
    P.dma("sp", lambda e: e.dma_start(out=k.cs, in_=k.cs_d), [], [k.b_cs])
    k.b_c = P.buf("consts", True)
    k.b_cbf = P.buf("cbf")
    k.b_g = P.buf("gains", True)
    k.b_eps = P.buf("eps")
    P.dma("sp", lambda e: e.dma_start(out=k.c32, in_=k.consts), [], [k.b_c])
    P.dma("sp", lambda e: e.dma_start(out=k.gsb, in_=k.gains), [], [k.b_g])
    P.op("dve", lambda e: e.tensor_copy(out=k.cbf, in_=k.c32), [k.b_c], [k.b_cbf])
    P.op("dve", lambda e: e.memset(k.epsb, EPS), [], [k.b_eps])
    P.op("dve", lambda e: e.memset(k.oneb, 1.0), [], [k.b_eps])
    k.PH0 = 9216
    P.persistent = False
    k.PHLIM = 212480

    if not (debug and "skip_pre" in debug):
        ffn_pass(k, 0, k.xT, k.h1T, final=False)
        P.barrier()
        if debug and "stop_after_ffn1" in debug:
            P.replay()
            return nc
        mixproj_pass(k)
        P.barrier()
    if debug and "stop_after_proj" in debug:
        P.replay()
        return nc
    gdn_pass(k)
    P.barrier()
    if debug and "stop_after_gdn" in debug:
        P.replay()
        return nc
    dsa_pass(k)
    P.barrier()
    if debug and "stop_after_dsa" in debug:
        P.replay()
        return nc
    merge_pass(k)
    P.barrier()
    ffn_pass(k, 1, k.d["h2T"], k.d["h3T"], final=False)
    P.barrier()
    ple_pass(k)
    P.barrier()
    P.replay()
    return nc


def O(k, eng, method, reads, writes, **kw):
    k.P.op(eng, lambda e: getattr(e, method)(**kw), reads, writes)


def DMA(k, eng, reads, writes, **kw):
    k.P.dma(eng, lambda e: e.dma_start(**kw), reads, writes)


def next_ps(k, hold=False):
    pool = k.ps_pool
    for _ in range(len(pool)):
        i = pool[k.ps_i % len(pool)]
        k.ps_i = (k.ps_i + 1) % len(pool)
        if i not in k.ps_held:
            if hold:
                k.ps_held.add(i)
            return k.ps[i], k.b_ps[i]
    raise RuntimeError("out of PSUM banks")


def rel_ps(k, b_ps):
    k.ps_held.discard(k.b_ps.index(b_ps))


def rms_stats(k, sq, b_sq, rstd, b_rstd, T, nchunk=8):
    P = k.P
    ps, b_ps = next_ps(k)
    ones = k.cbf[:, C_ONESD:C_ONESD + 128]
    for c in range(nchunk):
        P.op("pe", lambda e, c=c: e.matmul(ps[:, 0:T], lhsT=ones, rhs=sq[:, c, :], start=(c == 0), stop=(c == nchunk - 1)),
             [k.b_cbf, b_sq], [b_ps])
    P.op("act", lambda e: e.activation(out=rstd, in_=ps[:, 0:T], func=AF.Sqrt, bias=k.epsb, scale=1.0),
         [b_ps, k.b_eps], [b_rstd])
    P.op("dve", lambda e: e.reciprocal(out=rstd, in_=rstd), [b_rstd], [b_rstd])


def ffn_pass(k, which, srcT, dstT, final):
    P = k.P
    nc = k.nc
    T = TF
    cv = Carver(k.big, k.PH0, k.PHLIM)
    w_in = cv.t([128, 8, 2 * DFF], BF16)
    w_out = cv.t([128, NFC, D], BF16)
    hT = [cv.t([128, 8, T], F32)] * 2
    su = cv.t([128, 8, T], BF16)
    a = cv.t([128, NFC, T], BF16)
    y = cv.t([128, 8, T], F32)
    rstd = cv.t([128, T], F32)
    sg = [cv.t([128, T], F32)] * 2
    b_win = [P.buf("w_in%d_%d" % (which, c), True) for c in range(8)]
    b_wout = [P.buf("w_out%d_%d" % (which, c), True) for c in range(2)]
    b_h = [P.buf("hT%d" % which, True)] * 2
    b_su = P.buf("su%d" % which)
    b_a = [P.buf("a%d_%d" % (which, i)) for i in range(NFC)]
    b_y = P.buf("y%d" % which)
    b_rstd = P.buf("rstd%d" % which)
    b_sg = [P.buf("sg%d" % which)] * 2
    b_dst = P.buf("dst%d" % which, True)
    if final:
        wg = cv.t([128, 8, D], BF16)
        wp = cv.t([128, 2, D], BF16)
        pt = [cv.t([128, 2, T], BF16) for _ in range(2)]
        b_wg = P.buf("ple_wg", True)
        b_wp = P.buf("ple_wp", True)
        b_pt = [P.buf("ple_pt%d" % i, True) for i in range(2)]
        DMA(k, "pool", [], [b_wg], out=wg, in_=k.ple_wg)
        DMA(k, "pool", [], [b_wp], out=wp, in_=k.ple_wp)
    g_pre = k.gsb[:, 0:8] if which == 0 else k.gsb[:, 32:40]
    g_post = k.gsb[:, 8:16] if which == 0 else k.gsb[:, 40:48]
    wi_d = k.ffn_w_in[which]
    wo_d = k.ffn_w_out[which]
    for c in range(8):
        P.dma("pool", lambda e, c=c: e.dma_start(out=w_in[:, c, :], in_=wi_d[:, c, :]), [], [b_win[c]])
    for c in range(2):
        P.dma("pool", lambda e, c=c: e.dma_start(out=w_out[:, c * 11:(c + 1) * 11, :], in_=wo_d[:, c * 11:(c + 1) * 11, :]),
              [], [b_wout[c]])
    ghalf = cv.t([128, 8], F32)
    b_gh = P.buf("ghalf%d" % which)
    P.op("dve", lambda e: e.tensor_scalar(out=ghalf, in0=g_post, scalar1=0.5, scalar2=None, op0=ALU.mult),
         [k.b_g], [b_gh])
    ntile = S // T
    for it in range(ntile):
        t0 = it * T
        hb = hT[it % 2]
        bh = b_h[it % 2]
        P.dma("sp", lambda e, hb=hb, t0=t0: e.dma_start(out=hb, in_=srcT[:, t0:t0 + T].rearrange("(c p) t -> p c t", p=128)),
              [], [bh])
        for c in range(8):
            P.op("act", lambda e, c=c, hb=hb: e.activation(out=su[:, c, :], in_=hb[:, c, :], func=AF.Square), [bh], [b_su])
        rms_stats(k, su, b_su, rstd, b_rstd, T)
        for c in range(8):
            P.op("dve", lambda e, c=c, hb=hb: e.scalar_tensor_tensor(out=su[:, c, :], in0=hb[:, c, :], scalar=g_pre[:, c:c + 1],
                                                                   in1=rstd, op0=ALU.mult, op1=ALU.mult),
                 [bh, k.b_g, b_rstd], [b_su])
        for fc in range(NFC):
            psg, b_psg = next_ps(k)
            psu, b_psu = next_ps(k)
            for c in range(8):
                P.op("pe", lambda e, c=c, fc=fc, psg=psg: e.matmul(psg[:, 0:T], lhsT=w_in[:, c, fc * 128:(fc + 1) * 128],
                                                                 rhs=su[:, c, :], start=(c == 0), stop=(c == 7)),
                     [b_win[c], b_su], [b_psg])
            for c in range(8):
                P.op("pe", lambda e, c=c, fc=fc, psu=psu: e.matmul(psu[:, 0:T], lhsT=w_in[:, c, DFF + fc * 128:DFF + (fc + 1) * 128],
                                                                 rhs=su[:, c, :], start=(c == 0), stop=(c == 7)),
                     [b_win[c], b_su], [b_psu])
            sgb = sg[fc % 2]
            bsg = b_sg[fc % 2]
            P.op("act", lambda e, sgb=sgb, psg=psg: e.activation(out=sgb, in_=psg[:, 0:T], func=AF.Silu), [b_psg], [bsg])
            P.op("dve", lambda e, sgb=sgb, psu=psu, fc=fc: e.tensor_tensor(out=a[:, fc, :], in0=sgb, in1=psu[:, 0:T], op=ALU.mult),
                 [bsg, b_psu], [b_a[fc]])
        for dc in range(8):
            psy, b_psy = next_ps(k)
            for fc in range(NFC):
                P.op("pe", lambda e, dc=dc, fc=fc, psy=psy: e.matmul(psy[:, 0:T], lhsT=w_out[:, fc, dc * 128:(dc + 1) * 128],
                                                                   rhs=a[:, fc, :], start=(fc == 0), stop=(fc == NFC - 1)),
                     [b_wout[fc // 11], b_a[fc]], [b_psy])
            P.op("act", lambda e, dc=dc, psy=psy: e.activation(out=y[:, dc, :], in_=psy[:, 0:T], func=AF.Copy), [b_psy], [b_y])
            P.op("act", lambda e, dc=dc, psy=psy: e.activation(out=su[:, dc, :], in_=psy[:, 0:T], func=AF.Square), [b_psy], [b_su])
        rms_stats(k, su, b_su, rstd, b_rstd, T)
        for c in range(8):
            P.op("dve", lambda e, c=c: e.scalar_tensor_tensor(out=y[:, c, :], in0=y[:, c, :], scalar=ghalf[:, c:c + 1],
                                                            in1=rstd, op0=ALU.mult, op1=ALU.mult),
                 [b_y, b_gh, b_rstd], [b_y])
        for c in range(8):
            P.op("pool", lambda e, c=c, hb=hb: e.tensor_tensor(out=hb[:, c, :], in0=hb[:, c, :], in1=y[:, c, :], op=ALU.add),
                 [bh, b_y], [bh])
        if not final:
            P.dma("sp", lambda e, hb=hb, t0=t0: e.dma_start(out=dstT[:, t0:t0 + T].rearrange("(c p) t -> p c t", p=128), in_=hb),
                  [bh], [b_dst])
        else:
            gp_pre = k.gsb[:, 48:56]
            gp_post = k.gsb[:, 56:64]
            DMA(k, "pool", [], [b_pt[it % 2]], out=pt[it % 2], in_=k.pT[:, t0:t0 + T].rearrange("(c p) t -> p c t", p=128))
            for c in range(8):
                P.op("act", lambda e, c=c, hb=hb: e.activation(out=su[:, c, :], in_=hb[:, c, :], func=AF.Square), [bh], [b_su])
            rms_stats(k, su, b_su, rstd, b_rstd, T)
            for c in range(8):
                P.op("dve", lambda e, c=c, hb=hb: e.scalar_tensor_tensor(out=su[:, c, :], in0=hb[:, c, :], scalar=gp_pre[:, c:c + 1],
                                                                       in1=rstd, op0=ALU.mult, op1=ALU.mult),
                     [bh, k.b_g, b_rstd], [b_su])
            for dc in range(8):
                psg, b_psg = next_ps(k)
                for c in range(8):
                    O(k, "pe", "matmul", [b_wg, b_su], [b_psg], out=psg[:, 0:T], lhsT=wg[:, c, dc * 128:(dc + 1) * 128], rhs=su[:, c, :],
                      start=(c == 0), stop=(c == 7))
                psp, b_psp = next_ps(k)
                for c in range(2):
                    O(k, "pe", "matmul", [b_wp, b_pt[it % 2]], [b_psp], out=psp[:, 0:T], lhsT=wp[:, c, dc * 128:(dc + 1) * 128],
                      rhs=pt[it % 2][:, c, :], start=(c == 0), stop=(c == 1))
                sgb = sg[dc % 2]
                bsg = b_sg[dc % 2]
                O(k, "act", "activation", [b_psg], [bsg], out=sgb, in_=psg[:, 0:T], func=AF.Sigmoid)
                O(k, "dve", "tensor_tensor", [bsg, b_psp], [b_y], out=y[:, dc, :], in0=sgb, in1=psp[:, 0:T], op=ALU.mult)
            for c in range(8):
                O(k, "act", "activation", [b_y], [b_su], out=su[:, c, :], in_=y[:, c, :], func=AF.Square)
            rms_stats(k, su, b_su, rstd, b_rstd, T)
            for c in range(8):
                O(k, "dve", "scalar_tensor_tensor", [b_y, k.b_g, b_rstd], [b_y], out=y[:, c, :], in0=y[:, c, :], scalar=gp_post[:, c:c + 1],
                  in1=rstd, op0=ALU.mult, op1=ALU.mult)
            for c in range(8):
                O(k, "pool", "tensor_tensor", [bh, b_y], [bh], out=hb[:, c, :], in0=hb[:, c, :], in1=y[:, c, :], op=ALU.add)
            DMA(k, "sp", [bh], [b_dst], out=dstT[:, t0:t0 + T].rearrange("(c p) t -> p c t", p=128), in_=hb)


def ple_pass(k):
    P = k.P
    B = P.buf
    T = 512
    cv = Carver(k.big, k.PH0, k.PHLIM)
    wg = cv.t([128, 8, D], BF16)
    wp = cv.t([128, 2, D], BF16)
    hT = [cv.t([128, 8, T], F32) for _ in range(2)]
    pt = [cv.t([128, 2, T], BF16) for _ in range(2)]
    su = cv.t([128, 8, T], BF16)
    rstd = cv.t([128, T], F32)
    sg = [cv.t([128, T], F32) for _ in range(2)]
    y = cv.t([128, 8, T], F32)
    b_wg = B("ple_wg", True)
    b_wp = B("ple_wp", True)
    b_h = [B("ple_h%d" % i, True) for i in range(2)]
    b_pt = [B("ple_pt%d" % i, True) for i in range(2)]
    b_su = B("ple_su")
    b_rstd = B("ple_rstd")
    b_sg = [B("ple_sg%d" % i) for i in range(2)]
    b_y = B("ple_y")
    b_dst = B("ple_dst", True)
    gp_pre = k.gsb[:, 48:56]
    gp_post = k.gsb[:, 56:64]
    DMA(k, "pool", [], [b_wg], out=wg, in_=k.ple_wg)
    DMA(k, "pool", [], [b_wp], out=wp, in_=k.ple_wp)
    src = k.d["h3T"]
    for it in range(S // T):
        t0 = it * T
        hb = hT[it % 2]
        bh = b_h[it % 2]
        DMA(k, "sp", [], [bh], out=hb, in_=src[:, t0:t0 + T].rearrange("(c p) t -> p c t", p=128))
        DMA(k, "pool", [], [b_pt[it % 2]], out=pt[it % 2], in_=k.pT[:, t0:t0 + T].rearrange("(c p) t -> p c t", p=128))
        for c in range(8):
            O(k, "act", "activation", [bh], [b_su], out=su[:, c, :], in_=hb[:, c, :], func=AF.Square)
        rms_stats(k, su, b_su, rstd, b_rstd, T)
        for c in range(8):
            O(k, "dve", "scalar_tensor_tensor", [bh, k.b_g, b_rstd], [b_su], out=su[:, c, :], in0=hb[:, c, :], scalar=gp_pre[:, c:c + 1],
              in1=rstd, op0=ALU.mult, op1=ALU.mult)
        for dc in range(8):
            psg, b_psg = next_ps(k)
            for c in range(8):
                O(k, "pe", "matmul", [b_wg, b_su], [b_psg], out=psg[:, 0:T], lhsT=wg[:, c, dc * 128:(dc + 1) * 128], rhs=su[:, c, :],
                  start=(c == 0), stop=(c == 7))
            psp, b_psp = next_ps(k)
            for c in range(2):
                O(k, "pe", "matmul", [b_wp, b_pt[it % 2]], [b_psp], out=psp[:, 0:T], lhsT=wp[:, c, dc * 128:(dc + 1) * 128],
                  rhs=pt[it % 2][:, c, :], start=(c == 0), stop=(c == 1))
            sgb = sg[dc % 2]
            bsg = b_sg[dc % 2]
            O(k, "act", "activation", [b_psg], [bsg], out=sgb, in_=psg[:, 0:T], func=AF.Sigmoid)
            O(k, "dve", "tensor_tensor", [bsg, b_psp], [b_y], out=y[:, dc, :], in0=sgb, in1=psp[:, 0:T], op=ALU.mult)
        for c in range(8):
            O(k, "act", "activation", [b_y], [b_su], out=su[:, c, :], in_=y[:, c, :], func=AF.Square)
        rms_stats(k, su, b_su, rstd, b_rstd, T)
        for c in range(8):
            O(k, "dve", "scalar_tensor_tensor", [b_y, k.b_g, b_rstd], [b_y], out=y[:, c, :], in0=y[:, c, :], scalar=gp_post[:, c:c + 1],
              in1=rstd, op0=ALU.mult, op1=ALU.mult)
        for c in range(8):
            O(k, "pool", "tensor_tensor", [bh, b_y], [bh], out=hb[:, c, :], in0=hb[:, c, :], in1=y[:, c, :], op=ALU.add)
        DMA(k, "sp", [bh], [b_dst], out=k.outT[:, t0:t0 + T].rearrange("(c p) t -> p c t", p=128), in_=hb)


def host_rows(inputs):
    r = np.concatenate([np.asarray(inputs[n][0], np.float32).ravel() for n in ("dt_bias", "a_log", "dn_norm_g", "idx_k_norm_g")])
    return np.ascontiguousarray(np.broadcast_to(r[None, :], (128, R_N)))


def host_cs():
    pos = np.arange(S, dtype=np.float32)
    inv = (np.float32(500000.0) ** (-np.arange(0, 16, 2, dtype=np.float32) / np.float32(16.0))).astype(np.float32)
    ang = (pos[:, None] * inv[None, :]).astype(np.float32)
    cs = np.concatenate([np.cos(ang), np.sin(ang)], axis=1).astype(np.float32)
    return np.ascontiguousarray(cs.reshape(32, 128, 16).transpose(1, 0, 2))


def _fm(v):
    return np.ascontiguousarray(np.asarray(v, np.float32).reshape(8, 128).T)


def kernel(**inputs):
    dbg = os.environ.get("KDEBUG")
    debug = set(dbg.split(",")) if dbg else None
    x = np.asarray(inputs["x"], np.float32)
    p = np.asarray(inputs["p"], np.float32)[0]
    nb = x.shape[0]
    gains = np.concatenate([_fm(inputs[n][0]) for n in (
        "ffn1_norm_pre", "ffn1_norm_post", "mix_norm_pre", "mix_norm_post", "ffn2_norm_pre",
        "ffn2_norm_post", "ple_norm_pre", "ple_norm_post")], axis=1)

    def w_in_l(w):
        w = np.asarray(w, np.float32)
        return np.ascontiguousarray(w.reshape(w.shape[0] // 128, 128, -1).transpose(1, 0, 2))

    def w_out_l(w):
        return np.ascontiguousarray(np.asarray(w, np.float32).reshape(NFC, 128, -1).transpose(1, 0, 2))

    shared = {
        "consts": host_consts(),
        "gains": np.ascontiguousarray(gains),
        "mix_w1": w_in_l(np.asarray(inputs["mix_w_in"][0])[:, 0:NW1]),
        "mix_w2": w_in_l(np.asarray(inputs["mix_w_in"][0])[:, NW1:]),
        "w_br_a": w_in_l(inputs["w_br_a"][0]),
        "w_br_b": w_in_l(inputs["w_br_b"][0]),
        "mix_w_out": w_in_l(inputs["mix_w_out"][0]),
        "ple_wg": w_in_l(inputs["ple_w_gate"][0]),
        "ple_wp": w_in_l(inputs["ple_w_proj"][0]),
        "conv_w": np.ascontiguousarray(np.asarray(inputs["conv_w"][0], np.float32).T.reshape(12, 128, 4).transpose(1, 0, 2)),
        "rows": host_rows(inputs),
        "cs": host_cs(),
        "ffn1_w_in": w_in_l(inputs["ffn1_w_in"][0]),
        "ffn2_w_in": w_in_l(inputs["ffn2_w_in"][0]),
        "ffn1_w_out": w_out_l(inputs["ffn1_w_out"][0]),
        "ffn2_w_out": w_out_l(inputs["ffn2_w_out"][0]),
    }
    in_maps = []
    for b in range(nb):
        m = dict(shared)
        m["xT"] = np.ascontiguousarray(x[b].T)
        m["pT"] = np.ascontiguousarray(p[b].T)
        in_maps.append(m)
    nc = build(debug)
    if debug:
        in_maps = [{n: v for n, v in in_maps[0].items() if n in build.k.in_names}]
        nb = 1
    res = run_bass_kernel_spmd(nc, in_maps, core_ids=list(range(nb)))
    if debug:
        kernel.last = res.results
    out = np.stack([np.ascontiguousarray(r["outT"].T) for r in res.results], axis=0)
    return out.astype(np.float32)


R_DTB = 0
R_ALOG = 8
R_DNG = 16
R_IKG = 80
R_N = 144
NW1 = 4184


def mixproj_pass(k):
    P = k.P
    T = TM
    cv = Carver(k.big, k.PH0, k.PHLIM)
    w1 = cv.t([128, 8, NW1], BF16)
    hT = cv.t([128, 8, T], F32)
    su = cv.t([128, 8, T], BF16)
    rstd = cv.t([128, T], F32)
    cw = cv.t([128, 12, 4], F32)
    hal = cv.t([128, 12, 3], F32)
    cb = [cv.t([128, T + 3], F32) for _ in range(2)]
    acc = [cv.t([128, T], F32) for _ in range(2)]
    sil = [cv.t([128, T], F32) for _ in range(2)]
    sqb = [cv.t([128, T], BF16) for _ in range(2)]
    rn = [cv.t([128, T], F32) for _ in range(2)]
    qkT = cv.t([128, 8, T], BF16)
    vT = cv.t([128, 4, T], BF16)
    tokst = [cv.t([128, 512], BF16) for _ in range(2)]
    xs = [cv.t([128, 8, 64], F32) for _ in range(2)]
    xo = [cv.t([128, 8, 65], BF16) for _ in range(2)]
    tmp = [cv.t([128, 8, 8], F32) for _ in range(4)]
    sq5 = cv.t([128, 8, 64], F32)
    n2 = cv.t([128, 8], F32)
    zst = [cv.t([128, 512], F32) for _ in range(2)]
    gbst = [cv.t([128, 16], F32) for _ in range(2)]
    abt = cv.t([128, 16], F32)
    wist = [cv.t([128, 8], F32) for _ in range(2)]
    kis = cv.t([128, 72], F32)
    kio = cv.t([128, 64], BF16)
    kss = cv.t([128, 1], F32)
    vo = [cv.t([128, 8, 65], BF16) for _ in range(2)]
    st_qb = cv.t([65, 8, T], BF16)
    st_kb = cv.t([65, 8, T], BF16)
    st_qi = cv.t([64, 8, T], BF16)
    st_ki = cv.t([64, T], BF16)
    nea = cv.t([128, 8], F32)

    B = P.buf
    b_w1 = [B("w1_%d" % c, True) for c in range(8)]
    b_h = B("mp_h", True)
    b_su = B("mp_su")
    b_rstd = B("mp_rstd")
    b_cw = B("mp_cw", True)
    b_hal = [B("mp_hal%d" % i) for i in range(12)]
    b_cb = [B("mp_cb%d" % i) for i in range(2)]
    b_acc = [B("mp_acc%d" % i) for i in range(2)]
    b_sil = [B("mp_sil%d" % i) for i in range(2)]
    b_sqb = [B("mp_sqb%d" % i) for i in range(2)]
    b_rn = [B("mp_rn%d" % i) for i in range(2)]
    b_qkT = [B("mp_qkT%d" % i) for i in range(8)]
    b_vT = [B("mp_vT%d" % i) for i in range(4)]
    b_tokst = [B("mp_tokst%d" % i) for i in range(2)]
    b_xs = [B("mp_xs%d" % i) for i in range(2)]
    b_xo = [B("mp_xo%d" % i) for i in range(2)]
    b_tmp = [B("mp_tmp%d" % i) for i in range(4)]
    b_sq5 = B("mp_sq5")
    b_n2 = B("mp_n2")
    b_zst = [B("mp_zst%d" % i) for i in range(2)]
    b_gbst = [B("mp_gbst%d" % i) for i in range(2)]
    b_abt = B("mp_abt")
    b_wist = [B("mp_wist%d" % i) for i in range(2)]
    b_kis = B("mp_kis")
    b_kio = B("mp_kio")
    b_kss = B("mp_kss")
    b_vo = [B("mp_vo%d" % i) for i in range(2)]
    b_stqb = B("mp_stqb")
    b_stkb = B("mp_stkb")
    b_stqi = B("mp_stqi")
    b_stki = B("mp_stki")
    b_nea = B("mp_nea")
    d = k.d
    bd = k.bd

    ident_bf = k.cbf[:, C_IDENT:C_IDENT + 128]
    blk64 = k.cbf[:, C_BLK64:C_BLK64 + 128]
    g_pre = k.gsb[:, 16:24]

    for c in range(8):
        DMA(k, "pool", [], [b_w1[c]], out=w1[:, c, :], in_=k.mix_w1[:, c, :])
    DMA(k, "sp", [], [b_cw], out=cw, in_=k.conv_w)
    for fc in range(12):
        O(k, "pool", "memset", [], [b_hal[fc]], ap=hal[:, fc, :], constant=0.0)
    O(k, "act", "activation", [k.b_rows], [b_nea], out=nea, in_=k.rows[:, R_ALOG:R_ALOG + 8], func=AF.Exp)
    O(k, "dve", "tensor_scalar", [b_nea], [b_nea], out=nea, in0=nea, scalar1=-1.0, scalar2=None, op0=ALU.mult)
    O(k, "dve", "memset", [], [k.b_kmax], ap=k.kmax, constant=0.0)
    for i in range(2):
        O(k, "pool", "memset", [], [b_vo[i]], ap=vo[i][:, :, 64:65], constant=1.0)

    ntile = S // T
    for it in range(ntile):
        t0 = it * T
        DMA(k, "sp", [], [b_h], out=hT, in_=k.h1T[:, t0:t0 + T].rearrange("(c p) t -> p c t", p=128))
        for c in range(8):
            O(k, "act", "activation", [b_h], [b_su], out=su[:, c, :], in_=hT[:, c, :], func=AF.Square)
        rms_stats(k, su, b_su, rstd, b_rstd, T)
        for c in range(8):
            O(k, "dve", "scalar_tensor_tensor", [b_h, k.b_g, b_rstd], [b_su], out=su[:, c, :], in0=hT[:, c, :],
              scalar=g_pre[:, c:c + 1], in1=rstd, op0=ALU.mult, op1=ALU.mult)
        for fc in range(12):
            ps, b_ps = next_ps(k)
            for c in range(8):
                O(k, "pe", "matmul", [b_w1[c], b_su], [b_ps], out=ps[:, 0:T], lhsT=w1[:, c, fc * 128:(fc + 1) * 128],
                  rhs=su[:, c, :], start=(c == 0), stop=(c == 7))
            j = fc % 2
            O(k, "pool", "tensor_copy", [b_hal[fc]], [b_cb[j]], out=cb[j][:, 0:3], in_=hal[:, fc, :])
            O(k, "act", "activation", [b_ps], [b_cb[j]], out=cb[j][:, 3:T + 3], in_=ps[:, 0:T], func=AF.Copy)
            O(k, "pool", "tensor_copy", [b_cb[j]], [b_hal[fc]], out=hal[:, fc, :], in_=cb[j][:, T:T + 3])
            O(k, "dve", "tensor_scalar", [b_cb[j], b_cw], [b_acc[j]], out=acc[j], in0=cb[j][:, 0:T],
              scalar1=cw[:, fc, 0:1], scalar2=None, op0=ALU.mult)
            for jj in range(1, 4):
                O(k, "dve", "scalar_tensor_tensor", [b_cb[j], b_cw, b_acc[j]], [b_acc[j]], out=acc[j], in0=cb[j][:, jj:jj + T],
                  scalar=cw[:, fc, jj:jj + 1], in1=acc[j], op0=ALU.mult, op1=ALU.add)
            if fc < 8:
                O(k, "act", "activation", [b_acc[j]], [b_sil[j]], out=sil[j], in_=acc[j], func=AF.Silu)
                O(k, "act", "activation", [b_sil[j]], [b_sqb[j]], out=sqb[j], in_=sil[j], func=AF.Square)
                ps2, b_ps2 = next_ps(k)
                O(k, "pe", "matmul", [k.b_cbf, b_sqb[j]], [b_ps2], out=ps2[:, 0:T], lhsT=blk64, rhs=sqb[j], start=True, stop=True)
                O(k, "act", "activation", [b_ps2, k.b_eps], [b_rn[j]], out=rn[j], in_=ps2[:, 0:T], func=AF.Sqrt, bias=k.epsb, scale=1.0)
                O(k, "dve", "reciprocal", [b_rn[j]], [b_rn[j]], out=rn[j], in_=rn[j])
                O(k, "dve", "scalar_tensor_tensor", [b_sil[j], b_rn[j]], [b_qkT[fc]], out=qkT[:, fc, :], in0=sil[j],
                  scalar=(0.125 if fc < 4 else 1.0), in1=rn[j], op0=ALU.mult, op1=ALU.mult)
            else:
                O(k, "act", "activation", [b_acc[j]], [b_vT[fc - 8]], out=vT[:, fc - 8, :], in_=acc[j], func=AF.Silu)
        DMA(k, "sp", [b_qkT[i] for i in range(4)], [bd["qaT"]], out=d["qaT"][:, t0:t0 + T].rearrange("(c p) t -> p c t", p=128),
            in_=qkT[:, 0:4, :])
        DMA(k, "sp", [b_qkT[i] for i in range(4, 8)], [bd["kaT"]], out=d["kaT"][:, t0:t0 + T].rearrange("(c p) t -> p c t", p=128),
            in_=qkT[:, 4:8, :])
        for sub in range(T // 128):
            for wh in range(2):
                ps, b_ps = next_ps(k)
                psb = ps[:, :].bitcast(BF16)
                for c4 in range(4):
                    if wh == 0:
                        O(k, "pe", "transpose", [b_qkT[4 + c4], k.b_cbf], [b_ps], out=psb[:, c4 * 128:(c4 + 1) * 128],
                          in_=qkT[:, 4 + c4, sub * 128:(sub + 1) * 128], identity=ident_bf)
                    else:
                        O(k, "pe", "transpose", [b_vT[c4], k.b_cbf], [b_ps], out=psb[:, c4 * 128:(c4 + 1) * 128],
                          in_=vT[:, c4, sub * 128:(sub + 1) * 128], identity=ident_bf)
                O(k, "act", "activation", [b_ps], [b_tokst[wh]], out=tokst[wh], in_=psb[:, 0:512], func=AF.Copy)
                dst = "ka_tok" if wh == 0 else "va_tok"
                DMA(k, "sp", [b_tokst[wh]], [bd[dst]], out=d[dst][t0 + sub * 128:t0 + (sub + 1) * 128, :], in_=tokst[wh])
        for sub in range(T // 128):
            blk = (t0 // 128) + sub
            tsl = slice(sub * 128, (sub + 1) * 128)
            r0 = t0 + sub * 128
            cosb = k.cs[:, blk, 0:8].unsqueeze(1).to_broadcast([128, 8, 8])
            sinb = k.cs[:, blk, 8:16].unsqueeze(1).to_broadcast([128, 8, 8])

            def proj(col0, n):
                ps, b_ps = next_ps(k)
                for c in range(8):
                    O(k, "pe", "matmul", [b_w1[c], b_su], [b_ps], out=ps[:, 0:n], lhsT=su[:, c, tsl], rhs=w1[:, c, col0:col0 + n],
                      start=(c == 0), stop=(c == 7))
                return ps, b_ps

            def rope(x3, bx, o3, bo, nh):
                cb_ = cosb[:, 0:nh, :]
                sb_ = sinb[:, 0:nh, :]
                x1 = x3[:, :, 0:8]
                x2 = x3[:, :, 8:16]
                O(k, "dve", "tensor_tensor", [bx, k.b_cs], [b_tmp[0]], out=tmp[0][:, 0:nh, :], in0=x1, in1=cb_, op=ALU.mult)
                O(k, "dve", "tensor_tensor", [bx, k.b_cs], [b_tmp[1]], out=tmp[1][:, 0:nh, :], in0=x2, in1=sb_, op=ALU.mult)
                O(k, "pool", "tensor_tensor", [bx, k.b_cs], [b_tmp[2]], out=tmp[2][:, 0:nh, :], in0=x2, in1=cb_, op=ALU.mult)
                O(k, "pool", "tensor_tensor", [bx, k.b_cs], [b_tmp[3]], out=tmp[3][:, 0:nh, :], in0=x1, in1=sb_, op=ALU.mult)
                O(k, "dve", "tensor_tensor", [b_tmp[0], b_tmp[1]], [bo], out=o3[:, :, 0:8], in0=tmp[0][:, 0:nh, :],
                  in1=tmp[1][:, 0:nh, :], op=ALU.subtract)
                O(k, "pool", "tensor_tensor", [b_tmp[2], b_tmp[3]], [bo], out=o3[:, :, 8:16], in0=tmp[2][:, 0:nh, :],
                  in1=tmp[3][:, 0:nh, :], op=ALU.add)
                O(k, "act", "activation", [bx], [bo], out=o3[:, :, 16:64], in_=x3[:, :, 16:64], func=AF.Copy)

            ps, b_ps = proj(1536, 512)
            j = sub % 2
            O(k, "act", "activation", [b_ps], [b_zst[j]], out=zst[j], in_=ps[:, 0:512], func=AF.Silu)
            DMA(k, "sp", [b_zst[j]], [bd["z_tok"]], out=d["z_tok"][r0:r0 + 128, :], in_=zst[j])
            ps, b_ps = proj(2048, 16)
            O(k, "dve", "tensor_tensor", [b_ps, k.b_rows], [b_abt], out=abt[:, 0:8], in0=ps[:, 0:8], in1=k.rows[:, R_DTB:R_DTB + 8],
              op=ALU.add)
            O(k, "act", "activation", [b_abt], [b_abt], out=abt[:, 0:8], in_=abt[:, 0:8], func=AF.Exp)
            O(k, "act", "activation", [b_abt], [b_abt], out=abt[:, 0:8], in_=abt[:, 0:8], func=AF.Ln, bias=k.oneb, scale=1.0)
            O(k, "dve", "tensor_tensor", [b_abt, b_nea], [b_gbst[j]], out=gbst[j][:, 0:8], in0=abt[:, 0:8], in1=nea, op=ALU.mult)
            O(k, "act", "activation", [b_ps], [b_gbst[j]], out=gbst[j][:, 8:16], in_=ps[:, 8:16], func=AF.Sigmoid)
            DMA(k, "sp", [b_gbst[j]], [bd["gb_tok"]], out=d["gb_tok"][r0:r0 + 128, :], in_=gbst[j])
            for wh, col0 in ((0, 2064), (1, 2576), (2, 3600)):
                ps, b_ps = proj(col0, 512)
                jj = wh % 2
                x3 = xs[jj]
                O(k, "act", "activation", [b_ps], [b_xs[jj]], out=x3, in_=ps[:, 0:512].rearrange("p (h d) -> p h d", d=64),
                  func=AF.Copy, scale=(0.125 if wh == 0 else 1.0))
                rope(x3, b_xs[jj], xo[jj], b_xo[jj], 8)
                if wh < 2:
                    O(k, "dve", "tensor_tensor", [b_xs[jj]], [b_sq5], out=sq5, in0=x3, in1=x3, op=ALU.mult)
                    O(k, "dve", "tensor_reduce", [b_sq5], [b_n2], out=n2, in_=sq5, axis=AX.X, op=ALU.add)
                    if wh == 0:
                        O(k, "dve", "tensor_scalar", [b_n2], [b_xo[jj]], out=xo[jj][:, :, 64:65], in0=n2.unsqueeze(2), scalar1=-4.0,
                          scalar2=None, op0=ALU.mult)
                    else:
                        O(k, "pool", "memset", [], [b_xo[jj]], ap=xo[jj][:, :, 64:65], constant=1.0)
                        O(k, "dve", "tensor_tensor", [b_n2, k.b_kmax], [k.b_kmax], out=k.kmax, in0=k.kmax, in1=n2, op=ALU.max)
                nr = 65 if wh < 2 else 64
                pst, b_pst = next_ps(k)
                pstb = pst[0:nr, :].bitcast(BF16).rearrange("p (h t) -> p h t", t=128)
                for h in range(8):
                    O(k, "pe", "transpose", [b_xo[jj], k.b_cbf], [b_pst], out=pstb[:, h, :], in_=xo[jj][:, h, 0:nr], identity=ident_bf)
                stg, bst = ((st_qb, b_stqb), (st_kb, b_stkb), (st_qi, b_stqi))[wh]
                O(k, "act", "activation", [b_pst], [bst], out=stg[:, :, tsl], in_=pstb, func=AF.Copy)
            ps, b_ps = proj(3088, 512)
            O(k, "act", "activation", [b_ps], [b_vo[j]], out=vo[j][:, :, 0:64], in_=ps[:, 0:512].rearrange("p (h d) -> p h d", d=64),
              func=AF.Copy)
            DMA(k, "sp", [b_vo[j]], [bd["vb_tok"]], out=d["vb_tok"][r0:r0 + 128, :], in_=vo[j].rearrange("p h d -> p (h d)"))
            ps, b_ps = proj(4112, 72)
            O(k, "act", "activation", [b_ps], [b_kis], out=kis, in_=ps[:, 0:72], func=AF.Copy)
            O(k, "dve", "tensor_tensor", [b_kis], [b_sq5], out=sq5[:, 0, :], in0=kis[:, 0:64], in1=kis[:, 0:64], op=ALU.mult)
            O(k, "dve", "tensor_reduce", [b_sq5], [b_kss], out=kss, in_=sq5[:, 0, :], axis=AX.X, op=ALU.add)
            O(k, "act", "activation", [b_kss, k.b_eps], [b_kss], out=kss, in_=kss, func=AF.Sqrt, bias=k.epsb, scale=1.0 / 64.0)
            O(k, "dve", "reciprocal", [b_kss], [b_kss], out=kss, in_=kss)
            O(k, "dve", "scalar_tensor_tensor", [b_kis, b_kss, k.b_rows], [b_xs[0]], out=xs[0][:, 0, :], in0=kis[:, 0:64], scalar=kss[:, 0:1],
              in1=k.rows[:, R_IKG:R_IKG + 64], op0=ALU.mult, op1=ALU.mult)
            rope(xs[0][:, 0:1, :], b_xs[0], xo[0][:, 0:1, :], b_xo[0], 1)
            pst, b_pst = next_ps(k)
            pstb = pst[0:64, :].bitcast(BF16)
            O(k, "pe", "transpose", [b_xo[0], k.b_cbf], [b_pst], out=pstb[:, 0:128], in_=xo[0][:, 0, 0:64], identity=ident_bf)
            O(k, "act", "activation", [b_pst], [b_stki], out=st_ki[:, tsl], in_=pstb[:, 0:128], func=AF.Copy)
            O(k, "dve", "tensor_scalar", [b_kis], [b_wist[j]], out=wist[j], in0=kis[:, 64:72], scalar1=float(0.125 * 8 ** -0.5),
              scalar2=None, op0=ALU.mult)
            DMA(k, "sp", [b_wist[j]], [bd["wi_tok"]], out=d["wi_tok"][r0:r0 + 128, :], in_=wist[j])
        DMA(k, "sp", [b_stqb], [bd["qbT"]], out=d["qbT"][:, :, t0:t0 + T].rearrange("h r t -> r h t"), in_=st_qb)
        DMA(k, "sp", [b_stkb], [bd["kbT"]], out=d["kbT"][:, :, t0:t0 + T].rearrange("h r t -> r h t"), in_=st_kb)
        DMA(k, "sp", [b_stqi], [bd["qiT"]], out=d["qiT"][:, :, t0:t0 + T].rearrange("h r t -> r h t"), in_=st_qi)
        DMA(k, "sp", [b_stki], [bd["kiT"]], out=d["kiT"][:, t0:t0 + T], in_=st_ki)


def pipeline(n_items, pre_fn, seq_fn, nset, max_pre=2):
    pre_done = [False] * n_items
    seq_done = [0]
    active = []
    nxt = [0]

    def seq_all():
        for n in range(n_items):
            while not pre_done[n]:
                yield
            for _ in seq_fn(n):
                yield
            seq_done[0] = n + 1
            yield

    sg = seq_all()
    alive = True
    while alive or active:
        while nxt[0] < n_items and len(active) < max_pre and nxt[0] < seq_done[0] + nset:
            active.append((nxt[0], pre_fn(nxt[0])))
            nxt[0] += 1
        for item in list(active):
            n, g = item
            try:
                next(g)
            except StopIteration:
                pre_done[n] = True
                active.remove(item)
        if alive:
            try:
                next(sg)
            except StopIteration:
                alive = False


def pipeline3(n_items, fA, fB, fC, nbuf=2):
    done = [0, 0, 0]
    nxt = [0, 0, 0]
    gens = [None, None, None]
    fs = [fA, fB, fC]

    def can_start(si, i):
        if i >= n_items:
            return False
        if si == 0:
            return i < done[1] + nbuf
        if si == 1:
            return done[0] > i and i < done[2] + nbuf
        return done[1] > i

    while done[2] < n_items:
        progressed = False
        for si in range(3):
            if gens[si] is None and can_start(si, nxt[si]):
                gens[si] = fs[si](nxt[si])
                nxt[si] += 1
            if gens[si] is not None:
                progressed = True
                try:
                    next(gens[si])
                except StopIteration:
                    gens[si] = None
                    done[si] += 1
        assert progressed, "pipeline3 deadlock"


def gdn_pass(k):
    P = k.P
    B = P.buf
    d = k.d
    bd = k.bd
    cv = Carver(k.big, k.PH0, k.PHLIM)
    NSET = 3
    H8 = [64, 8, 64]
    gb_all = cv.t([64, 64, 16], F32)
    gcs = {nm: cv.t([64, 64, 8], F32) for nm in ("gc", "gl", "egc", "ekd", "gtot", "beg")}
    negU = cv.t([64, 64], F32)
    grp = [{nm: cv.t([64, 8, 512], BF16) for nm in ("qT", "kT", "ktok", "vtok")} for _ in range(2)]
    zt = [cv.t([64, 8, 64], F32) for _ in range(3)]
    sets = []
    for i in range(NSET):
        s_ = {nm: cv.t(H8, F32) for nm in ("Gb", "Gu", "E", "EL", "EU", "u")}
        s_.update({nm: cv.t(H8, BF16) for nm in ("X0", "X1", "Y0", "Y1", "Pm", "vb", "kbe", "kd", "wT", "qkT")})
        sets.append(s_)
    sq_ = []
    for i in range(2):
        s_ = {nm: cv.t(H8, F32) for nm in ("St", "oa", "o", "sq", "on")}
        s_.update({nm: cv.t(H8, BF16) for nm in ("vnew", "onb")})
        s_["ss"] = cv.t([64, 8], F32)
        sq_.append(s_)
    Sst = cv.t(H8, F32)
    Sb = cv.t(H8, BF16)
    oaT_st = [cv.t([128, 4, 512], BF16) for _ in range(2)]

    b_gb = B("g_gball", True)
    b_gcs = {nm: B("g_" + nm) for nm in gcs}
    b_negU = B("g_negU")
    b_grp = [{nm: B("g_grp%d_%s" % (i, nm), True) for nm in grp[i]} for i in range(2)]
    b_zt = [B("g_zt%d" % i, True) for i in range(3)]
    b_sets = [{nm: B("g_s%d_%s" % (i, nm)) for nm in sets[i]} for i in range(NSET)]
    b_sq = [{nm: B("g_q%d_%s" % (i, nm)) for nm in sq_[i]} for i in range(2)]
    b_S = B("g_S")
    b_Sb = B("g_Sb")
    b_oast = [B("g_oast%d" % i) for i in range(2)]

    U64 = k.c32[0:64, C_U64:C_U64 + 64]
    ONES64 = k.c32[0:64, C_ONES64:C_ONES64 + 64]
    SLb = k.c32[0:64, C_SL:C_SL + 64].unsqueeze(1).to_broadcast(H8)
    UIb = k.c32[0:64, C_UI:C_UI + 64].unsqueeze(1).to_broadcast(H8)
    Ib = k.cbf[0:64, C_IDENT:C_IDENT + 64].unsqueeze(1).to_broadcast(H8)
    id64 = k.cbf[0:64, C_IDENT:C_IDENT + 64]
    dngb = k.rows[0:64, R_DNG:R_DNG + 64].unsqueeze(1).to_broadcast(H8)

    def fl(v):
        return v.rearrange("p h d -> p (h d)")

    def bj(v2):
        return v2.unsqueeze(2).to_broadcast(H8)

    for q8 in range(8):
        DMA(k, "sp", [bd["gb_tok"]], [b_gb], out=gb_all[:, q8 * 8:(q8 + 1) * 8, :],
            in_=d["gb_tok"][q8 * 512:(q8 + 1) * 512, :].rearrange("(n c) j -> c n j", c=64))
    O(k, "dve", "tensor_scalar", [k.b_c], [b_negU], out=negU, in0=U64, scalar1=-1.0, scalar2=None, op0=ALU.mult)
    g_all = gb_all[:, :, 0:8]
    beta_all = gb_all[:, :, 8:16]
    ps, b_ps = next_ps(k)
    O(k, "pe", "matmul", [k.b_c, b_gb], [b_ps], out=ps[0:64, :].rearrange("p (n h) -> p n h", h=8), lhsT=U64, rhs=g_all,
      start=True, stop=True)
    O(k, "act", "activation", [b_ps], [b_gcs["gc"]], out=fl(gcs["gc"]), in_=ps[0:64, :], func=AF.Copy)
    ps, b_ps = next_ps(k)
    O(k, "pe", "matmul", [k.b_c, b_gb], [b_ps], out=ps[0:64, :].rearrange("p (n h) -> p n h", h=8), lhsT=ONES64, rhs=g_all,
      start=True, stop=True)
    O(k, "act", "activation", [b_ps], [b_gcs["gl"]], out=fl(gcs["gl"]), in_=ps[0:64, :], func=AF.Copy)
    O(k, "act", "activation", [b_gcs["gc"]], [b_gcs["egc"]], out=fl(gcs["egc"]), in_=fl(gcs["gc"]), func=AF.Exp)
    O(k, "act", "activation", [b_gcs["gl"]], [b_gcs["gtot"]], out=fl(gcs["gtot"]), in_=fl(gcs["gl"]), func=AF.Exp)
    O(k, "dve", "tensor_tensor", [b_gcs["gl"], b_gcs["gc"]], [b_gcs["ekd"]], out=fl(gcs["ekd"]), in0=fl(gcs["gl"]), in1=fl(gcs["gc"]),
      op=ALU.subtract)
    O(k, "act", "activation", [b_gcs["ekd"]], [b_gcs["ekd"]], out=fl(gcs["ekd"]), in_=fl(gcs["ekd"]), func=AF.Exp)
    O(k, "dve", "tensor_tensor", [b_gcs["egc"], b_gb], [b_gcs["beg"]], out=gcs["beg"], in0=gcs["egc"], in1=beta_all, op=ALU.mult)
    O(k, "dve", "memset", [], [b_S], ap=Sst, constant=0.0)
    O(k, "dve", "memset", [], [b_Sb], ap=Sb, constant=0.0)

    def mm8(ps, b_ps, lhs, b_lhs, rhs, b_rhs, lsl=None, rsl=None):
        for h in range(8):
            l_ = lhs[:, h, lsl] if lsl is not None else lhs[:, h, :]
            r_ = rhs[:, h, rsl] if rsl is not None else rhs[:, h, :]
            O(k, "pe", "matmul", [b_lhs, b_rhs], [b_ps], out=ps[0:64, h * 64:(h + 1) * 64], lhsT=l_, rhs=r_, start=True, stop=True)

    def load_group(g):
        gi = g % 2
        tsl = slice(g * 512, (g + 1) * 512)
        DMA(k, "sp", [bd["qaT"]], [b_grp[gi]["qT"]], out=grp[gi]["qT"], in_=d["qaT"].rearrange("(h r) t -> r h t", r=64)[:, :, tsl])
        DMA(k, "sp", [bd["kaT"]], [b_grp[gi]["kT"]], out=grp[gi]["kT"], in_=d["kaT"].rearrange("(h r) t -> r h t", r=64)[:, :, tsl])
        DMA(k, "sp", [bd["ka_tok"]], [b_grp[gi]["ktok"]], out=grp[gi]["ktok"],
            in_=d["ka_tok"][tsl, :].rearrange("(n c) f -> c n f", c=64))
        DMA(k, "sp", [bd["va_tok"]], [b_grp[gi]["vtok"]], out=grp[gi]["vtok"],
            in_=d["va_tok"][tsl, :].rearrange("(n c) f -> c n f", c=64))

    def pre(n):
        g = n // 8
        ci = n % 8
        gi = g % 2
        if ci == 0:
            load_group(g)
        G = grp[gi]
        bG = b_grp[gi]
        cs_ = slice(ci * 64, (ci + 1) * 64)
        s = sets[n % NSET]
        bs = b_sets[n % NSET]
        g_n = gb_all[:, n, 0:8]
        beta_n = gb_all[:, n, 8:16]
        ktok_n = G["ktok"][:, ci, :].rearrange("p (h d) -> p h d", d=64)
        vtok_n = G["vtok"][:, ci, :].rearrange("p (h d) -> p h d", d=64)
        O(k, "dve", "tensor_copy", [b_gb], [bs["Gb"]], out=s["Gb"], in_=bj(g_n))
        O(k, "pool", "tensor_tensor", [b_gb, b_negU], [bs["Gu"]], out=s["Gu"], in0=bj(g_n), in1=negU.unsqueeze(1).to_broadcast(H8),
          op=ALU.mult)
        O(k, "pool", "tensor_tensor", [bG["vtok"], b_gb], [bs["vb"]], out=s["vb"], in0=vtok_n, in1=bj(beta_n), op=ALU.mult)
        O(k, "pool", "tensor_tensor", [bG["ktok"], b_gcs["beg"]], [bs["kbe"]], out=s["kbe"], in0=ktok_n, in1=bj(gcs["beg"][:, n, :]),
          op=ALU.mult)
        O(k, "pool", "tensor_tensor", [bG["ktok"], b_gcs["ekd"]], [bs["kd"]], out=s["kd"], in0=ktok_n, in1=bj(gcs["ekd"][:, n, :]),
          op=ALU.mult)
        yield
        psd, b_psd = next_ps(k, True)
        O(k, "pe", "matmul", [k.b_c, bs["Gb"]], [b_psd], out=psd[0:64, :], lhsT=U64, rhs=fl(s["Gb"]), start=True, stop=False)
        O(k, "pe", "matmul", [k.b_c, bs["Gu"]], [b_psd], out=psd[0:64, :], lhsT=ONES64, rhs=fl(s["Gu"]), start=False, stop=True)
        yield
        O(k, "act", "activation", [b_psd], [bs["E"]], out=fl(s["E"]), in_=psd[0:64, :], func=AF.Abs)
        rel_ps(k, b_psd)
        O(k, "act", "activation", [bs["E"]], [bs["E"]], out=fl(s["E"]), in_=fl(s["E"]), func=AF.Exp, scale=-1.0)
        O(k, "pool", "tensor_tensor", [bs["E"], k.b_c], [bs["EU"]], out=s["EU"], in0=s["E"], in1=UIb, op=ALU.mult)
        O(k, "dve", "tensor_tensor", [bs["E"], k.b_c], [bs["EL"]], out=s["EL"], in0=s["E"], in1=SLb, op=ALU.mult)
        O(k, "dve", "tensor_tensor", [bs["EL"], b_gb], [bs["EL"]], out=s["EL"], in0=s["EL"], in1=bj(beta_n), op=ALU.mult)
        pkk, b_pkk = next_ps(k, True)
        mm8(pkk, b_pkk, G["kT"], bG["kT"], G["kT"], bG["kT"], cs_, cs_)
        pqk, b_pqk = next_ps(k, True)
        mm8(pqk, b_pqk, G["kT"], bG["kT"], G["qT"], bG["qT"], cs_, cs_)
        yield
        O(k, "dve", "scalar_tensor_tensor", [b_pkk, bs["EL"]], [bs["X0"]], out=fl(s["X0"]), in0=pkk[0:64, :], scalar=-1.0, in1=fl(s["EL"]),
          op0=ALU.mult, op1=ALU.mult)
        rel_ps(k, b_pkk)
        O(k, "dve", "tensor_tensor", [b_pqk, bs["EU"]], [bs["qkT"]], out=fl(s["qkT"]), in0=pqk[0:64, :], in1=fl(s["EU"]), op=ALU.mult)
        rel_ps(k, b_pqk)
        yield
        pt, b_pt = next_ps(k, True)
        ptb = pt[0:64, :].bitcast(BF16)
        for h in range(8):
            O(k, "pe", "transpose", [bs["X0"], k.b_cbf], [b_pt], out=ptb[:, h * 64:(h + 1) * 64], in_=s["X0"][:, h, :], identity=id64)
        yield
        v6 = "ab"
        if "a" in v6:
            O(k, "act", "activation", [b_pt], [bs["Y0"]], out=fl(s["Y0"]), in_=ptb[:, 0:512], func=AF.Copy)
        if "b" in v6:
            O(k, "dve", "tensor_tensor", [b_pt, k.b_cbf], [bs["Pm"]], out=s["Pm"], in0=ptb[:, 0:512].rearrange("p (h d) -> p h d", d=64), in1=Ib,
              op=ALU.add)
        if "c" in v6:
            O(k, "dve", "tensor_tensor", [bs["Y0"], k.b_cbf], [bs["Pm"]], out=s["Pm"], in0=s["Y0"], in1=Ib, op=ALU.add)
        rel_ps(k, b_pt)
        yield
        X, Y = "X0", "Y0"
        for lv in range(5):
            Xn = "X1" if X == "X0" else "X0"
            Yn = "Y1" if Y == "Y0" else "Y0"
            px, b_px = next_ps(k, True)
            mm8(px, b_px, s[Y], bs[Y], s[X], bs[X])
            if lv < 4:
                py, b_py = next_ps(k, True)
                mm8(py, b_py, s[X], bs[X], s[Y], bs[Y])
            yield
            O(k, "act", "activation", [b_px], [bs[Xn]], out=fl(s[Xn]), in_=px[0:64, :], func=AF.Copy)
            rel_ps(k, b_px)
            if lv < 4:
                O(k, "dve", "tensor_copy", [b_py], [bs[Yn]], out=fl(s[Yn]), in_=py[0:64, :])
                rel_ps(k, b_py)
            yield
            pp, b_pp = next_ps(k, True)
            mm8(pp, b_pp, s[Xn], bs[Xn], s["Pm"], bs["Pm"])
            yield
            O(k, "dve", "tensor_tensor", [b_pp, bs["Pm"]], [bs["Pm"]], out=fl(s["Pm"]), in0=pp[0:64, :], in1=fl(s["Pm"]), op=ALU.add)
            rel_ps(k, b_pp)
            yield
            X, Y = Xn, Yn
        pu, b_pu = next_ps(k, True)
        mm8(pu, b_pu, s["Pm"], bs["Pm"], s["vb"], bs["vb"])
        pw, b_pw = next_ps(k, True)
        mm8(pw, b_pw, s["kbe"], bs["kbe"], s["Pm"], bs["Pm"])
        yield
        O(k, "act", "activation", [b_pu], [bs["u"]], out=fl(s["u"]), in_=pu[0:64, :], func=AF.Copy)
        rel_ps(k, b_pu)
        O(k, "act", "activation", [b_pw], [bs["wT"]], out=fl(s["wT"]), in_=pw[0:64, :], func=AF.Copy)
        rel_ps(k, b_pw)
        yield

    def seq(n):
        g = n // 8
        ci = n % 8
        gi = g % 2
        G = grp[gi]
        bG = b_grp[gi]
        cs_ = slice(ci * 64, (ci + 1) * 64)
        s = sets[n % NSET]
        bs = b_sets[n % NSET]
        q = sq_[n % 2]
        bq = b_sq[n % 2]
        z = zt[n % 3]
        bz = b_zt[n % 3]
        DMA(k, "sp", [bd["z_tok"]], [bz], out=fl(z), in_=d["z_tok"][n * 64:(n + 1) * 64, :])
        pws, b_pws = next_ps(k, True)
        mm8(pws, b_pws, s["wT"], bs["wT"], Sb, b_Sb)
        po1, b_po1 = next_ps(k, True)
        mm8(po1, b_po1, G["qT"], bG["qT"], Sb, b_Sb, cs_, None)
        O(k, "pool", "tensor_tensor", [b_S, b_gcs["gtot"]], [bq["St"]], out=q["St"], in0=Sst, in1=bj(gcs["gtot"][:, n, :]), op=ALU.mult)
        yield
        O(k, "dve", "tensor_tensor", [bs["u"], b_pws], [bq["vnew"]], out=fl(q["vnew"]), in0=fl(s["u"]), in1=pws[0:64, :], op=ALU.subtract)
        rel_ps(k, b_pws)
        O(k, "dve", "tensor_tensor", [b_po1, b_gcs["egc"]], [bq["oa"]], out=q["oa"], in0=po1[0:64, :].rearrange("p (h d) -> p h d", d=64),
          in1=bj(gcs["egc"][:, n, :]), op=ALU.mult)
        rel_ps(k, b_po1)
        yield
        pkv, b_pkv = next_ps(k, True)
        mm8(pkv, b_pkv, s["kd"], bs["kd"], q["vnew"], bq["vnew"])
        po2, b_po2 = next_ps(k, True)
        mm8(po2, b_po2, s["qkT"], bs["qkT"], q["vnew"], bq["vnew"])
        yield
        O(k, "dve", "tensor_tensor", [bq["St"], b_pkv], [b_S], out=fl(Sst), in0=fl(q["St"]), in1=pkv[0:64, :], op=ALU.add)
        rel_ps(k, b_pkv)
        O(k, "act", "activation", [b_S], [b_Sb], out=fl(Sb), in_=fl(Sst), func=AF.Copy)
        O(k, "dve", "tensor_tensor", [bq["oa"], b_po2], [bq["o"]], out=fl(q["o"]), in0=fl(q["oa"]), in1=po2[0:64, :], op=ALU.add)
        rel_ps(k, b_po2)
        yield
        O(k, "pool", "tensor_tensor", [bq["o"]], [bq["sq"]], out=q["sq"], in0=q["o"], in1=q["o"], op=ALU.mult)
        O(k, "dve", "tensor_reduce", [bq["sq"]], [bq["ss"]], out=q["ss"], in_=q["sq"], axis=AX.X, op=ALU.add)
        O(k, "act", "activation", [bq["ss"], k.b_eps], [bq["ss"]], out=q["ss"], in_=q["ss"], func=AF.Sqrt, bias=k.epsb[0:64, :], scale=1.0 / 64.0)
        O(k, "dve", "reciprocal", [bq["ss"]], [bq["ss"]], out=q["ss"], in_=q["ss"])
        yield
        O(k, "dve", "tensor_tensor", [bq["o"], bq["ss"]], [bq["on"]], out=q["on"], in0=q["o"], in1=bj(q["ss"]), op=ALU.mult)
        O(k, "pool", "tensor_tensor", [bq["on"], k.b_rows], [bq["on"]], out=q["on"], in0=q["on"], in1=dngb, op=ALU.mult)
        O(k, "pool", "tensor_tensor", [bq["on"], bz], [bq["onb"]], out=q["onb"], in0=q["on"], in1=z, op=ALU.mult)
        yield
        pt, b_pt = next_ps(k, True)
        ptb = pt[:, :].bitcast(BF16)
        onf = fl(q["onb"])
        for c4 in range(4):
            O(k, "pe", "transpose", [bq["onb"], k.b_cbf], [b_pt], out=ptb[:, c4 * 64:(c4 + 1) * 64], in_=onf[:, c4 * 128:(c4 + 1) * 128],
              identity=id64)
        yield
        O(k, "act", "activation", [b_pt], [b_oast[gi]], out=oaT_st[gi][:, :, cs_], in_=ptb[:, 0:256].rearrange("p (c t) -> p c t", t=64),
          func=AF.Copy)
        rel_ps(k, b_pt)
        if ci == 7:
            DMA(k, "sp", [b_oast[gi]], [bd["oaT"]], out=d["oaT"][:, g * 512:(g + 1) * 512].rearrange("(c p) t -> p c t", p=128),
                in_=oaT_st[gi])
        yield

    if k.gdn_stages is not None:
        def pre_lim(n):
            for i, _ in enumerate(pre(n)):
                if i + 1 >= k.gdn_stages:
                    k.ps_held.clear()
                    return
                yield

        def seq_none(n):
            return
            yield
        pipeline(k.gdn_n, pre_lim, seq_none, NSET)
    else:
        pipeline(k.gdn_n, pre, seq, NSET)


NIT = 20
TOPK = 256


def dsa_pass(k):
    P = k.P
    B = P.buf
    d = k.d
    bd = k.bd
    cv = Carver(k.big, k.PH0, k.PHLIM)
    kiT = cv.t([64, S], BF16)
    kbT = cv.t([65, 8, S], BF16)
    vb = cv.t([128, 32, 520], BF16)
    cbias = cv.t([128, 8], F32)
    km8 = cv.t([8, 1], F32)
    dg = cv.t([8, 8], F32)
    sc = [cv.t([128, S], F32) for _ in range(2)]
    m01 = cv.t([128, S], BF16)
    junk = m01
    junk2 = cv.t([128, S], BF16)
    obu = [cv.t([128, 8, 65], F32) for _ in range(2)]
    rc8 = cv.t([128, 8, 1], F32)
    maskT = [cv.t([128, 32, 128], BF16) for _ in range(2)]
    qiq = [cv.t([64, 8, 128], BF16) for _ in range(2)]
    qbq = [cv.t([65, 8, 128], BF16) for _ in range(2)]
    wiq = [cv.t([128, 8], F32) for _ in range(2)]
    rl = [cv.t([128, 512], F32) for _ in range(2)]
    ex = [cv.t([128, 512], BF16) for _ in range(3)]
    pm = [cv.t([128, 512], BF16) for _ in range(3)]
    bis = [{nm: cv.t([128, 1], F32) for nm in ("hi", "lo", "rng", "c1", "s2", "s3", "b1", "nb2", "nb")} for _ in range(2)]
    t3v = [cv.t([128, 3], F32) for _ in range(2)]
    b_lo = [B("a_lo%d" % i) for i in range(2)]
    b_t3 = [B("a_t3%d" % i) for i in range(2)]
    b_s2 = [B("a_s2%d" % i) for i in range(2)]
    b_s3 = [B("a_s3%d" % i) for i in range(2)]
    Wt = [cv.t([128, 32], F32) for _ in range(2)]
    ob = [cv.t([128, 512], BF16) for _ in range(2)]
    rc = [cv.t([128, 1], F32) for _ in range(2)]
    obst = [cv.t([128, 4, 128], BF16) for _ in range(2)]

    b_kiT = B("a_kiT", True)
    b_kbT = [B("a_kbT%d" % h, True) for h in range(8)]
    b_vb = B("a_vb", True)
    b_cbias = B("a_cbias")
    b_km8 = B("a_km8")
    b_dg = B("a_dg")
    b_sc = [B("a_sc%d" % i) for i in range(2)]
    b_m01 = B("a_m01")
    b_junk = b_m01
    b_junk2 = B("a_junk2")
    b_obu = [B("a_obu%d" % i) for i in range(2)]
    b_rc8 = B("a_rc8")
    b_mid = [B("a_mid%d" % i) for i in range(2)]
    b_ssg = [B("a_ssg%d" % i) for i in range(2)]
    b_maskT = [B("a_maskT%d" % i) for i in range(2)]
    b_qiq = [B("a_qiq%d" % i, True) for i in range(2)]
    b_qbq = [B("a_qbq%d" % i, True) for i in range(2)]
    b_wiq = [B("a_wiq%d" % i, True) for i in range(2)]
    b_rl = [B("a_rl%d" % i) for i in range(2)]
    b_ex = [B("a_ex%d" % i) for i in range(3)]
    b_pm = [B("a_pm%d" % i) for i in range(3)]
    b_bis = [B("a_bis%d" % i) for i in range(2)]
    b_W = [B("a_W%d" % i) for i in range(2)]
    b_ob = [B("a_ob%d" % i) for i in range(2)]
    b_rc = [B("a_rc%d" % i) for i in range(2)]
    b_obst = [B("a_obst%d" % i) for i in range(2)]

    ident_bf = k.cbf[:, C_IDENT:C_IDENT + 128]
    ident_f = k.c32[:, C_IDENT:C_IDENT + 128]

    DMA(k, "sp", [bd["kiT"]], [b_kiT], out=kiT, in_=d["kiT"])
    for h in range(8):
        DMA(k, "sp" if h % 2 == 0 else "act", [bd["kbT"]], [b_kbT[h]], out=kbT[:, h, :], in_=d["kbT"][h])
    for q4 in range(4):
        DMA(k, "sp", [bd["vb_tok"]], [b_vb], out=vb[:, q4 * 8:(q4 + 1) * 8, :],
            in_=d["vb_tok"][q4 * 1024:(q4 + 1) * 1024, :].rearrange("(kt p) f -> p kt f", p=128))
    ps, b_ps = next_ps(k)
    O(k, "pe", "transpose", [k.b_kmax, k.b_c], [b_ps], out=ps[0:8, 0:128], in_=k.kmax, identity=ident_f)
    O(k, "dve", "tensor_reduce", [b_ps], [b_km8], out=km8, in_=ps[0:8, 0:128], axis=AX.X, op=ALU.max)
    O(k, "dve", "tensor_scalar", [k.b_c, b_km8], [b_dg], out=dg, in0=k.c32[0:8, C_IDENT:C_IDENT + 8], scalar1=km8[:, 0:1], scalar2=None,
      op0=ALU.mult)
    ps, b_ps = next_ps(k)
    O(k, "pe", "matmul", [k.b_c, b_dg], [b_ps], out=ps[:, 0:8], lhsT=k.c32[0:8, C_ONESD:C_ONESD + 128], rhs=dg, start=True, stop=True)
    O(k, "dve", "tensor_scalar", [b_ps], [b_cbias], out=cbias, in0=ps[:, 0:8], scalar1=-64.0, scalar2=None, op0=ALU.mult)

    k.ps_pool = [0, 1, 2, 3, 4, 5]
    k.ps_i = 0
    NQB = S // 128

    def idx(qb):
        par = qb % 2
        t0 = qb * 128
        nk = qb + 1
        N = nk * 128
        scp = sc[par]
        bsc = b_sc[par]
        bb = bis[par]
        bbis = b_bis[par]
        W = Wt[par]
        DMA(k, "sp", [bd["qiT"]], [b_qiq[par]], out=qiq[par], in_=d["qiT"][:, :, t0:t0 + 128].rearrange("h r t -> r h t"))
        DMA(k, "sp", [bd["wi_tok"]], [b_wiq[par]], out=wiq[par], in_=d["wi_tok"][t0:t0 + 128, :])
        for kg in range((N + 511) // 512):
            n = min(512, N - kg * 512)
            ksl = slice(kg * 512, kg * 512 + n)
            for h in range(8):
                ps, b_ps = next_ps(k)
                O(k, "pe", "matmul", [b_qiq[par], b_kiT], [b_ps], out=ps[:, 0:n], lhsT=qiq[par][:, h, :], rhs=kiT[:, ksl], start=True, stop=True)
                r = rl[h % 2]
                br = b_rl[h % 2]
                O(k, "act", "activation", [b_ps], [br], out=r[:, 0:n], in_=ps[:, 0:n], func=AF.Relu)
                if h == 0:
                    O(k, "dve", "tensor_scalar", [br, b_wiq[par]], [bsc], out=scp[:, ksl], in0=r[:, 0:n], scalar1=wiq[par][:, 0:1], scalar2=None,
                      op0=ALU.mult)
                else:
                    O(k, "dve", "scalar_tensor_tensor", [br, b_wiq[par], bsc], [bsc], out=scp[:, ksl], in0=r[:, 0:n], scalar=wiq[par][:, h:h + 1],
                      in1=scp[:, ksl], op0=ALU.mult, op1=ALU.add)
            yield
        dsl = slice(qb * 128, (qb + 1) * 128)
        O(k, "dve", "tensor_tensor", [bsc, k.b_c], [bsc], out=scp[:, dsl], in0=scp[:, dsl], in1=k.c32[:, C_NEGC:C_NEGC + 128], op=ALU.add)
        yield

    def idxB(qb):
        par = qb % 2
        t0 = qb * 128
        nk = qb + 1
        N = nk * 128
        scp = sc[par]
        bsc = b_sc[par]
        bb = bis[par]
        bbis = b_bis[par]
        W = Wt[par]
        DMA(k, "sp", [bd["qbT"]], [b_qbq[par]], out=qbq[par], in_=d["qbT"][:, :, t0:t0 + 128].rearrange("h r t -> r h t"))
        if qb >= 2:
            O(k, "dve", "tensor_reduce", [bsc], [bbis], out=bb["hi"], in_=scp[:, 0:N], axis=AX.X, op=ALU.max)
            O(k, "dve", "tensor_reduce", [bsc], [b_lo[par]], out=bb["lo"], in_=scp[:, 0:qb * 128], axis=AX.X, op=ALU.min)
            O(k, "dve", "tensor_tensor", [bbis, b_lo[par]], [bbis], out=bb["rng"], in0=bb["hi"], in1=bb["lo"], op=ALU.subtract)
            O(k, "dve", "tensor_scalar", [k.b_c, bbis], [b_W[par]], out=W[:, 0:16], in0=k.c32[:, C_POW4:C_POW4 + 16], scalar1=bb["rng"][:, 0:1],
              scalar2=None, op0=ALU.mult)
            yield
            cK = float(N - 2 * TOPK)
            for i in range(NIT // 2):
                O(k, "dve", "scalar_tensor_tensor", [k.b_c, b_W[par], b_lo[par]], [b_t3[par]], out=t3v[par], in0=k.c32[:, C_123:C_123 + 3],
                  scalar=W[:, i:i + 1], in1=bb["lo"].to_broadcast([128, 3]), op0=ALU.mult, op1=ALU.add)
                O(k, "dve", "tensor_scalar", [bsc, b_t3[par]], [b_junk, bbis], out=junk[:, 0:N], in0=scp[:, 0:N], scalar1=t3v[par][:, 0:1],
                  scalar2=None, op0=ALU.is_ge, op1=ALU.add, accum_out=bb["c1"])
                O(k, "act", "activation", [bsc, b_t3[par]], [b_s2[par]], out=junk2[:, 0:N], in_=scp[:, 0:N], func=AF.Sign,
                  bias=t3v[par][:, 1:2], scale=-1.0, accum_out=bb["s2"])
                O(k, "act", "activation", [bsc, b_t3[par]], [b_s3[par]], out=junk2[:, 0:N], in_=scp[:, 0:N], func=AF.Sign,
                  bias=t3v[par][:, 2:3], scale=-1.0, accum_out=bb["s3"])
                O(k, "dve", "tensor_scalar", [bbis], [bbis], out=bb["b1"], in0=bb["c1"], scalar1=float(TOPK), scalar2=None, op0=ALU.is_ge)
                O(k, "dve", "scalar_tensor_tensor", [b_s2[par], bbis], [bbis], out=bb["nb2"], in0=bb["s2"], scalar=cK, in1=bb["b1"],
                  op0=ALU.is_le, op1=ALU.add)
                O(k, "dve", "scalar_tensor_tensor", [b_s3[par], bbis], [bbis], out=bb["nb"], in0=bb["s3"], scalar=cK, in1=bb["nb2"],
                  op0=ALU.is_le, op1=ALU.add)
                O(k, "dve", "scalar_tensor_tensor", [bbis, b_W[par], b_lo[par]], [b_lo[par]], out=bb["lo"], in0=bb["nb"], scalar=W[:, i:i + 1],
                  in1=bb["lo"], op0=ALU.mult, op1=ALU.add)
                yield
            O(k, "dve", "tensor_scalar", [bsc, b_lo[par]], [b_m01], out=m01[:, 0:N], in0=scp[:, 0:N], scalar1=bb["lo"][:, 0:1], scalar2=None,
              op0=ALU.is_ge)
        else:
            O(k, "dve", "tensor_scalar", [bsc], [b_m01], out=m01[:, 0:N], in0=scp[:, 0:N], scalar1=-1e29, scalar2=None, op0=ALU.is_ge)
        yield
        for k8 in range((nk + 7) // 8):
            kts = list(range(k8 * 8, min(nk, k8 * 8 + 8)))
            ps, b_ps = next_ps(k)
            psb = ps[:, :].bitcast(BF16)
            for j, kt in enumerate(kts):
                O(k, "pe", "transpose", [b_m01, k.b_cbf], [b_ps], out=psb[:, j * 128:(j + 1) * 128], in_=m01[:, kt * 128:(kt + 1) * 128],
                  identity=ident_bf)
            O(k, "act", "activation", [b_ps], [b_maskT[par]], out=maskT[par][:, kts[0]:kts[-1] + 1, :],
              in_=psb[:, 0:len(kts) * 128].rearrange("p (a t) -> p a t", t=128), func=AF.Copy)
            yield

    ctr = [0]

    def att(qb):
        par = qb % 2
        t0 = qb * 128
        nk = qb + 1
        ngr = (nk + 3) // 4
        groups = [(h, kg) for h in range(8) for kg in range(ngr)]

        def emit_qk(h, kg):
            kts = list(range(kg * 4, min(nk, kg * 4 + 4)))
            ps, b_ps = next_ps(k, True)
            for j, kt in enumerate(kts):
                O(k, "pe", "matmul", [b_kbT[h], b_qbq[par]], [b_ps], out=ps[:, j * 128:(j + 1) * 128], lhsT=kbT[:, h, kt * 128:(kt + 1) * 128],
                  rhs=qbq[par][:, h, :], start=True, stop=True)
            return ps, b_ps, kts

        cur = emit_qk(*groups[0])
        for gi, (h, kg) in enumerate(groups):
            nxt = emit_qk(*groups[gi + 1]) if gi + 1 < len(groups) else None
            ps, b_ps, kts = cur
            n = len(kts) * 128
            po = k.ps[6 + h % 2]
            b_po = k.b_ps[6 + h % 2]
            i3 = ctr[0] % 3
            ctr[0] += 1
            O(k, "act", "activation", [b_ps, b_cbias], [b_ex[i3]], out=ex[i3][:, 0:n], in_=ps[:, 0:n], func=AF.Exp, bias=cbias[:, h:h + 1],
              scale=1.0)
            rel_ps(k, b_ps)
            O(k, "pool", "tensor_tensor", [b_ex[i3], b_maskT[par]], [b_pm[i3]], out=pm[i3][:, 0:n], in0=ex[i3][:, 0:n],
              in1=maskT[par][:, kts[0]:kts[-1] + 1, :].rearrange("p a t -> p (a t)"), op=ALU.mult)
            for j, kt in enumerate(kts):
                O(k, "pe", "matmul", [b_pm[i3], b_vb], [b_po], out=po[:, 0:65], lhsT=pm[i3][:, j * 128:(j + 1) * 128],
                  rhs=vb[:, kt, h * 65:(h + 1) * 65], start=(kt == 0), stop=(kt == nk - 1))
            if kg == ngr - 1:
                O(k, "act", "activation", [b_po], [b_obu[par]], out=obu[par][:, h, :], in_=po[:, 0:65], func=AF.Copy)
            cur = nxt
            yield
        O(k, "dve", "reciprocal", [b_obu[par]], [b_rc8], out=rc8, in_=obu[par][:, :, 64:65])
        O(k, "dve", "tensor_tensor", [b_obu[par], b_rc8], [b_ob[par]], out=ob[par].rearrange("p (h d) -> p h d", d=64), in0=obu[par][:, :, 0:64],
          in1=rc8.to_broadcast([128, 8, 64]), op=ALU.mult)
        ps, b_ps = next_ps(k)
        psb = ps[:, :].bitcast(BF16)
        for c4 in range(4):
            O(k, "pe", "transpose", [b_ob[par], k.b_cbf], [b_ps], out=psb[:, c4 * 128:(c4 + 1) * 128], in_=ob[par][:, c4 * 128:(c4 + 1) * 128],
              identity=ident_bf)
        O(k, "act", "activation", [b_ps], [b_obst[par]], out=obst[par], in_=psb[:, 0:512].rearrange("p (c t) -> p c t", t=128), func=AF.Copy)
        DMA(k, "sp", [b_obst[par]], [bd["obT"]], out=d["obT"][:, t0:t0 + 128].rearrange("(c p) t -> p c t", p=128), in_=obst[par])
        yield

    pipeline3(NQB, idx, idxB, att, 2)
    k.ps_pool = list(range(8))
    k.ps_i = 0


def merge_pass(k):
    P = k.P
    B = P.buf
    d = k.d
    bd = k.bd
    T = 512
    cv = Carver(k.big, k.PH0, k.PHLIM)
    w2 = cv.t([128, 8, 2048], BF16)
    wa = cv.t([128, 4, D], BF16)
    wb = cv.t([128, 4, D], BF16)
    wo = cv.t([128, 8, D], BF16)
    hT = cv.t([128, 8, T], F32)
    su = cv.t([128, 8, T], BF16)
    rstd = cv.t([128, T], F32)
    oa = cv.t([128, 4, T], BF16)
    obt = cv.t([128, 4, T], BF16)
    sga = [cv.t([128, T], F32) for _ in range(2)]
    sgb = [cv.t([128, T], F32) for _ in range(2)]
    mg = cv.t([128, 8, T], BF16)
    y = cv.t([128, 8, T], F32)
    b_w2 = [B("m_w2_%d" % c, True) for c in range(8)]
    b_wa = B("m_wa", True)
    b_wb = B("m_wb", True)
    b_wo = B("m_wo", True)
    b_h = B("m_h", True)
    b_su = B("m_su")
    b_rstd = B("m_rstd")
    b_oa = B("m_oa", True)
    b_ob = B("m_ob", True)
    b_sga = [B("m_sga%d" % i) for i in range(2)]
    b_sgb = [B("m_sgb%d" % i) for i in range(2)]
    b_mg = [B("m_mg%d" % i) for i in range(8)]
    b_y = B("m_y")
    g_pre = k.gsb[:, 16:24]
    g_post = k.gsb[:, 24:32]
    for c in range(8):
        DMA(k, "pool", [], [b_w2[c]], out=w2[:, c, :], in_=k.mix_w2[:, c, :])
    DMA(k, "pool", [], [b_wa], out=wa, in_=k.w_br_a)
    DMA(k, "pool", [], [b_wb], out=wb, in_=k.w_br_b)
    DMA(k, "pool", [], [b_wo], out=wo, in_=k.mix_w_out)
    for it in range(S // T):
        t0 = it * T
        DMA(k, "sp", [], [b_h], out=hT, in_=k.h1T[:, t0:t0 + T].rearrange("(c p) t -> p c t", p=128))
        DMA(k, "sp", [bd["oaT"]], [b_oa], out=oa, in_=d["oaT"][:, t0:t0 + T].rearrange("(c p) t -> p c t", p=128))
        DMA(k, "sp", [bd["obT"]], [b_ob], out=obt, in_=d["obT"][:, t0:t0 + T].rearrange("(c p) t -> p c t", p=128))
        for c in range(8):
            O(k, "act", "activation", [b_h], [b_su], out=su[:, c, :], in_=hT[:, c, :], func=AF.Square)
        rms_stats(k, su, b_su, rstd, b_rstd, T)
        for c in range(8):
            O(k, "dve", "scalar_tensor_tensor", [b_h, k.b_g, b_rstd], [b_su], out=su[:, c, :], in0=hT[:, c, :], scalar=g_pre[:, c:c + 1],
              in1=rstd, op0=ALU.mult, op1=ALU.mult)
        for dc in range(8):
            j = dc % 2
            dsl = slice(dc * 128, (dc + 1) * 128)
            pga, b_pga = next_ps(k)
            for c in range(8):
                O(k, "pe", "matmul", [b_w2[c], b_su], [b_pga], out=pga[:, 0:T], lhsT=w2[:, c, dc * 128:(dc + 1) * 128], rhs=su[:, c, :],
                  start=(c == 0), stop=(c == 7))
            pgb, b_pgb = next_ps(k)
            for c in range(8):
                O(k, "pe", "matmul", [b_w2[c], b_su], [b_pgb], out=pgb[:, 0:T], lhsT=w2[:, c, D + dc * 128:D + (dc + 1) * 128], rhs=su[:, c, :],
                  start=(c == 0), stop=(c == 7))
            pya, b_pya = next_ps(k)
            for c in range(4):
                O(k, "pe", "matmul", [b_wa, b_oa], [b_pya], out=pya[:, 0:T], lhsT=wa[:, c, dsl], rhs=oa[:, c, :], start=(c == 0), stop=(c == 3))
            pyb, b_pyb = next_ps(k)
            for c in range(4):
                O(k, "pe", "matmul", [b_wb, b_ob], [b_pyb], out=pyb[:, 0:T], lhsT=wb[:, c, dsl], rhs=obt[:, c, :], start=(c == 0), stop=(c == 3))
            O(k, "act", "activation", [b_pga], [b_sga[j]], out=sga[j], in_=pga[:, 0:T], func=AF.Sigmoid)
            O(k, "act", "activation", [b_pgb], [b_sgb[j]], out=sgb[j], in_=pgb[:, 0:T], func=AF.Sigmoid)
            O(k, "dve", "tensor_tensor", [b_sga[j], b_pya], [b_sga[j]], out=sga[j], in0=sga[j], in1=pya[:, 0:T], op=ALU.mult)
            O(k, "dve", "tensor_tensor", [b_sgb[j], b_pyb], [b_sgb[j]], out=sgb[j], in0=sgb[j], in1=pyb[:, 0:T], op=ALU.mult)
            O(k, "pool", "tensor_tensor", [b_sga[j], b_sgb[j]], [b_mg[dc]], out=mg[:, dc, :], in0=sga[j], in1=sgb[j], op=ALU.add)
        for dc in range(8):
            psy, b_psy = next_ps(k)
            for c in range(8):
                O(k, "pe", "matmul", [b_wo, b_mg[c]], [b_psy], out=psy[:, 0:T], lhsT=wo[:, c, dc * 128:(dc + 1) * 128], rhs=mg[:, c, :],
                  start=(c == 0), stop=(c == 7))
            O(k, "act", "activation", [b_psy], [b_y], out=y[:, dc, :], in_=psy[:, 0:T], func=AF.Copy)
            O(k, "act", "activation", [b_psy], [b_su], out=su[:, dc, :], in_=psy[:, 0:T], func=AF.Square)
        rms_stats(k, su, b_su, rstd, b_rstd, T)
        for c in range(8):
            O(k, "dve", "scalar_tensor_tensor", [b_y, k.b_g, b_rstd], [b_y], out=y[:, c, :], in0=y[:, c, :], scalar=g_post[:, c:c + 1],
              in1=rstd, op0=ALU.mult, op1=ALU.mult)
        for c in range(8):
            O(k, "pool", "tensor_tensor", [b_h, b_y], [b_h], out=hT[:, c, :], in0=hT[:, c, :], in1=y[:, c, :], op=ALU.add)
        DMA(k, "sp", [b_h], [bd["h2T"]], out=d["h2T"][:, t0:t0 + T].rearrange("(c p) t -> p c t", p=128), in_=hT)
```

```python
import os
import numpy as np
import concourse.bass as bass
import concourse.mybir as mybir
from concourse.bass_utils import run_bass_kernel_spmd
from contextlib import ExitStack

F32 = mybir.dt.float32
BF16 = mybir.dt.bfloat16
U8 = mybir.dt.uint8
ALU = mybir.AluOpType
AF = mybir.ActivationFunctionType
AX = mybir.AxisListType

ENGS = ("pe", "act", "dve", "pool", "sp")

S = 4096
D = 1024
DFF = 2816
NFC = DFF // 128
EPS = 1e-6
TF = 512
TM = 512


class Buf:
    __slots__ = ("name", "w", "r", "dkey", "excl")

    def __init__(self, name, dkey=None):
        self.name = name
        self.w = {}
        self.r = {}
        self.dkey = dkey
        self.excl = False


class Prog:
    def __init__(self, nc):
        self.nc = nc
        self.streams = {e: [] for e in ENGS}
        self.cnt = {e: 0 for e in ENGS}
        self.seen = {e: {} for e in ENGS}
        self.dma_keys = {}
        self.bufs = []
        self.pass_idx = 0
        self.persistent = True

    def buf(self, name, dma_dst=False):
        dkey = None
        if dma_dst:
            if self.persistent:
                dkey = ("d", name)
            else:
                dkey = ("p", self.pass_idx)
                self.pass_idx += 1
            self.dma_keys.setdefault(dkey, 0)
        b = Buf(name, dkey)
        self.bufs.append(b)
        return b

    def _emit(self, eng, fn, reads, writes, tok_key, tok_inc):
        waits = {}
        for b in reads:
            for k, v in b.w.items():
                if waits.get(k, 0) < v:
                    waits[k] = v
            if b.excl:
                for k, v in b.r.items():
                    if k != tok_key and waits.get(k, 0) < v:
                        waits[k] = v
        for b in writes:
            for k, v in b.w.items():
                if waits.get(k, 0) < v:
                    waits[k] = v
            for k, v in b.r.items():
                if waits.get(k, 0) < v:
                    waits[k] = v
        seen = self.seen[eng]
        wl = []
        for k, v in waits.items():
            if k == tok_key and (eng == "pe" or isinstance(k, tuple)):
                continue
            if seen.get(k, 0) >= v:
                continue
            seen[k] = v
            wl.append((k, v))
        if isinstance(tok_key, tuple):
            self.dma_keys[tok_key] += tok_inc
            val = self.dma_keys[tok_key]
        else:
            self.cnt[eng] += 1
            val = self.cnt[eng]
        for b in reads:
            if b.r.get(tok_key, 0) < val:
                b.r[tok_key] = val
        for b in writes:
            b.w = {tok_key: val}
            b.r = {}
        self.streams[eng].append((wl, fn, tok_key, tok_inc))

    def op(self, eng, fn, reads=(), writes=()):
        self._emit(eng, fn, list(reads), list(writes), eng, 1)

    def dma(self, eng, fn, reads, writes):
        dst = writes[0]
        assert dst.dkey is not None, dst.name
        self._emit(eng, fn, list(reads), list(writes), dst.dkey, 16)

    def barrier(self):
        allw = {e: self.cnt[e] for e in ENGS if self.cnt[e] > 0}
        for k, v in self.dma_keys.items():
            if v > 0:
                allw[k] = v
        for eng in ENGS:
            seen = self.seen[eng]
            wl = []
            for k, v in allw.items():
                if k == eng:
                    continue
                if seen.get(k, 0) >= v:
                    continue
                seen[k] = v
                wl.append((k, v))
            if wl:
                self.streams[eng].append((wl, None, None, 0))
        for b in self.bufs:
            b.w = {}
            b.r = {}
        self.pass_idx = 0

    def replay(self):
        nc = self.nc
        with ExitStack() as st:
            sems = {}
            for e in ENGS:
                if self.cnt[e] > 0:
                    sems[e] = st.enter_context(nc.semaphore("s_" + e))
            ndma = 0
            for i, k in enumerate(self.dma_keys):
                if self.dma_keys[k] > 0:
                    sems[k] = st.enter_context(nc.semaphore("sd%d" % i))
                    ndma += 1
            self.n_dma_sems = ndma
            block = st.enter_context(nc.Block())
            streams = self.streams

            def run(name, eng):
                for wl, fn, tk, inc in streams[name]:
                    for k, v in wl:
                        eng.wait_ge(sems[k], v)
                    if fn is not None:
                        fn(eng).then_inc(sems[tk], inc)

            @block.tensor
            def _(e):
                run("pe", e)

            @block.scalar
            def _(e):
                run("act", e)

            @block.vector
            def _(e):
                run("dve", e)

            @block.gpsimd
            def _(e):
                run("pool", e)

            @block.sync
            def _(e):
                run("sp", e)


class Carver:
    def __init__(self, big, start, limit):
        self.big = big
        self.off = start
        self.limit = limit

    def t(self, shape, dt, parts=128):
        esz = 4 if dt == F32 else 2
        n = int(np.prod(shape[1:])) * esz
        off = (self.off + 31) // 32 * 32
        assert off + n <= self.limit, ("SBUF overflow", off + n, self.limit)
        v = self.big[0:shape[0], off:off + n].bitcast(dt)
        if len(shape) == 3:
            v = v.rearrange("p (a b) -> p a b", b=shape[2])
        elif len(shape) == 4:
            v = v.rearrange("p (a b c) -> p a b c", b=shape[2], c=shape[3])
        self.off = off + n
        return v


C_IDENT = 0
C_ONESD = 128
C_BLK64 = 256
C_U64 = 384
C_ONES64 = 448
C_SL = 512
C_UI = 576
C_CAUS = 640
C_NEGC = 768
C_POW2 = 896
C_123 = 928
C_POW4 = 932
C_N = 948


def host_consts():
    c = np.zeros((128, C_N), np.float32)
    c[:, C_IDENT:C_IDENT + 128] = np.eye(128)
    c[:, C_ONESD:C_ONESD + 128] = 1.0 / D
    c[0:64, C_BLK64:C_BLK64 + 64] = 1.0
    c[64:128, C_BLK64 + 64:C_BLK64 + 128] = 1.0
    i = np.arange(64)
    c[0:64, C_U64:C_U64 + 64] = (i[:, None] <= i[None, :])
    c[0:64, C_ONES64:C_ONES64 + 64] = 1.0
    c[0:64, C_SL:C_SL + 64] = (i[:, None] > i[None, :])
    c[0:64, C_UI:C_UI + 64] = (i[None, :] >= i[:, None])
    t = np.arange(128)
    c[:, C_CAUS:C_CAUS + 128] = (t[None, :] <= t[:, None])
    c[:, C_NEGC:C_NEGC + 128] = np.where(t[None, :] <= t[:, None], 0.0, -1e30)
    c[:, C_POW2:C_POW2 + 32] = 2.0 ** -(np.arange(32) + 1.0)
    c[:, C_123:C_123 + 3] = np.array([1.0, 2.0, 3.0])
    c[:, C_POW4:C_POW4 + 16] = 4.0 ** -(np.arange(16) + 1.0)
    return c


class K:
    pass


def build(debug=None):
    nc = bass.Bass("TRN2", target_bir_lowering=False)
    k = K()
    build.k = k
    k.nc = nc
    k.debug = debug
    P = Prog(nc)
    k.P = P

    k.in_names = []
    skip_pre = bool(debug and "skip_pre" in debug)

    def din(name, shape, dt=F32):
        if skip_pre and int(np.prod(shape)) > 200000:
            return None
        k.in_names.append(name)
        return nc.dram_tensor(name, list(shape), dt, kind="ExternalInput").ap()

    def dscr(name, shape, dt=F32):
        kind = "ExternalOutput" if (debug and name in debug) else "Internal"
        return nc.dram_tensor(name, list(shape), dt, kind=kind).ap()

    k.xT = din("xT", [D, S])
    k.pT = din("pT", [256, S])
    k.consts = din("consts", [128, C_N])
    k.gains = din("gains", [128, 64])
    k.ffn_w_in = [din("ffn1_w_in", [128, 8, 2 * DFF]), din("ffn2_w_in", [128, 8, 2 * DFF])]
    k.ffn_w_out = [din("ffn1_w_out", [128, NFC, D]), din("ffn2_w_out", [128, NFC, D])]
    k.outT = nc.dram_tensor("outT", [D, S], F32, kind="ExternalOutput").ap()
    k.mix_w1 = din("mix_w1", [128, 8, NW1])
    k.mix_w2 = din("mix_w2", [128, 8, 2048])
    k.w_br_a = din("w_br_a", [128, 4, D])
    k.w_br_b = din("w_br_b", [128, 4, D])
    k.mix_w_out = din("mix_w_out", [128, 8, D])
    k.ple_wg = din("ple_wg", [128, 8, D])
    k.ple_wp = din("ple_wp", [128, 2, D])
    k.conv_w = din("conv_w", [128, 12, 4])
    k.rows_d = din("rows", [128, R_N])
    k.cs_d = din("cs", [128, 32, 16])
    k.h1T = dscr("h1T", [D, S])
    k.d = {}
    k.bd = {}
    for nm, shp, dt in (("qaT", [512, S], BF16), ("kaT", [512, S], BF16), ("ka_tok", [S, 512], BF16), ("va_tok", [S, 512], BF16),
                        ("z_tok", [S, 512], F32), ("gb_tok", [S, 16], F32), ("qbT", [8, 65, S], BF16), ("kbT", [8, 65, S], BF16),
                        ("vb_tok", [S, 8 * 65], BF16), ("qiT", [8, 64, S], BF16), ("kiT", [64, S], BF16), ("wi_tok", [S, 8], F32),
                        ("oaT", [512, S], BF16), ("obT", [512, S], BF16), ("h2T", [D, S], F32), ("h3T", [D, S], F32)):
        k.d[nm] = dscr(nm, shp, dt)
        k.bd[nm] = P.buf("d_" + nm, True)

    big = nc.alloc_sbuf_tensor("big", [128, 212480], U8)
    k.big = big
    k.ps = [nc.alloc_psum_tensor("ps%d" % i, [128, 512], F32) for i in range(8)]
    k.b_ps = [P.buf("ps%d" % i) for i in range(8)]
    for b_ in k.b_ps:
        b_.excl = True
    k.ps_i = 0
    k.ps_pool = list(range(8))
    k.ps_held = set()
    k.gdn_n = 64
    k.gdn_stages = None
    if debug:
        for f in debug:
            if f.startswith("gdn_n="):
                k.gdn_n = int(f.split("=")[1])
            if f.startswith("gdn_stages="):
                k.gdn_stages = int(f.split("=")[1])

    cv = Carver(big, 0, 9216)
    k.c32 = cv.t([128, C_N], F32)
    k.cbf = cv.t([128, C_N], BF16)
    k.gsb = cv.t([128, 64], F32)
    k.epsb = cv.t([128, 1], F32)
    k.rows = cv.t([128, R_N], F32)
    k.cs = cv.t([128, 32, 16], F32)
    k.kmax = cv.t([128, 8], F32)
    k.oneb = cv.t([128, 1], F32)
    k.b_rows = P.buf("rows", True)
    k.b_cs = P.buf("cs", True)
    k.b_kmax = P.buf("kmax")
    P.dma("sp", lambda e: e.dma_start(out=k.rows, in_=k.rows_d), [], [k.b_rows])
    P.dma("sp", lambda e: e.dma_start(out=k.cs, in_=k.cs_d), [], [k.b_cs])
    k.b_c = P.buf("consts", True)
    k.b_cbf = P.buf("cbf")
    k.b_g = P.buf("gains", True)
    k.b_eps = P.buf("eps")
    P.dma("sp", lambda e: e.dma_start(out=k.c32, in_=k.consts), [], [k.b_c])
    P.dma("sp", lambda e: e.dma_start(out=k.gsb, in_=k.gains), [], [k.b_g])
    P.op("dve", lambda e: e.tensor_copy(out=k.cbf, in_=k.c32), [k.b_c], [k.b_cbf])
    P.op("dve", lambda e: e.memset(k.epsb, EPS), [], [k.b_eps])
    P.op("dve", lambda e: e.memset(k.oneb, 1.0), [], [k.b_eps])
    k.PH0 = 9216
    P.persistent = False
    k.PHLIM = 212480

    if not (debug and "skip_pre" in debug):
        ffn_pass(k, 0, k.xT, k.h1T, final=False)
        P.barrier()
        if debug and "stop_after_ffn1" in debug:
            P.replay()
            return nc
        mixproj_pass(k)
        P.barrier()
    if debug and "stop_after_proj" in debug:
        P.replay()
        return nc
    gdn_pass(k)
    P.barrier()
    if debug and "stop_after_gdn" in debug:
        P.replay()
        return nc
    dsa_pass(k)
    P.barrier()
    if debug and "stop_after_dsa" in debug:
        P.replay()
        return nc
    merge_pass(k)
    P.barrier()
    ffn_pass(k, 1, k.d["h2T"], k.d["h3T"], final=False)
    P.barrier()
    ple_pass(k)
    P.barrier()
    P.replay()
    return nc


def O(k, eng, method, reads, writes, **kw):
    k.P.op(eng, lambda e: getattr(e, method)(**kw), reads, writes)


def DMA(k, eng, reads, writes, **kw):
    k.P.dma(eng, lambda e: e.dma_start(**kw), reads, writes)


def next_ps(k, hold=False):
    pool = k.ps_pool
    for _ in range(len(pool)):
        i = pool[k.ps_i % len(pool)]
        k.ps_i = (k.ps_i + 1) % len(pool)
        if i not in k.ps_held:
            if hold:
                k.ps_held.add(i)
            return k.ps[i], k.b_ps[i]
    raise RuntimeError("out of PSUM banks")


def rel_ps(k, b_ps):
    k.ps_held.discard(k.b_ps.index(b_ps))


def rms_stats(k, sq, b_sq, rstd, b_rstd, T, nchunk=8):
    P = k.P
    ps, b_ps = next_ps(k)
    ones = k.cbf[:, C_ONESD:C_ONESD + 128]
    for c in range(nchunk):
        P.op("pe", lambda e, c=c: e.matmul(ps[:, 0:T], lhsT=ones, rhs=sq[:, c, :], start=(c == 0), stop=(c == nchunk - 1)),
             [k.b_cbf, b_sq], [b_ps])
    P.op("act", lambda e: e.activation(out=rstd, in_=ps[:, 0:T], func=AF.Sqrt, bias=k.epsb, scale=1.0),
         [b_ps, k.b_eps], [b_rstd])
    P.op("dve", lambda e: e.reciprocal(out=rstd, in_=rstd), [b_rstd], [b_rstd])


def ffn_pass(k, which, srcT, dstT, final):
    P = k.P
    nc = k.nc
    T = TF
    cv = Carver(k.big, k.PH0, k.PHLIM)
    w_in = cv.t([128, 8, 2 * DFF], BF16)
    w_out = cv.t([128, NFC, D], BF16)
    hT = [cv.t([128, 8, T], F32)] * 2
    su = cv.t([128, 8, T], BF16)
    a = cv.t([128, NFC, T], BF16)
    y = cv.t([128, 8, T], F32)
    rstd = cv.t([128, T], F32)
    sg = [cv.t([128, T], F32)] * 2
    b_win = [P.buf("w_in%d_%d" % (which, c), True) for c in range(8)]
    b_wout = [P.buf("w_out%d_%d" % (which, c), True) for c in range(2)]
    b_h = [P.buf("hT%d" % which, True)] * 2
    b_su = P.buf("su%d" % which)
    b_a = [P.buf("a%d_%d" % (which, i)) for i in range(NFC)]
    b_y = P.buf("y%d" % which)
    b_rstd = P.buf("rstd%d" % which)
    b_sg = [P.buf("sg%d" % which)] * 2
    b_dst = P.buf("dst%d" % which, True)
    if final:
        wg = cv.t([128, 8, D], BF16)
        wp = cv.t([128, 2, D], BF16)
        pt = [cv.t([128, 2, T], BF16) for _ in range(2)]
        b_wg = P.buf("ple_wg", True)
        b_wp = P.buf("ple_wp", True)
        b_pt = [P.buf("ple_pt%d" % i, True) for i in range(2)]
        DMA(k, "pool", [], [b_wg], out=wg, in_=k.ple_wg)
        DMA(k, "pool", [], [b_wp], out=wp, in_=k.ple_wp)
    g_pre = k.gsb[:, 0:8] if which == 0 else k.gsb[:, 32:40]
    g_post = k.gsb[:, 8:16] if which == 0 else k.gsb[:, 40:48]
    wi_d = k.ffn_w_in[which]
    wo_d = k.ffn_w_out[which]
    for c in range(8):
        P.dma("pool", lambda e, c=c: e.dma_start(out=w_in[:, c, :], in_=wi_d[:, c, :]), [], [b_win[c]])
    for c in range(2):
        P.dma("pool", lambda e, c=c: e.dma_start(out=w_out[:, c * 11:(c + 1) * 11, :], in_=wo_d[:, c * 11:(c + 1) * 11, :]),
              [], [b_wout[c]])
    ghalf = cv.t([128, 8], F32)
    b_gh = P.buf("ghalf%d" % which)
    P.op("dve", lambda e: e.tensor_scalar(out=ghalf, in0=g_post, scalar1=0.5, scalar2=None, op0=ALU.mult),
         [k.b_g], [b_gh])
    ntile = S // T
    for it in range(ntile):
        t0 = it * T
        hb = hT[it % 2]
        bh = b_h[it % 2]
        P.dma("sp", lambda e, hb=hb, t0=t0: e.dma_start(out=hb, in_=srcT[:, t0:t0 + T].rearrange("(c p) t -> p c t", p=128)),
              [], [bh])
        for c in range(8):
            P.op("act", lambda e, c=c, hb=hb: e.activation(out=su[:, c, :], in_=hb[:, c, :], func=AF.Square), [bh], [b_su])
        rms_stats(k, su, b_su, rstd, b_rstd, T)
        for c in range(8):
            P.op("dve", lambda e, c=c, hb=hb: e.scalar_tensor_tensor(out=su[:, c, :], in0=hb[:, c, :], scalar=g_pre[:, c:c + 1],
                                                                   in1=rstd, op0=ALU.mult, op1=ALU.mult),
                 [bh, k.b_g, b_rstd], [b_su])
        for fc in range(NFC):
            psg, b_psg = next_ps(k)
            psu, b_psu = next_ps(k)
            for c in range(8):
                P.op("pe", lambda e, c=c, fc=fc, psg=psg: e.matmul(psg[:, 0:T], lhsT=w_in[:, c, fc * 128:(fc + 1) * 128],
                                                                 rhs=su[:, c, :], start=(c == 0), stop=(c == 7)),
                     [b_win[c], b_su], [b_psg])
            for c in range(8):
                P.op("pe", lambda e, c=c, fc=fc, psu=psu: e.matmul(psu[:, 0:T], lhsT=w_in[:, c, DFF + fc * 128:DFF + (fc + 1) * 128],
                                                                 rhs=su[:, c, :], start=(c == 0), stop=(c == 7)),
                     [b_win[c], b_su], [b_psu])
            sgb = sg[fc % 2]
            bsg = b_sg[fc % 2]
            P.op("act", lambda e, sgb=sgb, psg=psg: e.activation(out=sgb, in_=psg[:, 0:T], func=AF.Silu), [b_psg], [bsg])
            P.op("dve", lambda e, sgb=sgb, psu=psu, fc=fc: e.tensor_tensor(out=a[:, fc, :], in0=sgb, in1=psu[:, 0:T], op=ALU.mult),
                 [bsg, b_psu], [b_a[fc]])
        for dc in range(8):
            psy, b_psy = next_ps(k)
            for fc in range(NFC):
                P.op("pe", lambda e, dc=dc, fc=fc, psy=psy: e.matmul(psy[:, 0:T], lhsT=w_out[:, fc, dc * 128:(dc + 1) * 128],
                                                                   rhs=a[:, fc, :], start=(fc == 0), stop=(fc == NFC - 1)),
                     [b_wout[fc // 11], b_a[fc]], [b_psy])
            P.op("act", lambda e, dc=dc, psy=psy: e.activation(out=y[:, dc, :], in_=psy[:, 0:T], func=AF.Copy), [b_psy], [b_y])
            P.op("act", lambda e, dc=dc, psy=psy: e.activation(out=su[:, dc, :], in_=psy[:, 0:T], func=AF.Square), [b_psy], [b_su])
        rms_stats(k, su, b_su, rstd, b_rstd, T)
        for c in range(8):
            P.op("dve", lambda e, c=c: e.scalar_tensor_tensor(out=y[:, c, :], in0=y[:, c, :], scalar=ghalf[:, c:c + 1],
                                                            in1=rstd, op0=ALU.mult, op1=ALU.mult),
                 [b_y, b_gh, b_rstd], [b_y])
        for c in range(8):
            P.op("pool", lambda e, c=c, hb=hb: e.tensor_tensor(out=hb[:, c, :], in0=hb[:, c, :], in1=y[:, c, :], op=ALU.add),
                 [bh, b_y], [bh])
        if not final:
            P.dma("sp", lambda e, hb=hb, t0=t0: e.dma_start(out=dstT[:, t0:t0 + T].rearrange("(c p) t -> p c t", p=128), in_=hb),
                  [bh], [b_dst])
        else:
            gp_pre = k.gsb[:, 48:56]
            gp_post = k.gsb[:, 56:64]
            DMA(k, "pool", [], [b_pt[it % 2]], out=pt[it % 2], in_=k.pT[:, t0:t0 + T].rearrange("(c p) t -> p c t", p=128))
            for c in range(8):
                P.op("act", lambda e, c=c, hb=hb: e.activation(out=su[:, c, :], in_=hb[:, c, :], func=AF.Square), [bh], [b_su])
            rms_stats(k, su, b_su, rstd, b_rstd, T)
            for c in range(8):
                P.op("dve", lambda e, c=c, hb=hb: e.scalar_tensor_tensor(out=su[:, c, :], in0=hb[:, c, :], scalar=gp_pre[:, c:c + 1],
                                                                       in1=rstd, op0=ALU.mult, op1=ALU.mult),
                     [bh, k.b_g, b_rstd], [b_su])
            for dc in range(8):
                psg, b_psg = next_ps(k)
                for c in range(8):
                    O(k, "pe", "matmul", [b_wg, b_su], [b_psg], out=psg[:, 0:T], lhsT=wg[:, c, dc * 128:(dc + 1) * 128], rhs=su[:, c, :],
                      start=(c == 0), stop=(c == 7))
                psp, b_psp = next_ps(k)
                for c in range(2):
                    O(k, "pe", "matmul", [b_wp, b_pt[it % 2]], [b_psp], out=psp[:, 0:T], lhsT=wp[:, c, dc * 128:(dc + 1) * 128],
                      rhs=pt[it % 2][:, c, :], start=(c == 0), stop=(c == 1))
                sgb = sg[dc % 2]
                bsg = b_sg[dc % 2]
                O(k, "act", "activation", [b_psg], [bsg], out=sgb, in_=psg[:, 0:T], func=AF.Sigmoid)
                O(k, "dve", "tensor_tensor", [bsg, b_psp], [b_y], out=y[:, dc, :], in0=sgb, in1=psp[:, 0:T], op=ALU.mult)
            for c in range(8):
                O(k, "act", "activation", [b_y], [b_su], out=su[:, c, :], in_=y[:, c, :], func=AF.Square)
            rms_stats(k, su, b_su, rstd, b_rstd, T)
            for c in range(8):
                O(k, "dve", "scalar_tensor_tensor", [b_y, k.b_g, b_rstd], [b_y], out=y[:, c, :], in0=y[:, c, :], scalar=gp_post[:, c:c + 1],
                  in1=rstd, op0=ALU.mult, op1=ALU.mult)
            for c in range(8):
                O(k, "pool", "tensor_tensor", [bh, b_y], [bh], out=hb[:, c, :], in0=hb[:, c, :], in1=y[:, c, :], op=ALU.add)
            DMA(k, "sp", [bh], [b_dst], out=dstT[:, t0:t0 + T].rearrange("(c p) t -> p c t", p=128), in_=hb)


def ple_pass(k):
    P = k.P
    B = P.buf
    T = 512
    cv = Carver(k.big, k.PH0, k.PHLIM)
    wg = cv.t([128, 8, D], BF16)
    wp = cv.t([128, 2, D], BF16)
    hT = [cv.t([128, 8, T], F32) for _ in range(2)]
    pt = [cv.t([128, 2, T], BF16) for _ in range(2)]
    su = cv.t([128, 8, T], BF16)
    rstd = cv.t([128, T], F32)
    sg = [cv.t([128, T], F32) for _ in range(2)]
    y = cv.t([128, 8, T], F32)
    b_wg = B("ple_wg", True)
    b_wp = B("ple_wp", True)
    b_h = [B("ple_h%d" % i, True) for i in range(2)]
    b_pt = [B("ple_pt%d" % i, True) for i in range(2)]
    b_su = B("ple_su")
    b_rstd = B("ple_rstd")
    b_sg = [B("ple_sg%d" % i) for i in range(2)]
    b_y = B("ple_y")
    b_dst = B("ple_dst", True)
    gp_pre = k.gsb[:, 48:56]
    gp_post = k.gsb[:, 56:64]
    DMA(k, "pool", [], [b_wg], out=wg, in_=k.ple_wg)
    DMA(k, "pool", [], [b_wp], out=wp, in_=k.ple_wp)
    src = k.d["h3T"]
    for it in range(S // T):
        t0 = it * T
        hb = hT[it % 2]
        bh = b_h[it % 2]
        DMA(k, "sp", [], [bh], out=hb, in_=src[:, t0:t0 + T].rearrange("(c p) t -> p c t", p=128))
        DMA(k, "pool", [], [b_pt[it % 2]], out=pt[it % 2], in_=k.pT[:, t0:t0 + T].rearrange("(c p) t -> p c t", p=128))
        for c in range(8):
            O(k, "act", "activation", [bh], [b_su], out=su[:, c, :], in_=hb[:, c, :], func=AF.Square)
        rms_stats(k, su, b_su, rstd, b_rstd, T)
        for c in range(8):
            O(k, "dve", "scalar_tensor_tensor", [bh, k.b_g, b_rstd], [b_su], out=su[:, c, :], in0=hb[:, c, :], scalar=gp_pre[:, c:c + 1],
              in1=rstd, op0=ALU.mult, op1=ALU.mult)
        for dc in range(8):
            psg, b_psg = next_ps(k)
            for c in range(8):
                O(k, "pe", "matmul", [b_wg, b_su], [b_psg], out=psg[:, 0:T], lhsT=wg[:, c, dc * 128:(dc + 1) * 128], rhs=su[:, c, :],
                  start=(c == 0), stop=(c == 7))
            psp, b_psp = next_ps(k)
            for c in range(2):
                O(k, "pe", "matmul", [b_wp, b_pt[it % 2]], [b_psp], out=psp[:, 0:T], lhsT=wp[:, c, dc * 128:(dc + 1) * 128],
                  rhs=pt[it % 2][:, c, :], start=(c == 0), stop=(c == 1))
            sgb = sg[dc % 2]
            bsg = b_sg[dc % 2]
            O(k, "act", "activation", [b_psg], [bsg], out=sgb, in_=psg[:, 0:T], func=AF.Sigmoid)
            O(k, "dve", "tensor_tensor", [bsg, b_psp], [b_y], out=y[:, dc, :], in0=sgb, in1=psp[:, 0:T], op=ALU.mult)
        for c in range(8):
            O(k, "act", "activation", [b_y], [b_su], out=su[:, c, :], in_=y[:, c, :], func=AF.Square)
        rms_stats(k, su, b_su, rstd, b_rstd, T)
        for c in range(8):
            O(k, "dve", "scalar_tensor_tensor", [b_y, k.b_g, b_rstd], [b_y], out=y[:, c, :], in0=y[:, c, :], scalar=gp_post[:, c:c + 1],
              in1=rstd, op0=ALU.mult, op1=ALU.mult)
        for c in range(8):
            O(k, "pool", "tensor_tensor", [bh, b_y], [bh], out=hb[:, c, :], in0=hb[:, c, :], in1=y[:, c, :], op=ALU.add)
        DMA(k, "sp", [bh], [b_dst], out=k.outT[:, t0:t0 + T].rearrange("(c p) t -> p c t", p=128), in_=hb)


def host_rows(inputs):
    r = np.concatenate([np.asarray(inputs[n][0], np.float32).ravel() for n in ("dt_bias", "a_log", "dn_norm_g", "idx_k_norm_g")])
    return np.ascontiguousarray(np.broadcast_to(r[None, :], (128, R_N)))


def host_cs():
    pos = np.arange(S, dtype=np.float32)
    inv = (np.float32(500000.0) ** (-np.arange(0, 16, 2, dtype=np.float32) / np.float32(16.0))).astype(np.float32)
    ang = (pos[:, None] * inv[None, :]).astype(np.float32)
    cs = np.concatenate([np.cos(ang), np.sin(ang)], axis=1).astype(np.float32)
    return np.ascontiguousarray(cs.reshape(32, 128, 16).transpose(1, 0, 2))


def _fm(v):
    return np.ascontiguousarray(np.asarray(v, np.float32).reshape(8, 128).T)


def kernel(**inputs):
    dbg = os.environ.get("KDEBUG")
    debug = set(dbg.split(",")) if dbg else None
    x = np.asarray(inputs["x"], np.float32)
    p = np.asarray(inputs["p"], np.float32)[0]
    nb = x.shape[0]
    gains = np.concatenate([_fm(inputs[n][0]) for n in (
        "ffn1_norm_pre", "ffn1_norm_post", "mix_norm_pre", "mix_norm_post", "ffn2_norm_pre",
        "ffn2_norm_post", "ple_norm_pre", "ple_norm_post")], axis=1)

    def w_in_l(w):
        w = np.asarray(w, np.float32)
        return np.ascontiguousarray(w.reshape(w.shape[0] // 128, 128, -1).transpose(1, 0, 2))

    def w_out_l(w):
        return np.ascontiguousarray(np.asarray(w, np.float32).reshape(NFC, 128, -1).transpose(1, 0, 2))

    shared = {
        "consts": host_consts(),
        "gains": np.ascontiguousarray(gains),
        "mix_w1": w_in_l(np.asarray(inputs["mix_w_in"][0])[:, 0:NW1]),
        "mix_w2": w_in_l(np.asarray(inputs["mix_w_in"][0])[:, NW1:]),
        "w_br_a": w_in_l(inputs["w_br_a"][0]),
        "w_br_b": w_in_l(inputs["w_br_b"][0]),
        "mix_w_out": w_in_l(inputs["mix_w_out"][0]),
        "ple_wg": w_in_l(inputs["ple_w_gate"][0]),
        "ple_wp": w_in_l(inputs["ple_w_proj"][0]),
        "conv_w": np.ascontiguousarray(np.asarray(inputs["conv_w"][0], np.float32).T.reshape(12, 128, 4).transpose(1, 0, 2)),
        "rows": host_rows(inputs),
        "cs": host_cs(),
        "ffn1_w_in": w_in_l(inputs["ffn1_w_in"][0]),
        "ffn2_w_in": w_in_l(inputs["ffn2_w_in"][0]),
        "ffn1_w_out": w_out_l(inputs["ffn1_w_out"][0]),
        "ffn2_w_out": w_out_l(inputs["ffn2_w_out"][0]),
    }
    in_maps = []
    for b in range(nb):
        m = dict(shared)
        m["xT"] = np.ascontiguousarray(x[b].T)
        m["pT"] = np.ascontiguousarray(p[b].T)
        in_maps.append(m)
    nc = build(debug)
    if debug:
        in_maps = [{n: v for n, v in in_maps[0].items() if n in build.k.in_names}]
        nb = 1
    res = run_bass_kernel_spmd(nc, in_maps, core_ids=list(range(nb)))
    if debug:
        kernel.last = res.results
    out = np.stack([np.ascontiguousarray(r["outT"].T) for r in res.results], axis=0)
    return out.astype(np.float32)


R_DTB = 0
R_ALOG = 8
R_DNG = 16
R_IKG = 80
R_N = 144
NW1 = 4184


def mixproj_pass(k):
    P = k.P
    T = TM
    cv = Carver(k.big, k.PH0, k.PHLIM)
    w1 = cv.t([128, 8, NW1], BF16)
    hT = cv.t([128, 8, T], F32)
    su = cv.t([128, 8, T], BF16)
    rstd = cv.t([128, T], F32)
    cw = cv.t([128, 12, 4], F32)
    hal = cv.t([128, 12, 3], F32)
    cb = [cv.t([128, T + 3], F32) for _ in range(2)]
    acc = [cv.t([128, T], F32) for _ in range(2)]
    sil = [cv.t([128, T], F32) for _ in range(2)]
    sqb = [cv.t([128, T], BF16) for _ in range(2)]
    rn = [cv.t([128, T], F32) for _ in range(2)]
    qkT = cv.t([128, 8, T], BF16)
    vT = cv.t([128, 4, T], BF16)
    tokst = [cv.t([128, 512], BF16) for _ in range(2)]
    xs = [cv.t([128, 8, 64], F32) for _ in range(2)]
    xo = [cv.t([128, 8, 65], BF16) for _ in range(2)]
    tmp = [cv.t([128, 8, 8], F32) for _ in range(4)]
    sq5 = cv.t([128, 8, 64], F32)
    n2 = cv.t([128, 8], F32)
    zst = [cv.t([128, 512], F32) for _ in range(2)]
    gbst = [cv.t([128, 16], F32) for _ in range(2)]
    abt = cv.t([128, 16], F32)
    wist = [cv.t([128, 8], F32) for _ in range(2)]
    kis = cv.t([128, 72], F32)
    kio = cv.t([128, 64], BF16)
    kss = cv.t([128, 1], F32)
    vo = [cv.t([128, 8, 65], BF16) for _ in range(2)]
    st_qb = cv.t([65, 8, T], BF16)
    st_kb = cv.t([65, 8, T], BF16)
    st_qi = cv.t([64, 8, T], BF16)
    st_ki = cv.t([64, T], BF16)
    nea = cv.t([128, 8], F32)

    B = P.buf
    b_w1 = [B("w1_%d" % c, True) for c in range(8)]
    b_h = B("mp_h", True)
    b_su = B("mp_su")
    b_rstd = B("mp_rstd")
    b_cw = B("mp_cw", True)
    b_hal = [B("mp_hal%d" % i) for i in range(12)]
    b_cb = [B("mp_cb%d" % i) for i in range(2)]
    b_acc = [B("mp_acc%d" % i) for i in range(2)]
    b_sil = [B("mp_sil%d" % i) for i in range(2)]
    b_sqb = [B("mp_sqb%d" % i) for i in range(2)]
    b_rn = [B("mp_rn%d" % i) for i in range(2)]
    b_qkT = [B("mp_qkT%d" % i) for i in range(8)]
    b_vT = [B("mp_vT%d" % i) for i in range(4)]
    b_tokst = [B("mp_tokst%d" % i) for i in range(2)]
    b_xs = [B("mp_xs%d" % i) for i in range(2)]
    b_xo = [B("mp_xo%d" % i) for i in range(2)]
    b_tmp = [B("mp_tmp%d" % i) for i in range(4)]
    b_sq5 = B("mp_sq5")
    b_n2 = B("mp_n2")
    b_zst = [B("mp_zst%d" % i) for i in range(2)]
    b_gbst = [B("mp_gbst%d" % i) for i in range(2)]
    b_abt = B("mp_abt")
    b_wist = [B("mp_wist%d" % i) for i in range(2)]
    b_kis = B("mp_kis")
    b_kio = B("mp_kio")
    b_kss = B("mp_kss")
    b_vo = [B("mp_vo%d" % i) for i in range(2)]
    b_stqb = B("mp_stqb")
    b_stkb = B("mp_stkb")
    b_stqi = B("mp_stqi")
    b_stki = B("mp_stki")
    b_nea = B("mp_nea")
    d = k.d
    bd = k.bd

    ident_bf = k.cbf[:, C_IDENT:C_IDENT + 128]
    blk64 = k.cbf[:, C_BLK64:C_BLK64 + 128]
    g_pre = k.gsb[:, 16:24]

    for c in range(8):
        DMA(k, "pool", [], [b_w1[c]], out=w1[:, c, :], in_=k.mix_w1[:, c, :])
    DMA(k, "sp", [], [b_cw], out=cw, in_=k.conv_w)
    for fc in range(12):
        O(k, "pool", "memset", [], [b_hal[fc]], ap=hal[:, fc, :], constant=0.0)
    O(k, "act", "activation", [k.b_rows], [b_nea], out=nea, in_=k.rows[:, R_ALOG:R_ALOG + 8], func=AF.Exp)
    O(k, "dve", "tensor_scalar", [b_nea], [b_nea], out=nea, in0=nea, scalar1=-1.0, scalar2=None, op0=ALU.mult)
    O(k, "dve", "memset", [], [k.b_kmax], ap=k.kmax, constant=0.0)
    for i in range(2):
        O(k, "pool", "memset", [], [b_vo[i]], ap=vo[i][:, :, 64:65], constant=1.0)

    ntile = S // T
    for it in range(ntile):
        t0 = it * T
        DMA(k, "sp", [], [b_h], out=hT, in_=k.h1T[:, t0:t0 + T].rearrange("(c p) t -> p c t", p=128))
        for c in range(8):
            O(k, "act", "activation", [b_h], [b_su], out=su[:, c, :], in_=hT[:, c, :], func=AF.Square)
        rms_stats(k, su, b_su, rstd, b_rstd, T)
        for c in range(8):
            O(k, "dve", "scalar_tensor_tensor", [b_h, k.b_g, b_rstd], [b_su], out=su[:, c, :], in0=hT[:, c, :],
              scalar=g_pre[:, c:c + 1], in1=rstd, op0=ALU.mult, op1=ALU.mult)
        for fc in range(12):
            ps, b_ps = next_ps(k)
            for c in range(8):
                O(k, "pe", "matmul", [b_w1[c], b_su], [b_ps], out=ps[:, 0:T], lhsT=w1[:, c, fc * 128:(fc + 1) * 128],
                  rhs=su[:, c, :], start=(c == 0), stop=(c == 7))
            j = fc % 2
            O(k, "pool", "tensor_copy", [b_hal[fc]], [b_cb[j]], out=cb[j][:, 0:3], in_=hal[:, fc, :])
            O(k, "act", "activation", [b_ps], [b_cb[j]], out=cb[j][:, 3:T + 3], in_=ps[:, 0:T], func=AF.Copy)
            O(k, "pool", "tensor_copy", [b_cb[j]], [b_hal[fc]], out=hal[:, fc, :], in_=cb[j][:, T:T + 3])
            O(k, "dve", "tensor_scalar", [b_cb[j], b_cw], [b_acc[j]], out=acc[j], in0=cb[j][:, 0:T],
              scalar1=cw[:, fc, 0:1], scalar2=None, op0=ALU.mult)
            for jj in range(1, 4):
                O(k, "dve", "scalar_tensor_tensor", [b_cb[j], b_cw, b_acc[j]], [b_acc[j]], out=acc[j], in0=cb[j][:, jj:jj + T],
                  scalar=cw[:, fc, jj:jj + 1], in1=acc[j], op0=ALU.mult, op1=ALU.add)
            if fc < 8:
                O(k, "act", "activation", [b_acc[j]], [b_sil[j]], out=sil[j], in_=acc[j], func=AF.Silu)
                O(k, "act", "activation", [b_sil[j]], [b_sqb[j]], out=sqb[j], in_=sil[j], func=AF.Square)
                ps2, b_ps2 = next_ps(k)
                O(k, "pe", "matmul", [k.b_cbf, b_sqb[j]], [b_ps2], out=ps2[:, 0:T], lhsT=blk64, rhs=sqb[j], start=True, stop=True)
                O(k, "act", "activation", [b_ps2, k.b_eps], [b_rn[j]], out=rn[j], in_=ps2[:, 0:T], func=AF.Sqrt, bias=k.epsb, scale=1.0)
                O(k, "dve", "reciprocal", [b_rn[j]], [b_rn[j]], out=rn[j], in_=rn[j])
                O(k, "dve", "scalar_tensor_tensor", [b_sil[j], b_rn[j]], [b_qkT[fc]], out=qkT[:, fc, :], in0=sil[j],
                  scalar=(0.125 if fc < 4 else 1.0), in1=rn[j], op0=ALU.mult, op1=ALU.mult)
            else:
                O(k, "act", "activation", [b_acc[j]], [b_vT[fc - 8]], out=vT[:, fc - 8, :], in_=acc[j], func=AF.Silu)
        DMA(k, "sp", [b_qkT[i] for i in range(4)], [bd["qaT"]], out=d["qaT"][:, t0:t0 + T].rearrange("(c p) t -> p c t", p=128),
            in_=qkT[:, 0:4, :])
        DMA(k, "sp", [b_qkT[i] for i in range(4, 8)], [bd["kaT"]], out=d["kaT"][:, t0:t0 + T].rearrange("(c p) t -> p c t", p=128),
            in_=qkT[:, 4:8, :])
        for sub in range(T // 128):
            for wh in range(2):
                ps, b_ps = next_ps(k)
                psb = ps[:, :].bitcast(BF16)
                for c4 in range(4):
                    if wh == 0:
                        O(k, "pe", "transpose", [b_qkT[4 + c4], k.b_cbf], [b_ps], out=psb[:, c4 * 128:(c4 + 1) * 128],
                          in_=qkT[:, 4 + c4, sub * 128:(sub + 1) * 128], identity=ident_bf)
                    else:
                        O(k, "pe", "transpose", [b_vT[c4], k.b_cbf], [b_ps], out=psb[:, c4 * 128:(c4 + 1) * 128],
                          in_=vT[:, c4, sub * 128:(sub + 1) * 128], identity=ident_bf)
                O(k, "act", "activation", [b_ps], [b_tokst[wh]], out=tokst[wh], in_=psb[:, 0:512], func=AF.Copy)
                dst = "ka_tok" if wh == 0 else "va_tok"
                DMA(k, "sp", [b_tokst[wh]], [bd[dst]], out=d[dst][t0 + sub * 128:t0 + (sub + 1) * 128, :], in_=tokst[wh])
        for sub in range(T // 128):
            blk = (t0 // 128) + sub
            tsl = slice(sub * 128, (sub + 1) * 128)
            r0 = t0 + sub * 128
            cosb = k.cs[:, blk, 0:8].unsqueeze(1).to_broadcast([128, 8, 8])
            sinb = k.cs[:, blk, 8:16].unsqueeze(1).to_broadcast([128, 8, 8])

            def proj(col0, n):
                ps, b_ps = next_ps(k)
                for c in range(8):
                    O(k, "pe", "matmul", [b_w1[c], b_su], [b_ps], out=ps[:, 0:n], lhsT=su[:, c, tsl], rhs=w1[:, c, col0:col0 + n],
                      start=(c == 0), stop=(c == 7))
                return ps, b_ps

            def rope(x3, bx, o3, bo, nh):
                cb_ = cosb[:, 0:nh, :]
                sb_ = sinb[:, 0:nh, :]
                x1 = x3[:, :, 0:8]
                x2 = x3[:, :, 8:16]
                O(k, "dve", "tensor_tensor", [bx, k.b_cs], [b_tmp[0]], out=tmp[0][:, 0:nh, :], in0=x1, in1=cb_, op=ALU.mult)
                O(k, "dve", "tensor_tensor", [bx, k.b_cs], [b_tmp[1]], out=tmp[1][:, 0:nh, :], in0=x2, in1=sb_, op=ALU.mult)
                O(k, "pool", "tensor_tensor", [bx, k.b_cs], [b_tmp[2]], out=tmp[2][:, 0:nh, :], in0=x2, in1=cb_, op=ALU.mult)
                O(k, "pool", "tensor_tensor", [bx, k.b_cs], [b_tmp[3]], out=tmp[3][:, 0:nh, :], in0=x1, in1=sb_, op=ALU.mult)
                O(k, "dve", "tensor_tensor", [b_tmp[0], b_tmp[1]], [bo], out=o3[:, :, 0:8], in0=tmp[0][:, 0:nh, :],
                  in1=tmp[1][:, 0:nh, :], op=ALU.subtract)
                O(k, "pool", "tensor_tensor", [b_tmp[2], b_tmp[3]], [bo], out=o3[:, :, 8:16], in0=tmp[2][:, 0:nh, :],
                  in1=tmp[3][:, 0:nh, :], op=ALU.add)
                O(k, "act", "activation", [bx], [bo], out=o3[:, :, 16:64], in_=x3[:, :, 16:64], func=AF.Copy)

            ps, b_ps = proj(1536, 512)
            j = sub % 2
            O(k, "act", "activation", [b_ps], [b_zst[j]], out=zst[j], in_=ps[:, 0:512], func=AF.Silu)
            DMA(k, "sp", [b_zst[j]], [bd["z_tok"]], out=d["z_tok"][r0:r0 + 128, :], in_=zst[j])
            ps, b_ps = proj(2048, 16)
            O(k, "dve", "tensor_tensor", [b_ps, k.b_rows], [b_abt], out=abt[:, 0:8], in0=ps[:, 0:8], in1=k.rows[:, R_DTB:R_DTB + 8],
              op=ALU.add)
            O(k, "act", "activation", [b_abt], [b_abt], out=abt[:, 0:8], in_=abt[:, 0:8], func=AF.Exp)
            O(k, "act", "activation", [b_abt], [b_abt], out=abt[:, 0:8], in_=abt[:, 0:8], func=AF.Ln, bias=k.oneb, scale=1.0)
            O(k, "dve", "tensor_tensor", [b_abt, b_nea], [b_gbst[j]], out=gbst[j][:, 0:8], in0=abt[:, 0:8], in1=nea, op=ALU.mult)
            O(k, "act", "activation", [b_ps], [b_gbst[j]], out=gbst[j][:, 8:16], in_=ps[:, 8:16], func=AF.Sigmoid)
            DMA(k, "sp", [b_gbst[j]], [bd["gb_tok"]], out=d["gb_tok"][r0:r0 + 128, :], in_=gbst[j])
            for wh, col0 in ((0, 2064), (1, 2576), (2, 3600)):
                ps, b_ps = proj(col0, 512)
                jj = wh % 2
                x3 = xs[jj]
                O(k, "act", "activation", [b_ps], [b_xs[jj]], out=x3, in_=ps[:, 0:512].rearrange("p (h d) -> p h d", d=64),
                  func=AF.Copy, scale=(0.125 if wh == 0 else 1.0))
                rope(x3, b_xs[jj], xo[jj], b_xo[jj], 8)
                if wh < 2:
                    O(k, "dve", "tensor_tensor", [b_xs[jj]], [b_sq5], out=sq5, in0=x3, in1=x3, op=ALU.mult)
                    O(k, "dve", "tensor_reduce", [b_sq5], [b_n2], out=n2, in_=sq5, axis=AX.X, op=ALU.add)
                    if wh == 0:
                        O(k, "dve", "tensor_scalar", [b_n2], [b_xo[jj]], out=xo[jj][:, :, 64:65], in0=n2.unsqueeze(2), scalar1=-4.0,
                          scalar2=None, op0=ALU.mult)
                    else:
                        O(k, "pool", "memset", [], [b_xo[jj]], ap=xo[jj][:, :, 64:65], constant=1.0)
                        O(k, "dve", "tensor_tensor", [b_n2, k.b_kmax], [k.b_kmax], out=k.kmax, in0=k.kmax, in1=n2, op=ALU.max)
                nr = 65 if wh < 2 else 64
                pst, b_pst = next_ps(k)
                pstb = pst[0:nr, :].bitcast(BF16).rearrange("p (h t) -> p h t", t=128)
                for h in range(8):
                    O(k, "pe", "transpose", [b_xo[jj], k.b_cbf], [b_pst], out=pstb[:, h, :], in_=xo[jj][:, h, 0:nr], identity=ident_bf)
                stg, bst = ((st_qb, b_stqb), (st_kb, b_stkb), (st_qi, b_stqi))[wh]
                O(k, "act", "activation", [b_pst], [bst], out=stg[:, :, tsl], in_=pstb, func=AF.Copy)
            ps, b_ps = proj(3088, 512)
            O(k, "act", "activation", [b_ps], [b_vo[j]], out=vo[j][:, :, 0:64], in_=ps[:, 0:512].rearrange("p (h d) -> p h d", d=64),
              func=AF.Copy)
            DMA(k, "sp", [b_vo[j]], [bd["vb_tok"]], out=d["vb_tok"][r0:r0 + 128, :], in_=vo[j].rearrange("p h d -> p (h d)"))
            ps, b_ps = proj(4112, 72)
            O(k, "act", "activation", [b_ps], [b_kis], out=kis, in_=ps[:, 0:72], func=AF.Copy)
            O(k, "dve", "tensor_tensor", [b_kis], [b_sq5], out=sq5[:, 0, :], in0=kis[:, 0:64], in1=kis[:, 0:64], op=ALU.mult)
            O(k, "dve", "tensor_reduce", [b_sq5], [b_kss], out=kss, in_=sq5[:, 0, :], axis=AX.X, op=ALU.add)
            O(k, "act", "activation", [b_kss, k.b_eps], [b_kss], out=kss, in_=kss, func=AF.Sqrt, bias=k.epsb, scale=1.0 / 64.0)
            O(k, "dve", "reciprocal", [b_kss], [b_kss], out=kss, in_=kss)
            O(k, "dve", "scalar_tensor_tensor", [b_kis, b_kss, k.b_rows], [b_xs[0]], out=xs[0][:, 0, :], in0=kis[:, 0:64], scalar=kss[:, 0:1],
              in1=k.rows[:, R_IKG:R_IKG + 64], op0=ALU.mult, op1=ALU.mult)
            rope(xs[0][:, 0:1, :], b_xs[0], xo[0][:, 0:1, :], b_xo[0], 1)
            pst, b_pst = next_ps(k)
            pstb = pst[0:64, :].bitcast(BF16)
            O(k, "pe", "transpose", [b_xo[0], k.b_cbf], [b_pst], out=pstb[:, 0:128], in_=xo[0][:, 0, 0:64], identity=ident_bf)
            O(k, "act", "activation", [b_pst], [b_stki], out=st_ki[:, tsl], in_=pstb[:, 0:128], func=AF.Copy)
            O(k, "dve", "tensor_scalar", [b_kis], [b_wist[j]], out=wist[j], in0=kis[:, 64:72], scalar1=float(0.125 * 8 ** -0.5),
              scalar2=None, op0=ALU.mult)
            DMA(k, "sp", [b_wist[j]], [bd["wi_tok"]], out=d["wi_tok"][r0:r0 + 128, :], in_=wist[j])
        DMA(k, "sp", [b_stqb], [bd["qbT"]], out=d["qbT"][:, :, t0:t0 + T].rearrange("h r t -> r h t"), in_=st_qb)
        DMA(k, "sp", [b_stkb], [bd["kbT"]], out=d["kbT"][:, :, t0:t0 + T].rearrange("h r t -> r h t"), in_=st_kb)
        DMA(k, "sp", [b_stqi], [bd["qiT"]], out=d["qiT"][:, :, t0:t0 + T].rearrange("h r t -> r h t"), in_=st_qi)
        DMA(k, "sp", [b_stki], [bd["kiT"]], out=d["kiT"][:, t0:t0 + T], in_=st_ki)


def pipeline(n_items, pre_fn, seq_fn, nset, max_pre=2):
    pre_done = [False] * n_items
    seq_done = [0]
    active = []
    nxt = [0]

    def seq_all():
        for n in range(n_items):
            while not pre_done[n]:
                yield
            for _ in seq_fn(n):
                yield
            seq_done[0] = n + 1
            yield

    sg = seq_all()
    alive = True
    while alive or active:
        while nxt[0] < n_items and len(active) < max_pre and nxt[0] < seq_done[0] + nset:
            active.append((nxt[0], pre_fn(nxt[0])))
            nxt[0] += 1
        for item in list(active):
            n, g = item
            try:
                next(g)
            except StopIteration:
                pre_done[n] = True
                active.remove(item)
        if alive:
            try:
                next(sg)
            except StopIteration:
                alive = False


def pipeline3(n_items, fA, fB, fC, nbuf=2):
    done = [0, 0, 0]
    nxt = [0, 0, 0]
    gens = [None, None, None]
    fs = [fA, fB, fC]

    def can_start(si, i):
        if i >= n_items:
            return False
        if si == 0:
            return i < done[1] + nbuf
        if si == 1:
            return done[0] > i and i < done[2] + nbuf
        return done[1] > i

    while done[2] < n_items:
        progressed = False
        for si in range(3):
            if gens[si] is None and can_start(si, nxt[si]):
                gens[si] = fs[si](nxt[si])
                nxt[si] += 1
            if gens[si] is not None:
                progressed = True
                try:
                    next(gens[si])
                except StopIteration:
                    gens[si] = None
                    done[si] += 1
        assert progressed, "pipeline3 deadlock"


def gdn_pass(k):
    P = k.P
    B = P.buf
    d = k.d
    bd = k.bd
    cv = Carver(k.big, k.PH0, k.PHLIM)
    NSET = 3
    H8 = [64, 8, 64]
    gb_all = cv.t([64, 64, 16], F32)
    gcs = {nm: cv.t([64, 64, 8], F32) for nm in ("gc", "gl", "egc", "ekd", "gtot", "beg")}
    negU = cv.t([64, 64], F32)
    grp = [{nm: cv.t([64, 8, 512], BF16) for nm in ("qT", "kT", "ktok", "vtok")} for _ in range(2)]
    zt = [cv.t([64, 8, 64], F32) for _ in range(3)]
    sets = []
    for i in range(NSET):
        s_ = {nm: cv.t(H8, F32) for nm in ("Gb", "Gu", "E", "EL", "EU", "u")}
        s_.update({nm: cv.t(H8, BF16) for nm in ("X0", "X1", "Y0", "Y1", "Pm", "vb", "kbe", "kd", "wT", "qkT")})
        sets.append(s_)
    sq_ = []
    for i in range(2):
        s_ = {nm: cv.t(H8, F32) for nm in ("St", "oa", "o", "sq", "on")}
        s_.update({nm: cv.t(H8, BF16) for nm in ("vnew", "onb")})
        s_["ss"] = cv.t([64, 8], F32)
        sq_.append(s_)
    Sst = cv.t(H8, F32)
    Sb = cv.t(H8, BF16)
    oaT_st = [cv.t([128, 4, 512], BF16) for _ in range(2)]

    b_gb = B("g_gball", True)
    b_gcs = {nm: B("g_" + nm) for nm in gcs}
    b_negU = B("g_negU")
    b_grp = [{nm: B("g_grp%d_%s" % (i, nm), True) for nm in grp[i]} for i in range(2)]
    b_zt = [B("g_zt%d" % i, True) for i in range(3)]
    b_sets = [{nm: B("g_s%d_%s" % (i, nm)) for nm in sets[i]} for i in range(NSET)]
    b_sq = [{nm: B("g_q%d_%s" % (i, nm)) for nm in sq_[i]} for i in range(2)]
    b_S = B("g_S")
    b_Sb = B("g_Sb")
    b_oast = [B("g_oast%d" % i) for i in range(2)]

    U64 = k.c32[0:64, C_U64:C_U64 + 64]
    ONES64 = k.c32[0:64, C_ONES64:C_ONES64 + 64]
    SLb = k.c32[0:64, C_SL:C_SL + 64].unsqueeze(1).to_broadcast(H8)
    UIb = k.c32[0:64, C_UI:C_UI + 64].unsqueeze(1).to_broadcast(H8)
    Ib = k.cbf[0:64, C_IDENT:C_IDENT + 64].unsqueeze(1).to_broadcast(H8)
    id64 = k.cbf[0:64, C_IDENT:C_IDENT + 64]
    dngb = k.rows[0:64, R_DNG:R_DNG + 64].unsqueeze(1).to_broadcast(H8)

    def fl(v):
        return v.rearrange("p h d -> p (h d)")

    def bj(v2):
        return v2.unsqueeze(2).to_broadcast(H8)

    for q8 in range(8):
        DMA(k, "sp", [bd["gb_tok"]], [b_gb], out=gb_all[:, q8 * 8:(q8 + 1) * 8, :],
            in_=d["gb_tok"][q8 * 512:(q8 + 1) * 512, :].rearrange("(n c) j -> c n j", c=64))
    O(k, "dve", "tensor_scalar", [k.b_c], [b_negU], out=negU, in0=U64, scalar1=-1.0, scalar2=None, op0=ALU.mult)
    g_all = gb_all[:, :, 0:8]
    beta_all = gb_all[:, :, 8:16]
    ps, b_ps = next_ps(k)
    O(k, "pe", "matmul", [k.b_c, b_gb], [b_ps], out=ps[0:64, :].rearrange("p (n h) -> p n h", h=8), lhsT=U64, rhs=g_all,
      start=True, stop=True)
    O(k, "act", "activation", [b_ps], [b_gcs["gc"]], out=fl(gcs["gc"]), in_=ps[0:64, :], func=AF.Copy)
    ps, b_ps = next_ps(k)
    O(k, "pe", "matmul", [k.b_c, b_gb], [b_ps], out=ps[0:64, :].rearrange("p (n h) -> p n h", h=8), lhsT=ONES64, rhs=g_all,
      start=True, stop=True)
    O(k, "act", "activation", [b_ps], [b_gcs["gl"]], out=fl(gcs["gl"]), in_=ps[0:64, :], func=AF.Copy)
    O(k, "act", "activation", [b_gcs["gc"]], [b_gcs["egc"]], out=fl(gcs["egc"]), in_=fl(gcs["gc"]), func=AF.Exp)
    O(k, "act", "activation", [b_gcs["gl"]], [b_gcs["gtot"]], out=fl(gcs["gtot"]), in_=fl(gcs["gl"]), func=AF.Exp)
    O(k, "dve", "tensor_tensor", [b_gcs["gl"], b_gcs["gc"]], [b_gcs["ekd"]], out=fl(gcs["ekd"]), in0=fl(gcs["gl"]), in1=fl(gcs["gc"]),
      op=ALU.subtract)
    O(k, "act", "activation", [b_gcs["ekd"]], [b_gcs["ekd"]], out=fl(gcs["ekd"]), in_=fl(gcs["ekd"]), func=AF.Exp)
    O(k, "dve", "tensor_tensor", [b_gcs["egc"], b_gb], [b_gcs["beg"]], out=gcs["beg"], in0=gcs["egc"], in1=beta_all, op=ALU.mult)
    O(k, "dve", "memset", [], [b_S], ap=Sst, constant=0.0)
    O(k, "dve", "memset", [], [b_Sb], ap=Sb, constant=0.0)

    def mm8(ps, b_ps, lhs, b_lhs, rhs, b_rhs, lsl=None, rsl=None):
        for h in range(8):
            l_ = lhs[:, h, lsl] if lsl is not None else lhs[:, h, :]
            r_ = rhs[:, h, rsl] if rsl is not None else rhs[:, h, :]
            O(k, "pe", "matmul", [b_lhs, b_rhs], [b_ps], out=ps[0:64, h * 64:(h + 1) * 64], lhsT=l_, rhs=r_, start=True, stop=True)

    def load_group(g):
        gi = g % 2
        tsl = slice(g * 512, (g + 1) * 512)
        DMA(k, "sp", [bd["qaT"]], [b_grp[gi]["qT"]], out=grp[gi]["qT"], in_=d["qaT"].rearrange("(h r) t -> r h t", r=64)[:, :, tsl])
        DMA(k, "sp", [bd["kaT"]], [b_grp[gi]["kT"]], out=grp[gi]["kT"], in_=d["kaT"].rearrange("(h r) t -> r h t", r=64)[:, :, tsl])
        DMA(k, "sp", [bd["ka_tok"]], [b_grp[gi]["ktok"]], out=grp[gi]["ktok"],
            in_=d["ka_tok"][tsl, :].rearrange("(n c) f -> c n f", c=64))
        DMA(k, "sp", [bd["va_tok"]], [b_grp[gi]["vtok"]], out=grp[gi]["vtok"],
            in_=d["va_tok"][tsl, :].rearrange("(n c) f -> c n f", c=64))

    def pre(n):
        g = n // 8
        ci = n % 8
        gi = g % 2
        if ci == 0:
            load_group(g)
        G = grp[gi]
        bG = b_grp[gi]
        cs_ = slice(ci * 64, (ci + 1) * 64)
        s = sets[n % NSET]
        bs = b_sets[n % NSET]
        g_n = gb_all[:, n, 0:8]
        beta_n = gb_all[:, n, 8:16]
        ktok_n = G["ktok"][:, ci, :].rearrange("p (h d) -> p h d", d=64)
        vtok_n = G["vtok"][:, ci, :].rearrange("p (h d) -> p h d", d=64)
        O(k, "dve", "tensor_copy", [b_gb], [bs["Gb"]], out=s["Gb"], in_=bj(g_n))
        O(k, "pool", "tensor_tensor", [b_gb, b_negU], [bs["Gu"]], out=s["Gu"], in0=bj(g_n), in1=negU.unsqueeze(1).to_broadcast(H8),
          op=ALU.mult)
        O(k, "pool", "tensor_tensor", [bG["vtok"], b_gb], [bs["vb"]], out=s["vb"], in0=vtok_n, in1=bj(beta_n), op=ALU.mult)
        O(k, "pool", "tensor_tensor", [bG["ktok"], b_gcs["beg"]], [bs["kbe"]], out=s["kbe"], in0=ktok_n, in1=bj(gcs["beg"][:, n, :]),
          op=ALU.mult)
        O(k, "pool", "tensor_tensor", [bG["ktok"], b_gcs["ekd"]], [bs["kd"]], out=s["kd"], in0=ktok_n, in1=bj(gcs["ekd"][:, n, :]),
          op=ALU.mult)
        yield
        psd, b_psd = next_ps(k, True)
        O(k, "pe", "matmul", [k.b_c, bs["Gb"]], [b_psd], out=psd[0:64, :], lhsT=U64, rhs=fl(s["Gb"]), start=True, stop=False)
        O(k, "pe", "matmul", [k.b_c, bs["Gu"]], [b_psd], out=psd[0:64, :], lhsT=ONES64, rhs=fl(s["Gu"]), start=False, stop=True)
        yield
        O(k, "act", "activation", [b_psd], [bs["E"]], out=fl(s["E"]), in_=psd[0:64, :], func=AF.Abs)
        rel_ps(k, b_psd)
        O(k, "act", "activation", [bs["E"]], [bs["E"]], out=fl(s["E"]), in_=fl(s["E"]), func=AF.Exp, scale=-1.0)
        O(k, "pool", "tensor_tensor", [bs["E"], k.b_c], [bs["EU"]], out=s["EU"], in0=s["E"], in1=UIb, op=ALU.mult)
        O(k, "dve", "tensor_tensor", [bs["E"], k.b_c], [bs["EL"]], out=s["EL"], in0=s["E"], in1=SLb, op=ALU.mult)
        O(k, "dve", "tensor_tensor", [bs["EL"], b_gb], [bs["EL"]], out=s["EL"], in0=s["EL"], in1=bj(beta_n), op=ALU.mult)
        pkk, b_pkk = next_ps(k, True)
        mm8(pkk, b_pkk, G["kT"], bG["kT"], G["kT"], bG["kT"], cs_, cs_)
        pqk, b_pqk = next_ps(k, True)
        mm8(pqk, b_pqk, G["kT"], bG["kT"], G["qT"], bG["qT"], cs_, cs_)
        yield
        O(k, "dve", "scalar_tensor_tensor", [b_pkk, bs["EL"]], [bs["X0"]], out=fl(s["X0"]), in0=pkk[0:64, :], scalar=-1.0, in1=fl(s["EL"]),
          op0=ALU.mult, op1=ALU.mult)
        rel_ps(k, b_pkk)
        O(k, "dve", "tensor_tensor", [b_pqk, bs["EU"]], [bs["qkT"]], out=fl(s["qkT"]), in0=pqk[0:64, :], in1=fl(s["EU"]), op=ALU.mult)
        rel_ps(k, b_pqk)
        yield
        pt, b_pt = next_ps(k, True)
        ptb = pt[0:64, :].bitcast(BF16)
        for h in range(8):
            O(k, "pe", "transpose", [bs["X0"], k.b_cbf], [b_pt], out=ptb[:, h * 64:(h + 1) * 64], in_=s["X0"][:, h, :], identity=id64)
        yield
        v6 = "ab"
        if "a" in v6:
            O(k, "act", "activation", [b_pt], [bs["Y0"]], out=fl(s["Y0"]), in_=ptb[:, 0:512], func=AF.Copy)
        if "b" in v6:
            O(k, "dve", "tensor_tensor", [b_pt, k.b_cbf], [bs["Pm"]], out=s["Pm"], in0=ptb[:, 0:512].rearrange("p (h d) -> p h d", d=64), in1=Ib,
              op=ALU.add)
        if "c" in v6:
            O(k, "dve", "tensor_tensor", [bs["Y0"], k.b_cbf], [bs["Pm"]], out=s["Pm"], in0=s["Y0"], in1=Ib, op=ALU.add)
        rel_ps(k, b_pt)
        yield
        X, Y = "X0", "Y0"
        for lv in range(5):
            Xn = "X1" if X == "X0" else "X0"
            Yn = "Y1" if Y == "Y0" else "Y0"
            px, b_px = next_ps(k, True)
            mm8(px, b_px, s[Y], bs[Y], s[X], bs[X])
            if lv < 4:
                py, b_py = next_ps(k, True)
                mm8(py, b_py, s[X], bs[X], s[Y], bs[Y])
            yield
            O(k, "act", "activation", [b_px], [bs[Xn]], out=fl(s[Xn]), in_=px[0:64, :], func=AF.Copy)
            rel_ps(k, b_px)
            if lv < 4:
                O(k, "dve", "tensor_copy", [b_py], [bs[Yn]], out=fl(s[Yn]), in_=py[0:64, :])
                rel_ps(k, b_py)
            yield
            pp, b_pp = next_ps(k, True)
            mm8(pp, b_pp, s[Xn], bs[Xn], s["Pm"], bs["Pm"])
            yield
            O(k, "dve", "tensor_tensor", [b_pp, bs["Pm"]], [bs["Pm"]], out=fl(s["Pm"]), in0=pp[0:64, :], in1=fl(s["Pm"]), op=ALU.add)
            rel_ps(k, b_pp)
            yield
            X, Y = Xn, Yn
        pu, b_pu = next_ps(k, True)
        mm8(pu, b_pu, s["Pm"], bs["Pm"], s["vb"], bs["vb"])
        pw, b_pw = next_ps(k, True)
        mm8(pw, b_pw, s["kbe"], bs["kbe"], s["Pm"], bs["Pm"])
        yield
        O(k, "act", "activation", [b_pu], [bs["u"]], out=fl(s["u"]), in_=pu[0:64, :], func=AF.Copy)
        rel_ps(k, b_pu)
        O(k, "act", "activation", [b_pw], [bs["wT"]], out=fl(s["wT"]), in_=pw[0:64, :], func=AF.Copy)
        rel_ps(k, b_pw)
        yield

    def seq(n):
        g = n // 8
        ci = n % 8
        gi = g % 2
        G = grp[gi]
        bG = b_grp[gi]
        cs_ = slice(ci * 64, (ci + 1) * 64)
        s = sets[n % NSET]
        bs = b_sets[n % NSET]
        q = sq_[n % 2]
        bq = b_sq[n % 2]
        z = zt[n % 3]
        bz = b_zt[n % 3]
        DMA(k, "sp", [bd["z_tok"]], [bz], out=fl(z), in_=d["z_tok"][n * 64:(n + 1) * 64, :])
        pws, b_pws = next_ps(k, True)
        mm8(pws, b_pws, s["wT"], bs["wT"], Sb, b_Sb)
        po1, b_po1 = next_ps(k, True)
        mm8(po1, b_po1, G["qT"], bG["qT"], Sb, b_Sb, cs_, None)
        O(k, "pool", "tensor_tensor", [b_S, b_gcs["gtot"]], [bq["St"]], out=q["St"], in0=Sst, in1=bj(gcs["gtot"][:, n, :]), op=ALU.mult)
        yield
        O(k, "dve", "tensor_tensor", [bs["u"], b_pws], [bq["vnew"]], out=fl(q["vnew"]), in0=fl(s["u"]), in1=pws[0:64, :], op=ALU.subtract)
        rel_ps(k, b_pws)
        O(k, "dve", "tensor_tensor", [b_po1, b_gcs["egc"]], [bq["oa"]], out=q["oa"], in0=po1[0:64, :].rearrange("p (h d) -> p h d", d=64),
          in1=bj(gcs["egc"][:, n, :]), op=ALU.mult)
        rel_ps(k, b_po1)
        yield
        pkv, b_pkv = next_ps(k, True)
        mm8(pkv, b_pkv, s["kd"], bs["kd"], q["vnew"], bq["vnew"])
        po2, b_po2 = next_ps(k, True)
        mm8(po2, b_po2, s["qkT"], bs["qkT"], q["vnew"], bq["vnew"])
        yield
        O(k, "dve", "tensor_tensor", [bq["St"], b_pkv], [b_S], out=fl(Sst), in0=fl(q["St"]), in1=pkv[0:64, :], op=ALU.add)
        rel_ps(k, b_pkv)
        O(k, "act", "activation", [b_S], [b_Sb], out=fl(Sb), in_=fl(Sst), func=AF.Copy)
        O(k, "dve", "tensor_tensor", [bq["oa"], b_po2], [bq["o"]], out=fl(q["o"]), in0=fl(q["oa"]), in1=po2[0:64, :], op=ALU.add)
        rel_ps(k, b_po2)
        yield
        O(k, "pool", "tensor_tensor", [bq["o"]], [bq["sq"]], out=q["sq"], in0=q["o"], in1=q["o"], op=ALU.mult)
        O(k, "dve", "tensor_reduce", [bq["sq"]], [bq["ss"]], out=q["ss"], in_=q["sq"], axis=AX.X, op=ALU.add)
        O(k, "act", "activation", [bq["ss"], k.b_eps], [bq["ss"]], out=q["ss"], in_=q["ss"], func=AF.Sqrt, bias=k.epsb[0:64, :], scale=1.0 / 64.0)
        O(k, "dve", "reciprocal", [bq["ss"]], [bq["ss"]], out=q["ss"], in_=q["ss"])
        yield
        O(k, "dve", "tensor_tensor", [bq["o"], bq["ss"]], [bq["on"]], out=q["on"], in0=q["o"], in1=bj(q["ss"]), op=ALU.mult)
        O(k, "pool", "tensor_tensor", [bq["on"], k.b_rows], [bq["on"]], out=q["on"], in0=q["on"], in1=dngb, op=ALU.mult)
        O(k, "pool", "tensor_tensor", [bq["on"], bz], [bq["onb"]], out=q["onb"], in0=q["on"], in1=z, op=ALU.mult)
        yield
        pt, b_pt = next_ps(k, True)
        ptb = pt[:, :].bitcast(BF16)
        onf = fl(q["onb"])
        for c4 in range(4):
            O(k, "pe", "transpose", [bq["onb"], k.b_cbf], [b_pt], out=ptb[:, c4 * 64:(c4 + 1) * 64], in_=onf[:, c4 * 128:(c4 + 1) * 128],
              identity=id64)
        yield
        O(k, "act", "activation", [b_pt], [b_oast[gi]], out=oaT_st[gi][:, :, cs_], in_=ptb[:, 0:256].rearrange("p (c t) -> p c t", t=64),
          func=AF.Copy)
        rel_ps(k, b_pt)
        if ci == 7:
            DMA(k, "sp", [b_oast[gi]], [bd["oaT"]], out=d["oaT"][:, g * 512:(g + 1) * 512].rearrange("(c p) t -> p c t", p=128),
                in_=oaT_st[gi])
        yield

    if k.gdn_stages is not None:
        def pre_lim(n):
            for i, _ in enumerate(pre(n)):
                if i + 1 >= k.gdn_stages:
                    k.ps_held.clear()
                    return
                yield

        def seq_none(n):
            return
            yield
        pipeline(k.gdn_n, pre_lim, seq_none, NSET)
    else:
        pipeline(k.gdn_n, pre, seq, NSET)


NIT = 20
TOPK = 256


def dsa_pass(k):
    P = k.P
    B = P.buf
    d = k.d
    bd = k.bd
    cv = Carver(k.big, k.PH0, k.PHLIM)
    kiT = cv.t([64, S], BF16)
    kbT = cv.t([65, 8, S], BF16)
    vb = cv.t([128, 32, 520], BF16)
    cbias = cv.t([128, 8], F32)
    km8 = cv.t([8, 1], F32)
    dg = cv.t([8, 8], F32)
    sc = [cv.t([128, S], F32) for _ in range(2)]
    m01 = cv.t([128, S], BF16)
    junk = m01
    junk2 = cv.t([128, S], BF16)
    obu = [cv.t([128, 8, 65], F32) for _ in range(2)]
    rc8 = cv.t([128, 8, 1], F32)
    maskT = [cv.t([128, 32, 128], BF16) for _ in range(2)]
    qiq = [cv.t([64, 8, 128], BF16) for _ in range(2)]
    qbq = [cv.t([65, 8, 128], BF16) for _ in range(2)]
    wiq = [cv.t([128, 8], F32) for _ in range(2)]
    rl = [cv.t([128, 512], F32) for _ in range(2)]
    ex = [cv.t([128, 512], BF16) for _ in range(3)]
    pm = [cv.t([128, 512], BF16) for _ in range(3)]
    bis = [{nm: cv.t([128, 1], F32) for nm in ("hi", "lo", "rng", "c1", "s2", "s3", "b1", "nb2", "nb")} for _ in range(2)]
    t3v = [cv.t([128, 3], F32) for _ in range(2)]
    b_lo = [B("a_lo%d" % i) for i in range(2)]
    b_t3 = [B("a_t3%d" % i) for i in range(2)]
    b_s2 = [B("a_s2%d" % i) for i in range(2)]
    b_s3 = [B("a_s3%d" % i) for i in range(2)]
    Wt = [cv.t([128, 32], F32) for _ in range(2)]
    ob = [cv.t([128, 512], BF16) for _ in range(2)]
    rc = [cv.t([128, 1], F32) for _ in range(2)]
    obst = [cv.t([128, 4, 128], BF16) for _ in range(2)]

    b_kiT = B("a_kiT", True)
    b_kbT = [B("a_kbT%d" % h, True) for h in range(8)]
    b_vb = B("a_vb", True)
    b_cbias = B("a_cbias")
    b_km8 = B("a_km8")
    b_dg = B("a_dg")
    b_sc = [B("a_sc%d" % i) for i in range(2)]
    b_m01 = B("a_m01")
    b_junk = b_m01
    b_junk2 = B("a_junk2")
    b_obu = [B("a_obu%d" % i) for i in range(2)]
    b_rc8 = B("a_rc8")
    b_mid = [B("a_mid%d" % i) for i in range(2)]
    b_ssg = [B("a_ssg%d" % i) for i in range(2)]
    b_maskT = [B("a_maskT%d" % i) for i in range(2)]
    b_qiq = [B("a_qiq%d" % i, True) for i in range(2)]
    b_qbq = [B("a_qbq%d" % i, True) for i in range(2)]
    b_wiq = [B("a_wiq%d" % i, True) for i in range(2)]
    b_rl = [B("a_rl%d" % i) for i in range(2)]
    b_ex = [B("a_ex%d" % i) for i in range(3)]
    b_pm = [B("a_pm%d" % i) for i in range(3)]
    b_bis = [B("a_bis%d" % i) for i in range(2)]
    b_W = [B("a_W%d" % i) for i in range(2)]
    b_ob = [B("a_ob%d" % i) for i in range(2)]
    b_rc = [B("a_rc%d" % i) for i in range(2)]
    b_obst = [B("a_obst%d" % i) for i in range(2)]

    ident_bf = k.cbf[:, C_IDENT:C_IDENT + 128]
    ident_f = k.c32[:, C_IDENT:C_IDENT + 128]

    DMA(k, "sp", [bd["kiT"]], [b_kiT], out=kiT, in_=d["kiT"])
    for h in range(8):
        DMA(k, "sp" if h % 2 == 0 else "act", [bd["kbT"]], [b_kbT[h]], out=kbT[:, h, :], in_=d["kbT"][h])
    for q4 in range(4):
        DMA(k, "sp", [bd["vb_tok"]], [b_vb], out=vb[:, q4 * 8:(q4 + 1) * 8, :],
            in_=d["vb_tok"][q4 * 1024:(q4 + 1) * 1024, :].rearrange("(kt p) f -> p kt f", p=128))
    ps, b_ps = next_ps(k)
    O(k, "pe", "transpose", [k.b_kmax, k.b_c], [b_ps], out=ps[0:8, 0:128], in_=k.kmax, identity=ident_f)
    O(k, "dve", "tensor_reduce", [b_ps], [b_km8], out=km8, in_=ps[0:8, 0:128], axis=AX.X, op=ALU.max)
    O(k, "dve", "tensor_scalar", [k.b_c, b_km8], [b_dg], out=dg, in0=k.c32[0:8, C_IDENT:C_IDENT + 8], scalar1=km8[:, 0:1], scalar2=None,
      op0=ALU.mult)
    ps, b_ps = next_ps(k)
    O(k, "pe", "matmul", [k.b_c, b_dg], [b_ps], out=ps[:, 0:8], lhsT=k.c32[0:8, C_ONESD:C_ONESD + 128], rhs=dg, start=True, stop=True)
    O(k, "dve", "tensor_scalar", [b_ps], [b_cbias], out=cbias, in0=ps[:, 0:8], scalar1=-64.0, scalar2=None, op0=ALU.mult)

    k.ps_pool = [0, 1, 2, 3, 4, 5]
    k.ps_i = 0
    NQB = S // 128

    def idx(qb):
        par = qb % 2
        t0 = qb * 128
        nk = qb + 1
        N = nk * 128
        scp = sc[par]
        bsc = b_sc[par]
        bb = bis[par]
        bbis = b_bis[par]
        W = Wt[par]
        DMA(k, "sp", [bd["qiT"]], [b_qiq[par]], out=qiq[par], in_=d["qiT"][:, :, t0:t0 + 128].rearrange("h r t -> r h t"))
        DMA(k, "sp", [bd["wi_tok"]], [b_wiq[par]], out=wiq[par], in_=d["wi_tok"][t0:t0 + 128, :])
        for kg in range((N + 511) // 512):
            n = min(512, N - kg * 512)
            ksl = slice(kg * 512, kg * 512 + n)
            for h in range(8):
                ps, b_ps = next_ps(k)
                O(k, "pe", "matmul", [b_qiq[par], b_kiT], [b_ps], out=ps[:, 0:n], lhsT=qiq[par][:, h, :], rhs=kiT[:, ksl], start=True, stop=True)
                r = rl[h % 2]
                br = b_rl[h % 2]
                O(k, "act", "activation", [b_ps], [br], out=r[:, 0:n], in_=ps[:, 0:n], func=AF.Relu)
                if h == 0:
                    O(k, "dve", "tensor_scalar", [br, b_wiq[par]], [bsc], out=scp[:, ksl], in0=r[:, 0:n], scalar1=wiq[par][:, 0:1], scalar2=None,
                      op0=ALU.mult)
                else:
                    O(k, "dve", "scalar_tensor_tensor", [br, b_wiq[par], bsc], [bsc], out=scp[:, ksl], in0=r[:, 0:n], scalar=wiq[par][:, h:h + 1],
                      in1=scp[:, ksl], op0=ALU.mult, op1=ALU.add)
            yield
        dsl = slice(qb * 128, (qb + 1) * 128)
        O(k, "dve", "tensor_tensor", [bsc, k.b_c], [bsc], out=scp[:, dsl], in0=scp[:, dsl], in1=k.c32[:, C_NEGC:C_NEGC + 128], op=ALU.add)
        yield

    def idxB(qb):
        par = qb % 2
        t0 = qb * 128
        nk = qb + 1
        N = nk * 128
        scp = sc[par]
        bsc = b_sc[par]
        bb = bis[par]
        bbis = b_bis[par]
        W = Wt[par]
        DMA(k, "sp", [bd["qbT"]], [b_qbq[par]], out=qbq[par], in_=d["qbT"][:, :, t0:t0 + 128].rearrange("h r t -> r h t"))
        if qb >= 2:
            O(k, "dve", "tensor_reduce", [bsc], [bbis], out=bb["hi"], in_=scp[:, 0:N], axis=AX.X, op=ALU.max)
            O(k, "dve", "tensor_reduce", [bsc], [b_lo[par]], out=bb["lo"], in_=scp[:, 0:qb * 128], axis=AX.X, op=ALU.min)
            O(k, "dve", "tensor_tensor", [bbis, b_lo[par]], [bbis], out=bb["rng"], in0=bb["hi"], in1=bb["lo"], op=ALU.subtract)
            O(k, "dve", "tensor_scalar", [k.b_c, bbis], [b_W[par]], out=W[:, 0:16], in0=k.c32[:, C_POW4:C_POW4 + 16], scalar1=bb["rng"][:, 0:1],
              scalar2=None, op0=ALU.mult)
            yield
            cK = float(N - 2 * TOPK)
            def emit_t3(i):
                O(k, "dve", "scalar_tensor_tensor", [k.b_c, b_W[par], b_lo[par]], [b_t3[par]], out=t3v[par], in0=k.c32[:, C_123:C_123 + 3],
                  scalar=W[:, i:i + 1], in1=bb["lo"].to_broadcast([128, 3]), op0=ALU.mult, op1=ALU.add)

            emit_t3(0)
            yield
            for i in range(NIT // 2):
                O(k, "dve", "tensor_scalar", [bsc, b_t3[par]], [b_junk, bbis], out=junk[:, 0:N], in0=scp[:, 0:N], scalar1=t3v[par][:, 0:1],
                  scalar2=None, op0=ALU.is_ge, op1=ALU.add, accum_out=bb["c1"])
                O(k, "act", "activation", [bsc, b_t3[par]], [b_s2[par]], out=junk2[:, 0:N], in_=scp[:, 0:N], func=AF.Sign,
                  bias=t3v[par][:, 1:2], scale=-1.0, accum_out=bb["s2"])
                O(k, "act", "activation", [bsc, b_t3[par]], [b_s3[par]], out=junk2[:, 0:N], in_=scp[:, 0:N], func=AF.Sign,
                  bias=t3v[par][:, 2:3], scale=-1.0, accum_out=bb["s3"])
                yield
                O(k, "dve", "tensor_scalar", [bbis], [bbis], out=bb["b1"], in0=bb["c1"], scalar1=float(TOPK), scalar2=None, op0=ALU.is_ge)
                O(k, "dve", "scalar_tensor_tensor", [b_s2[par], bbis], [bbis], out=bb["nb2"], in0=bb["s2"], scalar=cK, in1=bb["b1"],
                  op0=ALU.is_le, op1=ALU.add)
                O(k, "dve", "scalar_tensor_tensor", [b_s3[par], bbis], [bbis], out=bb["nb"], in0=bb["s3"], scalar=cK, in1=bb["nb2"],
                  op0=ALU.is_le, op1=ALU.add)
                O(k, "dve", "scalar_tensor_tensor", [bbis, b_W[par], b_lo[par]], [b_lo[par]], out=bb["lo"], in0=bb["nb"], scalar=W[:, i:i + 1],
                  in1=bb["lo"], op0=ALU.mult, op1=ALU.add)
                if i + 1 < NIT // 2:
                    emit_t3(i + 1)
                yield
            O(k, "dve", "tensor_scalar", [bsc, b_lo[par]], [b_m01], out=m01[:, 0:N], in0=scp[:, 0:N], scalar1=bb["lo"][:, 0:1], scalar2=None,
              op0=ALU.is_ge)
        else:
            O(k, "dve", "tensor_scalar", [bsc], [b_m01], out=m01[:, 0:N], in0=scp[:, 0:N], scalar1=-1e29, scalar2=None, op0=ALU.is_ge)
        yield
        for k8 in range((nk + 7) // 8):
            kts = list(range(k8 * 8, min(nk, k8 * 8 + 8)))
            ps, b_ps = next_ps(k)
            psb = ps[:, :].bitcast(BF16)
            for j, kt in enumerate(kts):
                O(k, "pe", "transpose", [b_m01, k.b_cbf], [b_ps], out=psb[:, j * 128:(j + 1) * 128], in_=m01[:, kt * 128:(kt + 1) * 128],
                  identity=ident_bf)
            O(k, "act", "activation", [b_ps], [b_maskT[par]], out=maskT[par][:, kts[0]:kts[-1] + 1, :],
              in_=psb[:, 0:len(kts) * 128].rearrange("p (a t) -> p a t", t=128), func=AF.Copy)
            yield

    ctr = [0]

    def att(qb):
        par = qb % 2
        t0 = qb * 128
        nk = qb + 1
        ngr = (nk + 3) // 4
        groups = [(h, kg) for h in range(8) for kg in range(ngr)]

        def emit_qk(h, kg):
            kts = list(range(kg * 4, min(nk, kg * 4 + 4)))
            ps, b_ps = next_ps(k, True)
            for j, kt in enumerate(kts):
                O(k, "pe", "matmul", [b_kbT[h], b_qbq[par]], [b_ps], out=ps[:, j * 128:(j + 1) * 128], lhsT=kbT[:, h, kt * 128:(kt + 1) * 128],
                  rhs=qbq[par][:, h, :], start=True, stop=True)
            return ps, b_ps, kts

        cur = emit_qk(*groups[0])
        for gi, (h, kg) in enumerate(groups):
            nxt = emit_qk(*groups[gi + 1]) if gi + 1 < len(groups) else None
            ps, b_ps, kts = cur
            n = len(kts) * 128
            po = k.ps[6 + h % 2]
            b_po = k.b_ps[6 + h % 2]
            i3 = ctr[0] % 3
            ctr[0] += 1
            O(k, "act", "activation", [b_ps, b_cbias], [b_ex[i3]], out=ex[i3][:, 0:n], in_=ps[:, 0:n], func=AF.Exp, bias=cbias[:, h:h + 1],
              scale=1.0)
            rel_ps(k, b_ps)
            O(k, "pool", "tensor_tensor", [b_ex[i3], b_maskT[par]], [b_pm[i3]], out=pm[i3][:, 0:n], in0=ex[i3][:, 0:n],
              in1=maskT[par][:, kts[0]:kts[-1] + 1, :].rearrange("p a t -> p (a t)"), op=ALU.mult)
            for j, kt in enumerate(kts):
                O(k, "pe", "matmul", [b_pm[i3], b_vb], [b_po], out=po[:, 0:65], lhsT=pm[i3][:, j * 128:(j + 1) * 128],
                  rhs=vb[:, kt, h * 65:(h + 1) * 65], start=(kt == 0), stop=(kt == nk - 1))
            if kg == ngr - 1:
                O(k, "act", "activation", [b_po], [b_obu[par]], out=obu[par][:, h, :], in_=po[:, 0:65], func=AF.Copy)
            cur = nxt
            yield
        O(k, "dve", "reciprocal", [b_obu[par]], [b_rc8], out=rc8, in_=obu[par][:, :, 64:65])
        O(k, "dve", "tensor_tensor", [b_obu[par], b_rc8], [b_ob[par]], out=ob[par].rearrange("p (h d) -> p h d", d=64), in0=obu[par][:, :, 0:64],
          in1=rc8.to_broadcast([128, 8, 64]), op=ALU.mult)
        ps, b_ps = next_ps(k)
        psb = ps[:, :].bitcast(BF16)
        for c4 in range(4):
            O(k, "pe", "transpose", [b_ob[par], k.b_cbf], [b_ps], out=psb[:, c4 * 128:(c4 + 1) * 128], in_=ob[par][:, c4 * 128:(c4 + 1) * 128],
              identity=ident_bf)
        O(k, "act", "activation", [b_ps], [b_obst[par]], out=obst[par], in_=psb[:, 0:512].rearrange("p (c t) -> p c t", t=128), func=AF.Copy)
        DMA(k, "sp", [b_obst[par]], [bd["obT"]], out=d["obT"][:, t0:t0 + 128].rearrange("(c p) t -> p c t", p=128), in_=obst[par])
        yield

    pipeline3(NQB, idx, idxB, att, 2)
    k.ps_pool = list(range(8))
    k.ps_i = 0


def merge_pass(k):
    P = k.P
    B = P.buf
    d = k.d
    bd = k.bd
    T = 512
    cv = Carver(k.big, k.PH0, k.PHLIM)
    w2 = cv.t([128, 8, 2048], BF16)
    wa = cv.t([128, 4, D], BF16)
    wb = cv.t([128, 4, D], BF16)
    wo = cv.t([128, 8, D], BF16)
    hT = cv.t([128, 8, T], F32)
    su = cv.t([128, 8, T], BF16)
    rstd = cv.t([128, T], F32)
    oa = cv.t([128, 4, T], BF16)
    obt = cv.t([128, 4, T], BF16)
    sga = [cv.t([128, T], F32) for _ in range(2)]
    sgb = [cv.t([128, T], F32) for _ in range(2)]
    mg = cv.t([128, 8, T], BF16)
    y = cv.t([128, 8, T], F32)
    b_w2 = [B("m_w2_%d" % c, True) for c in range(8)]
    b_wa = B("m_wa", True)
    b_wb = B("m_wb", True)
    b_wo = B("m_wo", True)
    b_h = B("m_h", True)
    b_su = B("m_su")
    b_rstd = B("m_rstd")
    b_oa = B("m_oa", True)
    b_ob = B("m_ob", True)
    b_sga = [B("m_sga%d" % i) for i in range(2)]
    b_sgb = [B("m_sgb%d" % i) for i in range(2)]
    b_mg = [B("m_mg%d" % i) for i in range(8)]
    b_y = B("m_y")
    g_pre = k.gsb[:, 16:24]
    g_post = k.gsb[:, 24:32]
    for c in range(8):
        DMA(k, "pool", [], [b_w2[c]], out=w2[:, c, :], in_=k.mix_w2[:, c, :])
    DMA(k, "pool", [], [b_wa], out=wa, in_=k.w_br_a)
    DMA(k, "pool", [], [b_wb], out=wb, in_=k.w_br_b)
    DMA(k, "pool", [], [b_wo], out=wo, in_=k.mix_w_out)
    for it in range(S // T):
        t0 = it * T
        DMA(k, "sp", [], [b_h], out=hT, in_=k.h1T[:, t0:t0 + T].rearrange("(c p) t -> p c t", p=128))
        DMA(k, "sp", [bd["oaT"]], [b_oa], out=oa, in_=d["oaT"][:, t0:t0 + T].rearrange("(c p) t -> p c t", p=128))
        DMA(k, "sp", [bd["obT"]], [b_ob], out=obt, in_=d["obT"][:, t0:t0 + T].rearrange("(c p) t -> p c t", p=128))
        for c in range(8):
            O(k, "act", "activation", [b_h], [b_su], out=su[:, c, :], in_=hT[:, c, :], func=AF.Square)
        rms_stats(k, su, b_su, rstd, b_rstd, T)
        for c in range(8):
            O(k, "dve", "scalar_tensor_tensor", [b_h, k.b_g, b_rstd], [b_su], out=su[:, c, :], in0=hT[:, c, :], scalar=g_pre[:, c:c + 1],
              in1=rstd, op0=ALU.mult, op1=ALU.mult)
        for dc in range(8):
            j = dc % 2
            dsl = slice(dc * 128, (dc + 1) * 128)
            pga, b_pga = next_ps(k)
            for c in range(8):
                O(k, "pe", "matmul", [b_w2[c], b_su], [b_pga], out=pga[:, 0:T], lhsT=w2[:, c, dc * 128:(dc + 1) * 128], rhs=su[:, c, :],
                  start=(c == 0), stop=(c == 7))
            pgb, b_pgb = next_ps(k)
            for c in range(8):
                O(k, "pe", "matmul", [b_w2[c], b_su], [b_pgb], out=pgb[:, 0:T], lhsT=w2[:, c, D + dc * 128:D + (dc + 1) * 128], rhs=su[:, c, :],
                  start=(c == 0), stop=(c == 7))
            pya, b_pya = next_ps(k)
            for c in range(4):
                O(k, "pe", "matmul", [b_wa, b_oa], [b_pya], out=pya[:, 0:T], lhsT=wa[:, c, dsl], rhs=oa[:, c, :], start=(c == 0), stop=(c == 3))
            pyb, b_pyb = next_ps(k)
            for c in range(4):
                O(k, "pe", "matmul", [b_wb, b_ob], [b_pyb], out=pyb[:, 0:T], lhsT=wb[:, c, dsl], rhs=obt[:, c, :], start=(c == 0), stop=(c == 3))
            O(k, "act", "activation", [b_pga], [b_sga[j]], out=sga[j], in_=pga[:, 0:T], func=AF.Sigmoid)
            O(k, "act", "activation", [b_pgb], [b_sgb[j]], out=sgb[j], in_=pgb[:, 0:T], func=AF.Sigmoid)
            O(k, "dve", "tensor_tensor", [b_sga[j], b_pya], [b_sga[j]], out=sga[j], in0=sga[j], in1=pya[:, 0:T], op=ALU.mult)
            O(k, "dve", "tensor_tensor", [b_sgb[j], b_pyb], [b_sgb[j]], out=sgb[j], in0=sgb[j], in1=pyb[:, 0:T], op=ALU.mult)
            O(k, "pool", "tensor_tensor", [b_sga[j], b_sgb[j]], [b_mg[dc]], out=mg[:, dc, :], in0=sga[j], in1=sgb[j], op=ALU.add)
        for dc in range(8):
            psy, b_psy = next_ps(k)
            for c in range(8):
                O(k, "pe", "matmul", [b_wo, b_mg[c]], [b_psy], out=psy[:, 0:T], lhsT=wo[:, c, dc * 128:(dc + 1) * 128], rhs=mg[:, c, :],
                  start=(c == 0), stop=(c == 7))
            O(k, "act", "activation", [b_psy], [b_y], out=y[:, dc, :], in_=psy[:, 0:T], func=AF.Copy)
            O(k, "act", "activation", [b_psy], [b_su], out=su[:, dc, :], in_=psy[:, 0:T], func=AF.Square)
        rms_stats(k, su, b_su, rstd, b_rstd, T)
        for c in range(8):
            O(k, "dve", "scalar_tensor_tensor", [b_y, k.b_g, b_rstd], [b_y], out=y[:, c, :], in0=y[:, c, :], scalar=g_post[:, c:c + 1],
              in1=rstd, op0=ALU.mult, op1=ALU.mult)
        for c in range(8):
            O(k, "pool", "tensor_tensor", [b_h, b_y], [b_h], out=hT[:, c, :], in0=hT[:, c, :], in1=y[:, c, :], op=ALU.add)
        DMA(k, "sp", [b_h], [bd["h2T"]], out=d["h2T"][:, t0:t0 + T].rearrange("(c p) t -> p c t", p=128), in_=hT)
```

```python
import os
import numpy as np
import concourse.bass as bass
import concourse.mybir as mybir
from concourse.bass_utils import run_bass_kernel_spmd
from contextlib import ExitStack

F32 = mybir.dt.float32
BF16 = mybir.dt.bfloat16
U8 = mybir.dt.uint8
ALU = mybir.AluOpType
AF = mybir.ActivationFunctionType
AX = mybir.AxisListType

ENGS = ("pe", "act", "dve", "pool", "sp")

S = 4096
D = 1024
DFF = 2816
NFC = DFF // 128
EPS = 1e-6
TF = 512
TM = 512


class Buf:
    __slots__ = ("name", "w", "r", "dkey", "excl")

    def __init__(self, name, dkey=None):
        self.name = name
        self.w = {}
        self.r = {}
        self.dkey = dkey
        self.excl = False


class Prog:
    def __init__(self, nc):
        self.nc = nc
        self.streams = {e: [] for e in ENGS}
        self.cnt = {e: 0 for e in ENGS}
        self.seen = {e: {} for e in ENGS}
        self.dma_keys = {}
        self.bufs = []
        self.pass_idx = 0
        self.persistent = True

    def buf(self, name, dma_dst=False):
        dkey = None
        if dma_dst:
            if self.persistent:
                dkey = ("d", name)
            else:
                dkey = ("p", self.pass_idx)
                self.pass_idx += 1
            self.dma_keys.setdefault(dkey, 0)
        b = Buf(name, dkey)
        self.bufs.append(b)
        return b

    def _emit(self, eng, fn, reads, writes, tok_key, tok_inc):
        waits = {}
        for b in reads:
            for k, v in b.w.items():
                if waits.get(k, 0) < v:
                    waits[k] = v
            if b.excl:
                for k, v in b.r.items():
                    if k != tok_key and waits.get(k, 0) < v:
                        waits[k] = v
        for b in writes:
            for k, v in b.w.items():
                if waits.get(k, 0) < v:
                    waits[k] = v
            for k, v in b.r.items():
                if waits.get(k, 0) < v:
                    waits[k] = v
        seen = self.seen[eng]
        wl = []
        for k, v in waits.items():
            if k == tok_key and (eng == "pe" or isinstance(k, tuple)):
                continue
            if seen.get(k, 0) >= v:
                continue
            seen[k] = v
            wl.append((k, v))
        if isinstance(tok_key, tuple):
            self.dma_keys[tok_key] += tok_inc
            val = self.dma_keys[tok_key]
        else:
            self.cnt[eng] += 1
            val = self.cnt[eng]
        for b in reads:
            if b.r.get(tok_key, 0) < val:
                b.r[tok_key] = val
        for b in writes:
            b.w = {tok_key: val}
            b.r = {}
        self.streams[eng].append((wl, fn, tok_key, tok_inc))

    def op(self, eng, fn, reads=(), writes=()):
        self._emit(eng, fn, list(reads), list(writes), eng, 1)

    def dma(self, eng, fn, reads, writes):
        dst = writes[0]
        assert dst.dkey is not None, dst.name
        self._emit(eng, fn, list(reads), list(writes), dst.dkey, 16)

    def barrier(self):
        allw = {e: self.cnt[e] for e in ENGS if self.cnt[e] > 0}
        for k, v in self.dma_keys.items():
            if v > 0:
                allw[k] = v
        for eng in ENGS:
            seen = self.seen[eng]
            wl = []
            for k, v in allw.items():
                if k == eng:
                    continue
                if seen.get(k, 0) >= v:
                    continue
                seen[k] = v
                wl.append((k, v))
            if wl:
                self.streams[eng].append((wl, None, None, 0))
        for b in self.bufs:
            b.w = {}
            b.r = {}
        self.pass_idx = 0

    def replay(self):
        nc = self.nc
        with ExitStack() as st:
            sems = {}
            for e in ENGS:
                if self.cnt[e] > 0:
                    sems[e] = st.enter_context(nc.semaphore("s_" + e))
            ndma = 0
            for i, k in enumerate(self.dma_keys):
                if self.dma_keys[k] > 0:
                    sems[k] = st.enter_context(nc.semaphore("sd%d" % i))
                    ndma += 1
            self.n_dma_sems = ndma
            block = st.enter_context(nc.Block())
            streams = self.streams

            def run(name, eng):
                for wl, fn, tk, inc in streams[name]:
                    for k, v in wl:
                        eng.wait_ge(sems[k], v)
                    if fn is not None:
                        fn(eng).then_inc(sems[tk], inc)

            @block.tensor
            def _(e):
                run("pe", e)

            @block.scalar
            def _(e):
                run("act", e)

            @block.vector
            def _(e):
                run("dve", e)

            @block.gpsimd
            def _(e):
                run("pool", e)

            @block.sync
            def _(e):
                run("sp", e)


class Carver:
    def __init__(self, big, start, limit):
        self.big = big
        self.off = start
        self.limit = limit

    def t(self, shape, dt, parts=128):
        esz = 4 if dt == F32 else 2
        n = int(np.prod(shape[1:])) * esz
        off = (self.off + 31) // 32 * 32
        assert off + n <= self.limit, ("SBUF overflow", off + n, self.limit)
        v = self.big[0:shape[0], off:off + n].bitcast(dt)
        if len(shape) == 3:
            v = v.rearrange("p (a b) -> p a b", b=shape[2])
        elif len(shape) == 4:
            v = v.rearrange("p (a b c) -> p a b c", b=shape[2], c=shape[3])
        self.off = off + n
        return v


C_IDENT = 0
C_ONESD = 128
C_BLK64 = 256
C_U64 = 384
C_ONES64 = 448
C_SL = 512
C_UI = 576
C_CAUS = 640
C_NEGC = 768
C_POW2 = 896
C_123 = 928
C_POW4 = 932
C_N = 948


def host_consts():
    c = np.zeros((128, C_N), np.float32)
    c[:, C_IDENT:C_IDENT + 128] = np.eye(128)
    c[:, C_ONESD:C_ONESD + 128] = 1.0 / D
    c[0:64, C_BLK64:C_BLK64 + 64] = 1.0
    c[64:128, C_BLK64 + 64:C_BLK64 + 128] = 1.0
    i = np.arange(64)
    c[0:64, C_U64:C_U64 + 64] = (i[:, None] <= i[None, :])
    c[0:64, C_ONES64:C_ONES64 + 64] = 1.0
    c[0:64, C_SL:C_SL + 64] = (i[:, None] > i[None, :])
    c[0:64, C_UI:C_UI + 64] = (i[None, :] >= i[:, None])
    t = np.arange(128)
    c[:, C_CAUS:C_CAUS + 128] = (t[None, :] <= t[:, None])
    c[:, C_NEGC:C_NEGC + 128] = np.where(t[None, :] <= t[:, None], 0.0, -1e30)
    c[:, C_POW2:C_POW2 + 32] = 2.0 ** -(np.arange(32) + 1.0)
    c[:, C_123:C_123 + 3] = np.array([1.0, 2.0, 3.0])
    c[:, C_POW4:C_POW4 + 16] = 4.0 ** -(np.arange(16) + 1.0)
    return c


class K:
    pass


def build(debug=None):
    nc = bass.Bass("TRN2", target_bir_lowering=False)
    k = K()
    build.k = k
    k.nc = nc
    k.debug = debug
    P = Prog(nc)
    k.P = P

    k.in_names = []
    skip_pre = bool(debug and "skip_pre" in debug)

    def din(name, shape, dt=F32):
        if skip_pre and int(np.prod(shape)) > 200000:
            return None
        k.in_names.append(name)
        return nc.dram_tensor(name, list(shape), dt, kind="ExternalInput").ap()

    def dscr(name, shape, dt=F32):
        kind = "ExternalOutput" if (debug and name in debug) else "Internal"
        return nc.dram_tensor(name, list(shape), dt, kind=kind).ap()

    k.xT = din("xT", [D, S])
    k.pT = din("pT", [256, S])
    k.consts = din("consts", [128, C_N])
    k.gains = din("gains", [128, 64])
    k.ffn_w_in = [din("ffn1_w_in", [128, 8, 2 * DFF]), din("ffn2_w_in", [128, 8, 2 * DFF])]
    k.ffn_w_out = [din("ffn1_w_out", [128, NFC, D]), din("ffn2_w_out", [128, NFC, D])]
    k.outT = nc.dram_tensor("outT", [D, S], F32, kind="ExternalOutput").ap()
    k.mix_w1 = din("mix_w1", [128, 8, NW1])
    k.mix_w2 = din("mix_w2", [128, 8, 2048])
    k.w_br_a = din("w_br_a", [128, 4, D])
    k.w_br_b = din("w_br_b", [128, 4, D])
    k.mix_w_out = din("mix_w_out", [128, 8, D])
    k.ple_wg = din("ple_wg", [128, 8, D])
    k.ple_wp = din("ple_wp", [128, 2, D])
    k.conv_w = din("conv_w", [128, 12, 4])
    k.rows_d = din("rows", [128, R_N])
    k.cs_d = din("cs", [128, 32, 16])
    k.h1T = dscr("h1T", [D, S])
    k.d = {}
    k.bd = {}
    for nm, shp, dt in (("qaT", [512, S], BF16), ("kaT", [512, S], BF16), ("ka_tok", [S, 512], BF16), ("va_tok", [S, 512], BF16),
                        ("z_tok", [S, 512], F32), ("gb_tok", [S, 16], F32), ("qbT", [8, 65, S], BF16), ("kbT", [8, 65, S], BF16),
                        ("vb_tok", [S, 8 * 65], BF16), ("qiT", [8, 64, S], BF16), ("kiT", [64, S], BF16), ("wi_tok", [S, 8], F32),
                        ("oaT", [512, S], BF16), ("obT", [512, S], BF16), ("h2T", [D, S], F32), ("h3T", [D, S], F32)):
        k.d[nm] = dscr(nm, shp, dt)
        k.bd[nm] = P.buf("d_" + nm, True)

    big = nc.alloc_sbuf_tensor("big", [128, 212480], U8)
    k.big = big
    k.ps = [nc.alloc_psum_tensor("ps%d" % i, [128, 512], F32) for i in range(8)]
    k.b_ps = [P.buf("ps%d" % i) for i in range(8)]
    for b_ in k.b_ps:
        b_.excl = True
    k.ps_i = 0
    k.ps_pool = list(range(8))
    k.ps_held = set()
    k.gdn_n = 64
    k.gdn_stages = None
    if debug:
        for f in debug:
            if f.startswith("gdn_n="):
                k.gdn_n = int(f.split("=")[1])
            if f.startswith("gdn_stages="):
                k.gdn_stages = int(f.split("=")[1])

    cv = Carver(big, 0, 9216)
    k.c32 = cv.t([128, C_N], F32)
    k.cbf = cv.t([128, C_N], BF16)
    k.gsb = cv.t([128, 64], F32)
    k.epsb = cv.t([128, 1], F32)
    k.rows = cv.t([128, R_N], F32)
    k.cs = cv.t([128, 32, 16], F32)
    k.kmax = cv.t([128, 8], F32)
    k.oneb = cv.t([128, 1], F32)
    k.b_rows = P.buf("rows", True)
    k.b_cs = P.buf("cs", True)
    k.b_kmax = P.buf("kmax")
    P.dma("sp", lambda e: e.dma_start(out=k.rows, in_=k.rows_d), [], [k.b_rows])
    P.dma("sp", lambda e: e.dma_start(out=k.cs, in_=k.cs_d), [], [k.b_cs])
    k.b_c = P.buf("consts", True)
    k.b_cbf = P.buf("cbf")
    k.b_g = P.buf("gains", True)
    k.b_eps = P.buf("eps")
    P.dma("sp", lambda e: e.dma_start(out=k.c32, in_=k.consts), [], [k.b_c])
    P.dma("sp", lambda e: e.dma_start(out=k.gsb, in_=k.gains), [], [k.b_g])
    P.op("dve", lambda e: e.tensor_copy(out=k.cbf, in_=k.c32), [k.b_c], [k.b_cbf])
    P.op("dve", lambda e: e.memset(k.epsb, EPS), [], [k.b_eps])
    P.op("dve", lambda e: e.memset(k.oneb, 1.0), [], [k.b_eps])
    k.PH0 = 9216
    P.persistent = False
    k.PHLIM = 212480

    if not (debug and "skip_pre" in debug):
        ffn_pass(k, 0, k.xT, k.h1T, final=False)
        P.barrier()
        if debug and "stop_after_ffn1" in debug:
            P.replay()
            return nc
        mixproj_pass(k)
        P.barrier()
    if debug and "stop_after_proj" in debug:
        P.replay()
        return nc
    gdn_pass(k)
    P.barrier()
    if debug and "stop_after_gdn" in debug:
        P.replay()
        return nc
    dsa_pass(k)
    P.barrier()
    if debug and "stop_after_dsa" in debug:
        P.replay()
        return nc
    merge_pass(k)
    P.barrier()
    ffn_pass(k, 1, k.d["h2T"], k.d["h3T"], final=False)
    P.barrier()
    ple_pass(k)
    P.barrier()
    P.replay()
    return nc


def O(k, eng, method, reads, writes, **kw):
    k.P.op(eng, lambda e: getattr(e, method)(**kw), reads, writes)


def DMA(k, eng, reads, writes, **kw):
    k.P.dma(eng, lambda e: e.dma_start(**kw), reads, writes)


def next_ps(k, hold=False):
    pool = k.ps_pool
    for _ in range(len(pool)):
        i = pool[k.ps_i % len(pool)]
        k.ps_i = (k.ps_i + 1) % len(pool)
        if i not in k.ps_held:
            if hold:
                k.ps_held.add(i)
            return k.ps[i], k.b_ps[i]
    raise RuntimeError("out of PSUM banks")


def rel_ps(k, b_ps):
    k.ps_held.discard(k.b_ps.index(b_ps))


def rms_stats(k, sq, b_sq, rstd, b_rstd, T, nchunk=8):
    P = k.P
    ps, b_ps = next_ps(k)
    ones = k.cbf[:, C_ONESD:C_ONESD + 128]
    for c in range(nchunk):
        P.op("pe", lambda e, c=c: e.matmul(ps[:, 0:T], lhsT=ones, rhs=sq[:, c, :], start=(c == 0), stop=(c == nchunk - 1)),
             [k.b_cbf, b_sq], [b_ps])
    P.op("act", lambda e: e.activation(out=rstd, in_=ps[:, 0:T], func=AF.Sqrt, bias=k.epsb, scale=1.0),
         [b_ps, k.b_eps], [b_rstd])
    P.op("dve", lambda e: e.reciprocal(out=rstd, in_=rstd), [b_rstd], [b_rstd])


def ffn_pass(k, which, srcT, dstT, final):
    P = k.P
    nc = k.nc
    T = TF
    cv = Carver(k.big, k.PH0, k.PHLIM)
    w_in = cv.t([128, 8, 2 * DFF], BF16)
    w_out = cv.t([128, NFC, D], BF16)
    hT = [cv.t([128, 8, T], F32)] * 2
    su = cv.t([128, 8, T], BF16)
    a = cv.t([128, NFC, T], BF16)
    y = cv.t([128, 8, T], F32)
    rstd = cv.t([128, T], F32)
    sg = [cv.t([128, T], F32)] * 2
    b_win = [P.buf("w_in%d_%d" % (which, c), True) for c in range(8)]
    b_wout = [P.buf("w_out%d_%d" % (which, c), True) for c in range(2)]
    b_h = [P.buf("hT%d" % which, True)] * 2
    b_su = P.buf("su%d" % which)
    b_a = [P.buf("a%d_%d" % (which, i)) for i in range(NFC)]
    b_y = P.buf("y%d" % which)
    b_rstd = P.buf("rstd%d" % which)
    b_sg = [P.buf("sg%d" % which)] * 2
    b_dst = P.buf("dst%d" % which, True)
    if final:
        wg = cv.t([128, 8, D], BF16)
        wp = cv.t([128, 2, D], BF16)
        pt = [cv.t([128, 2, T], BF16) for _ in range(2)]
        b_wg = P.buf("ple_wg", True)
        b_wp = P.buf("ple_wp", True)
        b_pt = [P.buf("ple_pt%d" % i, True) for i in range(2)]
        DMA(k, "pool", [], [b_wg], out=wg, in_=k.ple_wg)
        DMA(k, "pool", [], [b_wp], out=wp, in_=k.ple_wp)
    g_pre = k.gsb[:, 0:8] if which == 0 else k.gsb[:, 32:40]
    g_post = k.gsb[:, 8:16] if which == 0 else k.gsb[:, 40:48]
    wi_d = k.ffn_w_in[which]
    wo_d = k.ffn_w_out[which]
    for c in range(8):
        P.dma("pool", lambda e, c=c: e.dma_start(out=w_in[:, c, :], in_=wi_d[:, c, :]), [], [b_win[c]])
    for c in range(2):
        P.dma("pool", lambda e, c=c: e.dma_start(out=w_out[:, c * 11:(c + 1) * 11, :], in_=wo_d[:, c * 11:(c + 1) * 11, :]),
              [], [b_wout[c]])
    ghalf = cv.t([128, 8], F32)
    b_gh = P.buf("ghalf%d" % which)
    P.op("dve", lambda e: e.tensor_scalar(out=ghalf, in0=g_post, scalar1=0.5, scalar2=None, op0=ALU.mult),
         [k.b_g], [b_gh])
    ntile = S // T
    for it in range(ntile):
        t0 = it * T
        hb = hT[it % 2]
        bh = b_h[it % 2]
        P.dma("sp", lambda e, hb=hb, t0=t0: e.dma_start(out=hb, in_=srcT[:, t0:t0 + T].rearrange("(c p) t -> p c t", p=128)),
              [], [bh])
        for c in range(8):
            P.op("act", lambda e, c=c, hb=hb: e.activation(out=su[:, c, :], in_=hb[:, c, :], func=AF.Square), [bh], [b_su])
        rms_stats(k, su, b_su, rstd, b_rstd, T)
        for c in range(8):
            P.op("dve", lambda e, c=c, hb=hb: e.scalar_tensor_tensor(out=su[:, c, :], in0=hb[:, c, :], scalar=g_pre[:, c:c + 1],
                                                                   in1=rstd, op0=ALU.mult, op1=ALU.mult),
                 [bh, k.b_g, b_rstd], [b_su])
        for fc in range(NFC):
            psg, b_psg = next_ps(k)
            psu, b_psu = next_ps(k)
            for c in range(8):
                P.op("pe", lambda e, c=c, fc=fc, psg=psg: e.matmul(psg[:, 0:T], lhsT=w_in[:, c, fc * 128:(fc + 1) * 128],
                                                                 rhs=su[:, c, :], start=(c == 0), stop=(c == 7)),
                     [b_win[c], b_su], [b_psg])
            for c in range(8):
                P.op("pe", lambda e, c=c, fc=fc, psu=psu: e.matmul(psu[:, 0:T], lhsT=w_in[:, c, DFF + fc * 128:DFF + (fc + 1) * 128],
                                                                 rhs=su[:, c, :], start=(c == 0), stop=(c == 7)),
                     [b_win[c], b_su], [b_psu])
            sgb = sg[fc % 2]
            bsg = b_sg[fc % 2]
            P.op("act", lambda e, sgb=sgb, psg=psg: e.activation(out=sgb, in_=psg[:, 0:T], func=AF.Silu), [b_psg], [bsg])
            P.op("dve", lambda e, sgb=sgb, psu=psu, fc=fc: e.tensor_tensor(out=a[:, fc, :], in0=sgb, in1=psu[:, 0:T], op=ALU.mult),
                 [bsg, b_psu], [b_a[fc]])
        for dc in range(8):
            psy, b_psy = next_ps(k)
            for fc in range(NFC):
                P.op("pe", lambda e, dc=dc, fc=fc, psy=psy: e.matmul(psy[:, 0:T], lhsT=w_out[:, fc, dc * 128:(dc + 1) * 128],
                                                                   rhs=a[:, fc, :], start=(fc == 0), stop=(fc == NFC - 1)),
                     [b_wout[fc // 11], b_a[fc]], [b_psy])
            P.op("act", lambda e, dc=dc, psy=psy: e.activation(out=y[:, dc, :], in_=psy[:, 0:T], func=AF.Copy), [b_psy], [b_y])
            P.op("act", lambda e, dc=dc, psy=psy: e.activation(out=su[:, dc, :], in_=psy[:, 0:T], func=AF.Square), [b_psy], [b_su])
        rms_stats(k, su, b_su, rstd, b_rstd, T)
        for c in range(8):
            P.op("dve", lambda e, c=c: e.scalar_tensor_tensor(out=y[:, c, :], in0=y[:, c, :], scalar=ghalf[:, c:c + 1],
                                                            in1=rstd, op0=ALU.mult, op1=ALU.mult),
                 [b_y, b_gh, b_rstd], [b_y])
        for c in range(8):
            P.op("pool", lambda e, c=c, hb=hb: e.tensor_tensor(out=hb[:, c, :], in0=hb[:, c, :], in1=y[:, c, :], op=ALU.add),
                 [bh, b_y], [bh])
        if not final:
            P.dma("sp", lambda e, hb=hb, t0=t0: e.dma_start(out=dstT[:, t0:t0 + T].rearrange("(c p) t -> p c t", p=128), in_=hb),
                  [bh], [b_dst])
        else:
            gp_pre = k.gsb[:, 48:56]
            gp_post = k.gsb[:, 56:64]
            DMA(k, "pool", [], [b_pt[it % 2]], out=pt[it % 2], in_=k.pT[:, t0:t0 + T].rearrange("(c p) t -> p c t", p=128))
            for c in range(8):
                P.op("act", lambda e, c=c, hb=hb: e.activation(out=su[:, c, :], in_=hb[:, c, :], func=AF.Square), [bh], [b_su])
            rms_stats(k, su, b_su, rstd, b_rstd, T)
            for c in range(8):
                P.op("dve", lambda e, c=c, hb=hb: e.scalar_tensor_tensor(out=su[:, c, :], in0=hb[:, c, :], scalar=gp_pre[:, c:c + 1],
                                                                       in1=rstd, op0=ALU.mult, op1=ALU.mult),
                     [bh, k.b_g, b_rstd], [b_su])
            for dc in range(8):
                psg, b_psg = next_ps(k)
                for c in range(8):
                    O(k, "pe", "matmul", [b_wg, b_su], [b_psg], out=psg[:, 0:T], lhsT=wg[:, c, dc * 128:(dc + 1) * 128], rhs=su[:, c, :],
                      start=(c == 0), stop=(c == 7))
                psp, b_psp = next_ps(k)
                for c in range(2):
                    O(k, "pe", "matmul", [b_wp, b_pt[it % 2]], [b_psp], out=psp[:, 0:T], lhsT=wp[:, c, dc * 128:(dc + 1) * 128],
                      rhs=pt[it % 2][:, c, :], start=(c == 0), stop=(c == 1))
                sgb = sg[dc % 2]
                bsg = b_sg[dc % 2]
                O(k, "act", "activation", [b_psg], [bsg], out=sgb, in_=psg[:, 0:T], func=AF.Sigmoid)
                O(k, "dve", "tensor_tensor", [bsg, b_psp], [b_y], out=y[:, dc, :], in0=sgb, in1=psp[:, 0:T], op=ALU.mult)
            for c in range(8):
                O(k, "act", "activation", [b_y], [b_su], out=su[:, c, :], in_=y[:, c, :], func=AF.Square)
            rms_stats(k, su, b_su, rstd, b_rstd, T)
            for c in range(8):
                O(k, "dve", "scalar_tensor_tensor", [b_y, k.b_g, b_rstd], [b_y], out=y[:, c, :], in0=y[:, c, :], scalar=gp_post[:, c:c + 1],
                  in1=rstd, op0=ALU.mult, op1=ALU.mult)
            for c in range(8):
                O(k, "pool", "tensor_tensor", [bh, b_y], [bh], out=hb[:, c, :], in0=hb[:, c, :], in1=y[:, c, :], op=ALU.add)
            DMA(k, "sp", [bh], [b_dst], out=dstT[:, t0:t0 + T].rearrange("(c p) t -> p c t", p=128), in_=hb)


def ple_pass(k):
    P = k.P
    B = P.buf
    T = 512
    cv = Carver(k.big, k.PH0, k.PHLIM)
    wg = cv.t([128, 8, D], BF16)
    wp = cv.t([128, 2, D], BF16)
    hT = [cv.t([128, 8, T], F32) for _ in range(2)]
    pt = [cv.t([128, 2, T], BF16) for _ in range(2)]
    su = cv.t([128, 8, T], BF16)
    rstd = cv.t([128, T], F32)
    sg = [cv.t([128, T], F32) for _ in range(2)]
    y = cv.t([128, 8, T], F32)
    b_wg = B("ple_wg", True)
    b_wp = B("ple_wp", True)
    b_h = [B("ple_h%d" % i, True) for i in range(2)]
    b_pt = [B("ple_pt%d" % i, True) for i in range(2)]
    b_su = B("ple_su")
    b_rstd = B("ple_rstd")
    b_sg = [B("ple_sg%d" % i) for i in range(2)]
    b_y = B("ple_y")
    b_dst = B("ple_dst", True)
    gp_pre = k.gsb[:, 48:56]
    gp_post = k.gsb[:, 56:64]
    DMA(k, "pool", [], [b_wg], out=wg, in_=k.ple_wg)
    DMA(k, "pool", [], [b_wp], out=wp, in_=k.ple_wp)
    src = k.d["h3T"]
    for it in range(S // T):
        t0 = it * T
        hb = hT[it % 2]
        bh = b_h[it % 2]
        DMA(k, "sp", [], [bh], out=hb, in_=src[:, t0:t0 + T].rearrange("(c p) t -> p c t", p=128))
        DMA(k, "pool", [], [b_pt[it % 2]], out=pt[it % 2], in_=k.pT[:, t0:t0 + T].rearrange("(c p) t -> p c t", p=128))
        for c in range(8):
            O(k, "act", "activation", [bh], [b_su], out=su[:, c, :], in_=hb[:, c, :], func=AF.Square)
        rms_stats(k, su, b_su, rstd, b_rstd, T)
        for c in range(8):
            O(k, "dve", "scalar_tensor_tensor", [bh, k.b_g, b_rstd], [b_su], out=su[:, c, :], in0=hb[:, c, :], scalar=gp_pre[:, c:c + 1],
              in1=rstd, op0=ALU.mult, op1=ALU.mult)
        for dc in range(8):
            psg, b_psg = next_ps(k)
            for c in range(8):
                O(k, "pe", "matmul", [b_wg, b_su], [b_psg], out=psg[:, 0:T], lhsT=wg[:, c, dc * 128:(dc + 1) * 128], rhs=su[:, c, :],
                  start=(c == 0), stop=(c == 7))
            psp, b_psp = next_ps(k)
            for c in range(2):
                O(k, "pe", "matmul", [b_wp, b_pt[it % 2]], [b_psp], out=psp[:, 0:T], lhsT=wp[:, c, dc * 128:(dc + 1) * 128],
                  rhs=pt[it % 2][:, c, :], start=(c == 0), stop=(c == 1))
            sgb = sg[dc % 2]
            bsg = b_sg[dc % 2]
            O(k, "act", "activation", [b_psg], [bsg], out=sgb, in_=psg[:, 0:T], func=AF.Sigmoid)
            O(k, "dve", "tensor_tensor", [bsg, b_psp], [b_y], out=y[:, dc, :], in0=sgb, in1=psp[:, 0:T], op=ALU.mult)
        for c in range(8):
            O(k, "act", "activation", [b_y], [b_su], out=su[:, c, :], in_=y[:, c, :], func=AF.Square)
        rms_stats(k, su, b_su, rstd, b_rstd, T)
        for c in range(8):
            O(k, "dve", "scalar_tensor_tensor", [b_y, k.b_g, b_rstd], [b_y], out=y[:, c, :], in0=y[:, c, :], scalar=gp_post[:, c:c + 1],
              in1=rstd, op0=ALU.mult, op1=ALU.mult)
        for c in range(8):
            O(k, "pool", "tensor_tensor", [bh, b_y], [bh], out=hb[:, c, :], in0=hb[:, c, :], in1=y[:, c, :], op=ALU.add)
        DMA(k, "sp", [bh], [b_dst], out=k.outT[:, t0:t0 + T].rearrange("(c p) t -> p c t", p=128), in_=hb)


def host_rows(inputs):
    r = np.concatenate([np.asarray(inputs[n][0], np.float32).ravel() for n in ("dt_bias", "a_log", "dn_norm_g", "idx_k_norm_g")])
    return np.ascontiguousarray(np.broadcast_to(r[None, :], (128, R_N)))


def host_cs():
    pos = np.arange(S, dtype=np.float32)
    inv = (np.float32(500000.0) ** (-np.arange(0, 16, 2, dtype=np.float32) / np.float32(16.0))).astype(np.float32)
    ang = (pos[:, None] * inv[None, :]).astype(np.float32)
    cs = np.concatenate([np.cos(ang), np.sin(ang)], axis=1).astype(np.float32)
    return np.ascontiguousarray(cs.reshape(32, 128, 16).transpose(1, 0, 2))


def _fm(v):
    return np.ascontiguousarray(np.asarray(v, np.float32).reshape(8, 128).T)


def kernel(**inputs):
    dbg = os.environ.get("KDEBUG")
    debug = set(dbg.split(",")) if dbg else None
    x = np.asarray(inputs["x"], np.float32)
    p = np.asarray(inputs["p"], np.float32)[0]
    nb = x.shape[0]
    gains = np.concatenate([_fm(inputs[n][0]) for n in (
        "ffn1_norm_pre", "ffn1_norm_post", "mix_norm_pre", "mix_norm_post", "ffn2_norm_pre",
        "ffn2_norm_post", "ple_norm_pre", "ple_norm_post")], axis=1)

    def w_in_l(w):
        w = np.asarray(w, np.float32)
        return np.ascontiguousarray(w.reshape(w.shape[0] // 128, 128, -1).transpose(1, 0, 2))

    def w_out_l(w):
        return np.ascontiguousarray(np.asarray(w, np.float32).reshape(NFC, 128, -1).transpose(1, 0, 2))

    shared = {
        "consts": host_consts(),
        "gains": np.ascontiguousarray(gains),
        "mix_w1": w_in_l(np.asarray(inputs["mix_w_in"][0])[:, 0:NW1]),
        "mix_w2": w_in_l(np.asarray(inputs["mix_w_in"][0])[:, NW1:]),
        "w_br_a": w_in_l(inputs["w_br_a"][0]),
        "w_br_b": w_in_l(inputs["w_br_b"][0]),
        "mix_w_out": w_in_l(inputs["mix_w_out"][0]),
        "ple_wg": w_in_l(inputs["ple_w_gate"][0]),
        "ple_wp": w_in_l(inputs["ple_w_proj"][0]),
        "conv_w": np.ascontiguousarray(np.asarray(inputs["conv_w"][0], np.float32).T.reshape(12, 128, 4).transpose(1, 0, 2)),
        "rows": host_rows(inputs),
        "cs": host_cs(),
        "ffn1_w_in": w_in_l(inputs["ffn1_w_in"][0]),
        "ffn2_w_in": w_in_l(inputs["ffn2_w_in"][0]),
        "ffn1_w_out": w_out_l(inputs["ffn1_w_out"][0]),
        "ffn2_w_out": w_out_l(inputs["ffn2_w_out"][0]),
    }
    in_maps = []
    for b in range(nb):
        m = dict(shared)
        m["xT"] = np.ascontiguousarray(x[b].T)
        m["pT"] = np.ascontiguousarray(p[b].T)
        in_maps.append(m)
    nc = build(debug)
    if debug:
        in_maps = [{n: v for n, v in in_maps[0].items() if n in build.k.in_names}]
        nb = 1
    res = run_bass_kernel_spmd(nc, in_maps, core_ids=list(range(nb)))
    if debug:
        kernel.last = res.results
    out = np.stack([np.ascontiguousarray(r["outT"].T) for r in res.results], axis=0)
    return out.astype(np.float32)


R_DTB = 0
R_ALOG = 8
R_DNG = 16
R_IKG = 80
R_N = 144
NW1 = 4184


def mixproj_pass(k):
    P = k.P
    T = TM
    cv = Carver(k.big, k.PH0, k.PHLIM)
    w1 = cv.t([128, 8, NW1], BF16)
    hT = cv.t([128, 8, T], F32)
    su = cv.t([128, 8, T], BF16)
    rstd = cv.t([128, T], F32)
    cw = cv.t([128, 12, 4], F32)
    hal = cv.t([128, 12, 3], F32)
    cb = [cv.t([128, T + 3], F32) for _ in range(2)]
    acc = [cv.t([128, T], F32) for _ in range(2)]
    sil = [cv.t([128, T], F32) for _ in range(2)]
    sqb = [cv.t([128, T], BF16) for _ in range(2)]
    rn = [cv.t([128, T], F32) for _ in range(2)]
    qkT = cv.t([128, 8, T], BF16)
    vT = cv.t([128, 4, T], BF16)
    tokst = [cv.t([128, 512], BF16) for _ in range(2)]
    xs = [cv.t([128, 8, 64], F32) for _ in range(2)]
    xo = [cv.t([128, 8, 65], BF16) for _ in range(4)]
    tmp = [cv.t([128, 8, 8], F32) for _ in range(4)]
    sq5 = cv.t([128, 8, 64], F32)
    n2 = cv.t([128, 8], F32)
    zst = [cv.t([128, 512], F32) for _ in range(2)]
    gbst = [cv.t([128, 16], F32) for _ in range(2)]
    abt = cv.t([128, 16], F32)
    wist = [cv.t([128, 8], F32) for _ in range(2)]
    kis = cv.t([128, 72], F32)
    kio = cv.t([128, 64], BF16)
    kss = cv.t([128, 1], F32)
    vo = [cv.t([128, 8, 65], BF16) for _ in range(2)]
    st_qb = cv.t([65, 8, T], BF16)
    st_kb = cv.t([65, 8, T], BF16)
    st_qi = cv.t([64, 8, T], BF16)
    st_ki = cv.t([64, T], BF16)
    nea = cv.t([128, 8], F32)

    B = P.buf
    b_w1 = [B("w1_%d" % c, True) for c in range(8)]
    b_h = B("mp_h", True)
    b_su = B("mp_su")
    b_rstd = B("mp_rstd")
    b_cw = B("mp_cw", True)
    b_hal = [B("mp_hal%d" % i) for i in range(12)]
    b_cb = [B("mp_cb%d" % i) for i in range(2)]
    b_acc = [B("mp_acc%d" % i) for i in range(2)]
    b_sil = [B("mp_sil%d" % i) for i in range(2)]
    b_sqb = [B("mp_sqb%d" % i) for i in range(2)]
    b_rn = [B("mp_rn%d" % i) for i in range(2)]
    b_qkT = [B("mp_qkT%d" % i) for i in range(8)]
    b_vT = [B("mp_vT%d" % i) for i in range(4)]
    b_tokst = [B("mp_tokst%d" % i) for i in range(2)]
    b_xs = [B("mp_xs%d" % i) for i in range(2)]
    b_xo = [B("mp_xo%d" % i) for i in range(4)]
    b_tmp = [B("mp_tmp%d" % i) for i in range(4)]
    b_sq5 = B("mp_sq5")
    b_n2 = B("mp_n2")
    b_zst = [B("mp_zst%d" % i) for i in range(2)]
    b_gbst = [B("mp_gbst%d" % i) for i in range(2)]
    b_abt = B("mp_abt")
    b_wist = [B("mp_wist%d" % i) for i in range(2)]
    b_kis = B("mp_kis")
    b_kio = B("mp_kio")
    b_kss = B("mp_kss")
    b_vo = [B("mp_vo%d" % i) for i in range(2)]
    b_stqb = B("mp_stqb")
    b_stkb = B("mp_stkb")
    b_stqi = B("mp_stqi")
    b_stki = B("mp_stki")
    b_nea = B("mp_nea")
    d = k.d
    bd = k.bd

    ident_bf = k.cbf[:, C_IDENT:C_IDENT + 128]
    blk64 = k.cbf[:, C_BLK64:C_BLK64 + 128]
    g_pre = k.gsb[:, 16:24]

    for c in range(8):
        DMA(k, "pool", [], [b_w1[c]], out=w1[:, c, :], in_=k.mix_w1[:, c, :])
    DMA(k, "sp", [], [b_cw], out=cw, in_=k.conv_w)
    for fc in range(12):
        O(k, "pool", "memset", [], [b_hal[fc]], ap=hal[:, fc, :], constant=0.0)
    O(k, "act", "activation", [k.b_rows], [b_nea], out=nea, in_=k.rows[:, R_ALOG:R_ALOG + 8], func=AF.Exp)
    O(k, "dve", "tensor_scalar", [b_nea], [b_nea], out=nea, in0=nea, scalar1=-1.0, scalar2=None, op0=ALU.mult)
    O(k, "dve", "memset", [], [k.b_kmax], ap=k.kmax, constant=0.0)
    for i in range(2):
        O(k, "pool", "memset", [], [b_vo[i]], ap=vo[i][:, :, 64:65], constant=1.0)

    ntile = S // T
    for it in range(ntile):
        t0 = it * T
        DMA(k, "sp", [], [b_h], out=hT, in_=k.h1T[:, t0:t0 + T].rearrange("(c p) t -> p c t", p=128))
        for c in range(8):
            O(k, "act", "activation", [b_h], [b_su], out=su[:, c, :], in_=hT[:, c, :], func=AF.Square)
        rms_stats(k, su, b_su, rstd, b_rstd, T)
        for c in range(8):
            O(k, "dve", "scalar_tensor_tensor", [b_h, k.b_g, b_rstd], [b_su], out=su[:, c, :], in0=hT[:, c, :],
              scalar=g_pre[:, c:c + 1], in1=rstd, op0=ALU.mult, op1=ALU.mult)
        for fc in range(12):
            ps, b_ps = next_ps(k)
            for c in range(8):
                O(k, "pe", "matmul", [b_w1[c], b_su], [b_ps], out=ps[:, 0:T], lhsT=w1[:, c, fc * 128:(fc + 1) * 128],
                  rhs=su[:, c, :], start=(c == 0), stop=(c == 7))
            j = fc % 2
            O(k, "pool", "tensor_copy", [b_hal[fc]], [b_cb[j]], out=cb[j][:, 0:3], in_=hal[:, fc, :])
            O(k, "act", "activation", [b_ps], [b_cb[j]], out=cb[j][:, 3:T + 3], in_=ps[:, 0:T], func=AF.Copy)
            O(k, "pool", "tensor_copy", [b_cb[j]], [b_hal[fc]], out=hal[:, fc, :], in_=cb[j][:, T:T + 3])
            O(k, "dve", "tensor_scalar", [b_cb[j], b_cw], [b_acc[j]], out=acc[j], in0=cb[j][:, 0:T],
              scalar1=cw[:, fc, 0:1], scalar2=None, op0=ALU.mult)
            for jj in range(1, 4):
                O(k, "dve", "scalar_tensor_tensor", [b_cb[j], b_cw, b_acc[j]], [b_acc[j]], out=acc[j], in0=cb[j][:, jj:jj + T],
                  scalar=cw[:, fc, jj:jj + 1], in1=acc[j], op0=ALU.mult, op1=ALU.add)
            if fc < 8:
                O(k, "act", "activation", [b_acc[j]], [b_sil[j]], out=sil[j], in_=acc[j], func=AF.Silu)
                O(k, "act", "activation", [b_sil[j]], [b_sqb[j]], out=sqb[j], in_=sil[j], func=AF.Square)
                ps2, b_ps2 = next_ps(k)
                O(k, "pe", "matmul", [k.b_cbf, b_sqb[j]], [b_ps2], out=ps2[:, 0:T], lhsT=blk64, rhs=sqb[j], start=True, stop=True)
                O(k, "act", "activation", [b_ps2, k.b_eps], [b_rn[j]], out=rn[j], in_=ps2[:, 0:T], func=AF.Sqrt, bias=k.epsb, scale=1.0)
                O(k, "dve", "reciprocal", [b_rn[j]], [b_rn[j]], out=rn[j], in_=rn[j])
                O(k, "dve", "scalar_tensor_tensor", [b_sil[j], b_rn[j]], [b_qkT[fc]], out=qkT[:, fc, :], in0=sil[j],
                  scalar=(0.125 if fc < 4 else 1.0), in1=rn[j], op0=ALU.mult, op1=ALU.mult)
            else:
                O(k, "act", "activation", [b_acc[j]], [b_vT[fc - 8]], out=vT[:, fc - 8, :], in_=acc[j], func=AF.Silu)
        DMA(k, "sp", [b_qkT[i] for i in range(4)], [bd["qaT"]], out=d["qaT"][:, t0:t0 + T].rearrange("(c p) t -> p c t", p=128),
            in_=qkT[:, 0:4, :])
        DMA(k, "sp", [b_qkT[i] for i in range(4, 8)], [bd["kaT"]], out=d["kaT"][:, t0:t0 + T].rearrange("(c p) t -> p c t", p=128),
            in_=qkT[:, 4:8, :])
        for sub in range(T // 128):
            for wh in range(2):
                ps, b_ps = next_ps(k)
                psb = ps[:, :].bitcast(BF16)
                for c4 in range(4):
                    if wh == 0:
                        O(k, "pe", "transpose", [b_qkT[4 + c4], k.b_cbf], [b_ps], out=psb[:, c4 * 128:(c4 + 1) * 128],
                          in_=qkT[:, 4 + c4, sub * 128:(sub + 1) * 128], identity=ident_bf)
                    else:
                        O(k, "pe", "transpose", [b_vT[c4], k.b_cbf], [b_ps], out=psb[:, c4 * 128:(c4 + 1) * 128],
                          in_=vT[:, c4, sub * 128:(sub + 1) * 128], identity=ident_bf)
                O(k, "act", "activation", [b_ps], [b_tokst[wh]], out=tokst[wh], in_=psb[:, 0:512], func=AF.Copy)
                dst = "ka_tok" if wh == 0 else "va_tok"
                DMA(k, "sp", [b_tokst[wh]], [bd[dst]], out=d[dst][t0 + sub * 128:t0 + (sub + 1) * 128, :], in_=tokst[wh])
        def subgen(sub, t0=t0):
            blk = (t0 // 128) + sub
            tsl = slice(sub * 128, (sub + 1) * 128)
            r0 = t0 + sub * 128
            cosb = k.cs[:, blk, 0:8].unsqueeze(1).to_broadcast([128, 8, 8])
            sinb = k.cs[:, blk, 8:16].unsqueeze(1).to_broadcast([128, 8, 8])

            def proj(col0, n):
                ps, b_ps = next_ps(k)
                for c in range(8):
                    O(k, "pe", "matmul", [b_w1[c], b_su], [b_ps], out=ps[:, 0:n], lhsT=su[:, c, tsl], rhs=w1[:, c, col0:col0 + n],
                      start=(c == 0), stop=(c == 7))
                return ps, b_ps

            def rope(x3, bx, o3, bo, nh):
                cb_ = cosb[:, 0:nh, :]
                sb_ = sinb[:, 0:nh, :]
                x1 = x3[:, :, 0:8]
                x2 = x3[:, :, 8:16]
                O(k, "dve", "tensor_tensor", [bx, k.b_cs], [b_tmp[0]], out=tmp[0][:, 0:nh, :], in0=x1, in1=cb_, op=ALU.mult)
                O(k, "dve", "tensor_tensor", [bx, k.b_cs], [b_tmp[1]], out=tmp[1][:, 0:nh, :], in0=x2, in1=sb_, op=ALU.mult)
                O(k, "pool", "tensor_tensor", [bx, k.b_cs], [b_tmp[2]], out=tmp[2][:, 0:nh, :], in0=x2, in1=cb_, op=ALU.mult)
                O(k, "pool", "tensor_tensor", [bx, k.b_cs], [b_tmp[3]], out=tmp[3][:, 0:nh, :], in0=x1, in1=sb_, op=ALU.mult)
                O(k, "dve", "tensor_tensor", [b_tmp[0], b_tmp[1]], [bo], out=o3[:, :, 0:8], in0=tmp[0][:, 0:nh, :],
                  in1=tmp[1][:, 0:nh, :], op=ALU.subtract)
                O(k, "pool", "tensor_tensor", [b_tmp[2], b_tmp[3]], [bo], out=o3[:, :, 8:16], in0=tmp[2][:, 0:nh, :],
                  in1=tmp[3][:, 0:nh, :], op=ALU.add)
                O(k, "act", "activation", [bx], [bo], out=o3[:, :, 16:64], in_=x3[:, :, 16:64], func=AF.Copy)

            ps, b_ps = proj(1536, 512)
            j = sub % 2
            O(k, "act", "activation", [b_ps], [b_zst[j]], out=zst[j], in_=ps[:, 0:512], func=AF.Silu)
            DMA(k, "sp", [b_zst[j]], [bd["z_tok"]], out=d["z_tok"][r0:r0 + 128, :], in_=zst[j])
            yield
            ps, b_ps = proj(2048, 16)
            O(k, "dve", "tensor_tensor", [b_ps, k.b_rows], [b_abt], out=abt[:, 0:8], in0=ps[:, 0:8], in1=k.rows[:, R_DTB:R_DTB + 8],
              op=ALU.add)
            O(k, "act", "activation", [b_abt], [b_abt], out=abt[:, 0:8], in_=abt[:, 0:8], func=AF.Exp)
            O(k, "act", "activation", [b_abt], [b_abt], out=abt[:, 0:8], in_=abt[:, 0:8], func=AF.Ln, bias=k.oneb, scale=1.0)
            O(k, "dve", "tensor_tensor", [b_abt, b_nea], [b_gbst[j]], out=gbst[j][:, 0:8], in0=abt[:, 0:8], in1=nea, op=ALU.mult)
            O(k, "act", "activation", [b_ps], [b_gbst[j]], out=gbst[j][:, 8:16], in_=ps[:, 8:16], func=AF.Sigmoid)
            DMA(k, "sp", [b_gbst[j]], [bd["gb_tok"]], out=d["gb_tok"][r0:r0 + 128, :], in_=gbst[j])
            yield
            for wh, col0 in ((0, 2064), (1, 2576), (2, 3600)):
                ps, b_ps = proj(col0, 512)
                jj = wh % 2
                xi = (sub % 2) * 2 + jj
                x3 = xs[jj]
                O(k, "act", "activation", [b_ps], [b_xs[jj]], out=x3, in_=ps[:, 0:512].rearrange("p (h d) -> p h d", d=64),
                  func=AF.Copy, scale=(0.125 if wh == 0 else 1.0))
                rope(x3, b_xs[jj], xo[xi], b_xo[xi], 8)
                if wh < 2:
                    O(k, "dve", "tensor_tensor", [b_xs[jj]], [b_sq5], out=sq5, in0=x3, in1=x3, op=ALU.mult)
                    O(k, "dve", "tensor_reduce", [b_sq5], [b_n2], out=n2, in_=sq5, axis=AX.X, op=ALU.add)
                    if wh == 0:
                        O(k, "dve", "tensor_scalar", [b_n2], [b_xo[xi]], out=xo[xi][:, :, 64:65], in0=n2.unsqueeze(2), scalar1=-4.0,
                          scalar2=None, op0=ALU.mult)
                    else:
                        O(k, "pool", "memset", [], [b_xo[xi]], ap=xo[xi][:, :, 64:65], constant=1.0)
                        O(k, "dve", "tensor_tensor", [b_n2, k.b_kmax], [k.b_kmax], out=k.kmax, in0=k.kmax, in1=n2, op=ALU.max)
                nr = 65 if wh < 2 else 64
                yield
                pst, b_pst = next_ps(k)
                pstb = pst[0:nr, :].bitcast(BF16).rearrange("p (h t) -> p h t", t=128)
                for h in range(8):
                    O(k, "pe", "transpose", [b_xo[xi], k.b_cbf], [b_pst], out=pstb[:, h, :], in_=xo[xi][:, h, 0:nr], identity=ident_bf)
                stg, bst = ((st_qb, b_stqb), (st_kb, b_stkb), (st_qi, b_stqi))[wh]
                O(k, "act", "activation", [b_pst], [bst], out=stg[:, :, tsl], in_=pstb, func=AF.Copy)
                yield
            ps, b_ps = proj(3088, 512)
            O(k, "act", "activation", [b_ps], [b_vo[j]], out=vo[j][:, :, 0:64], in_=ps[:, 0:512].rearrange("p (h d) -> p h d", d=64),
              func=AF.Copy)
            DMA(k, "sp", [b_vo[j]], [bd["vb_tok"]], out=d["vb_tok"][r0:r0 + 128, :], in_=vo[j].rearrange("p h d -> p (h d)"))
            yield
            ps, b_ps = proj(4112, 72)
            O(k, "act", "activation", [b_ps], [b_kis], out=kis, in_=ps[:, 0:72], func=AF.Copy)
            O(k, "dve", "tensor_tensor", [b_kis], [b_sq5], out=sq5[:, 0, :], in0=kis[:, 0:64], in1=kis[:, 0:64], op=ALU.mult)
            O(k, "dve", "tensor_reduce", [b_sq5], [b_kss], out=kss, in_=sq5[:, 0, :], axis=AX.X, op=ALU.add)
            O(k, "act", "activation", [b_kss, k.b_eps], [b_kss], out=kss, in_=kss, func=AF.Sqrt, bias=k.epsb, scale=1.0 / 64.0)
            O(k, "dve", "reciprocal", [b_kss], [b_kss], out=kss, in_=kss)
            O(k, "dve", "scalar_tensor_tensor", [b_kis, b_kss, k.b_rows], [b_xs[0]], out=xs[0][:, 0, :], in0=kis[:, 0:64], scalar=kss[:, 0:1],
              in1=k.rows[:, R_IKG:R_IKG + 64], op0=ALU.mult, op1=ALU.mult)
            O(k, "dve", "tensor_scalar", [b_kis], [b_wist[j]], out=wist[j], in0=kis[:, 64:72], scalar1=float(0.125 * 8 ** -0.5),
              scalar2=None, op0=ALU.mult)
            DMA(k, "sp", [b_wist[j]], [bd["wi_tok"]], out=d["wi_tok"][r0:r0 + 128, :], in_=wist[j])
            xk = (sub % 2) * 2
            rope(xs[0][:, 0:1, :], b_xs[0], xo[xk][:, 0:1, :], b_xo[xk], 1)
            yield
            pst, b_pst = next_ps(k)
            pstb = pst[0:64, :].bitcast(BF16)
            O(k, "pe", "transpose", [b_xo[xk], k.b_cbf], [b_pst], out=pstb[:, 0:128], in_=xo[xk][:, 0, 0:64], identity=ident_bf)
            O(k, "act", "activation", [b_pst], [b_stki], out=st_ki[:, tsl], in_=pstb[:, 0:128], func=AF.Copy)
            yield

        for pair in range(T // 256):
            gens_ = [subgen(2 * pair), subgen(2 * pair + 1)]
            while gens_:
                for g_ in list(gens_):
                    try:
                        next(g_)
                    except StopIteration:
                        gens_.remove(g_)
        DMA(k, "sp", [b_stqb], [bd["qbT"]], out=d["qbT"][:, :, t0:t0 + T].rearrange("h r t -> r h t"), in_=st_qb)
        DMA(k, "sp", [b_stkb], [bd["kbT"]], out=d["kbT"][:, :, t0:t0 + T].rearrange("h r t -> r h t"), in_=st_kb)
        DMA(k, "sp", [b_stqi], [bd["qiT"]], out=d["qiT"][:, :, t0:t0 + T].rearrange("h r t -> r h t"), in_=st_qi)
        DMA(k, "sp", [b_stki], [bd["kiT"]], out=d["kiT"][:, t0:t0 + T], in_=st_ki)


def pipeline(n_items, pre_fn, seq_fn, nset, max_pre=2):
    pre_done = [False] * n_items
    seq_done = [0]
    active = []
    nxt = [0]

    def seq_all():
        for n in range(n_items):
            while not pre_done[n]:
                yield
            for _ in seq_fn(n):
                yield
            seq_done[0] = n + 1
            yield

    sg = seq_all()
    alive = True
    while alive or active:
        while nxt[0] < n_items and len(active) < max_pre and nxt[0] < seq_done[0] + nset:
            active.append((nxt[0], pre_fn(nxt[0])))
            nxt[0] += 1
        for item in list(active):
            n, g = item
            try:
                next(g)
            except StopIteration:
                pre_done[n] = True
                active.remove(item)
        if alive:
            try:
                next(sg)
            except StopIteration:
                alive = False


def pipeline3(n_items, fA, fB, fC, nbuf=2):
    done = [0, 0, 0]
    nxt = [0, 0, 0]
    gens = [None, None, None]
    fs = [fA, fB, fC]

    def can_start(si, i):
        if i >= n_items:
            return False
        if si == 0:
            return i < done[1] + nbuf
        if si == 1:
            return done[0] > i and i < done[2] + nbuf
        return done[1] > i

    while done[2] < n_items:
        progressed = False
        for si in range(3):
            if gens[si] is None and can_start(si, nxt[si]):
                gens[si] = fs[si](nxt[si])
                nxt[si] += 1
            if gens[si] is not None:
                progressed = True
                try:
                    next(gens[si])
                except StopIteration:
                    gens[si] = None
                    done[si] += 1
        assert progressed, "pipeline3 deadlock"


def gdn_pass(k):
    P = k.P
    B = P.buf
    d = k.d
    bd = k.bd
    cv = Carver(k.big, k.PH0, k.PHLIM)
    NSET = 3
    H8 = [64, 8, 64]
    gb_all = cv.t([64, 64, 16], F32)
    gcs = {nm: cv.t([64, 64, 8], F32) for nm in ("gc", "gl", "egc", "ekd", "gtot", "beg")}
    negU = cv.t([64, 64], F32)
    grp = [{nm: cv.t([64, 8, 512], BF16) for nm in ("qT", "kT", "ktok", "vtok")} for _ in range(2)]
    zt = [cv.t([64, 8, 64], F32) for _ in range(3)]
    sets = []
    for i in range(NSET):
        s_ = {nm: cv.t(H8, F32) for nm in ("Gb", "Gu", "E", "EL", "EU", "u")}
        s_.update({nm: cv.t(H8, BF16) for nm in ("X0", "X1", "Y0", "Y1", "Pm", "vb", "kbe", "kd", "wT", "qkT")})
        sets.append(s_)
    sq_ = []
    for i in range(2):
        s_ = {nm: cv.t(H8, F32) for nm in ("St", "oa", "o", "sq", "on")}
        s_.update({nm: cv.t(H8, BF16) for nm in ("vnew", "onb")})
        s_["ss"] = cv.t([64, 8], F32)
        sq_.append(s_)
    Sst = cv.t(H8, F32)
    Sb = cv.t(H8, BF16)
    oaT_st = [cv.t([128, 4, 512], BF16) for _ in range(2)]

    b_gb = B("g_gball", True)
    b_gcs = {nm: B("g_" + nm) for nm in gcs}
    b_negU = B("g_negU")
    b_grp = [{nm: B("g_grp%d_%s" % (i, nm), True) for nm in grp[i]} for i in range(2)]
    b_zt = [B("g_zt%d" % i, True) for i in range(3)]
    b_sets = [{nm: B("g_s%d_%s" % (i, nm)) for nm in sets[i]} for i in range(NSET)]
    b_sq = [{nm: B("g_q%d_%s" % (i, nm)) for nm in sq_[i]} for i in range(2)]
    b_S = B("g_S")
    b_Sb = B("g_Sb")
    b_oast = [B("g_oast%d" % i) for i in range(2)]

    U64 = k.c32[0:64, C_U64:C_U64 + 64]
    ONES64 = k.c32[0:64, C_ONES64:C_ONES64 + 64]
    SLb = k.c32[0:64, C_SL:C_SL + 64].unsqueeze(1).to_broadcast(H8)
    UIb = k.c32[0:64, C_UI:C_UI + 64].unsqueeze(1).to_broadcast(H8)
    Ib = k.cbf[0:64, C_IDENT:C_IDENT + 64].unsqueeze(1).to_broadcast(H8)
    id64 = k.cbf[0:64, C_IDENT:C_IDENT + 64]
    dngb = k.rows[0:64, R_DNG:R_DNG + 64].unsqueeze(1).to_broadcast(H8)

    def fl(v):
        return v.rearrange("p h d -> p (h d)")

    def bj(v2):
        return v2.unsqueeze(2).to_broadcast(H8)

    for q8 in range(8):
        DMA(k, "sp", [bd["gb_tok"]], [b_gb], out=gb_all[:, q8 * 8:(q8 + 1) * 8, :],
            in_=d["gb_tok"][q8 * 512:(q8 + 1) * 512, :].rearrange("(n c) j -> c n j", c=64))
    O(k, "dve", "tensor_scalar", [k.b_c], [b_negU], out=negU, in0=U64, scalar1=-1.0, scalar2=None, op0=ALU.mult)
    g_all = gb_all[:, :, 0:8]
    beta_all = gb_all[:, :, 8:16]
    ps, b_ps = next_ps(k)
    O(k, "pe", "matmul", [k.b_c, b_gb], [b_ps], out=ps[0:64, :].rearrange("p (n h) -> p n h", h=8), lhsT=U64, rhs=g_all,
      start=True, stop=True)
    O(k, "act", "activation", [b_ps], [b_gcs["gc"]], out=fl(gcs["gc"]), in_=ps[0:64, :], func=AF.Copy)
    ps, b_ps = next_ps(k)
    O(k, "pe", "matmul", [k.b_c, b_gb], [b_ps], out=ps[0:64, :].rearrange("p (n h) -> p n h", h=8), lhsT=ONES64, rhs=g_all,
      start=True, stop=True)
    O(k, "act", "activation", [b_ps], [b_gcs["gl"]], out=fl(gcs["gl"]), in_=ps[0:64, :], func=AF.Copy)
    O(k, "act", "activation", [b_gcs["gc"]], [b_gcs["egc"]], out=fl(gcs["egc"]), in_=fl(gcs["gc"]), func=AF.Exp)
    O(k, "act", "activation", [b_gcs["gl"]], [b_gcs["gtot"]], out=fl(gcs["gtot"]), in_=fl(gcs["gl"]), func=AF.Exp)
    O(k, "dve", "tensor_tensor", [b_gcs["gl"], b_gcs["gc"]], [b_gcs["ekd"]], out=fl(gcs["ekd"]), in0=fl(gcs["gl"]), in1=fl(gcs["gc"]),
      op=ALU.subtract)
    O(k, "act", "activation", [b_gcs["ekd"]], [b_gcs["ekd"]], out=fl(gcs["ekd"]), in_=fl(gcs["ekd"]), func=AF.Exp)
    O(k, "dve", "tensor_tensor", [b_gcs["egc"], b_gb], [b_gcs["beg"]], out=gcs["beg"], in0=gcs["egc"], in1=beta_all, op=ALU.mult)
    O(k, "dve", "memset", [], [b_S], ap=Sst, constant=0.0)
    O(k, "dve", "memset", [], [b_Sb], ap=Sb, constant=0.0)

    def mm8(ps, b_ps, lhs, b_lhs, rhs, b_rhs, lsl=None, rsl=None):
        for h in range(8):
            l_ = lhs[:, h, lsl] if lsl is not None else lhs[:, h, :]
            r_ = rhs[:, h, rsl] if rsl is not None else rhs[:, h, :]
            O(k, "pe", "matmul", [b_lhs, b_rhs], [b_ps], out=ps[0:64, h * 64:(h + 1) * 64], lhsT=l_, rhs=r_, start=True, stop=True)

    def load_group(g):
        gi = g % 2
        tsl = slice(g * 512, (g + 1) * 512)
        DMA(k, "sp", [bd["qaT"]], [b_grp[gi]["qT"]], out=grp[gi]["qT"], in_=d["qaT"].rearrange("(h r) t -> r h t", r=64)[:, :, tsl])
        DMA(k, "sp", [bd["kaT"]], [b_grp[gi]["kT"]], out=grp[gi]["kT"], in_=d["kaT"].rearrange("(h r) t -> r h t", r=64)[:, :, tsl])
        DMA(k, "sp", [bd["ka_tok"]], [b_grp[gi]["ktok"]], out=grp[gi]["ktok"],
            in_=d["ka_tok"][tsl, :].rearrange("(n c) f -> c n f", c=64))
        DMA(k, "sp", [bd["va_tok"]], [b_grp[gi]["vtok"]], out=grp[gi]["vtok"],
            in_=d["va_tok"][tsl, :].rearrange("(n c) f -> c n f", c=64))

    def pre(n):
        g = n // 8
        ci = n % 8
        gi = g % 2
        if ci == 0:
            load_group(g)
        G = grp[gi]
        bG = b_grp[gi]
        cs_ = slice(ci * 64, (ci + 1) * 64)
        s = sets[n % NSET]
        bs = b_sets[n % NSET]
        g_n = gb_all[:, n, 0:8]
        beta_n = gb_all[:, n, 8:16]
        ktok_n = G["ktok"][:, ci, :].rearrange("p (h d) -> p h d", d=64)
        vtok_n = G["vtok"][:, ci, :].rearrange("p (h d) -> p h d", d=64)
        O(k, "dve", "tensor_copy", [b_gb], [bs["Gb"]], out=s["Gb"], in_=bj(g_n))
        O(k, "pool", "tensor_tensor", [b_gb, b_negU], [bs["Gu"]], out=s["Gu"], in0=bj(g_n), in1=negU.unsqueeze(1).to_broadcast(H8),
          op=ALU.mult)
        O(k, "pool", "tensor_tensor", [bG["vtok"], b_gb], [bs["vb"]], out=s["vb"], in0=vtok_n, in1=bj(beta_n), op=ALU.mult)
        O(k, "pool", "tensor_tensor", [bG["ktok"], b_gcs["beg"]], [bs["kbe"]], out=s["kbe"], in0=ktok_n, in1=bj(gcs["beg"][:, n, :]),
          op=ALU.mult)
        O(k, "pool", "tensor_tensor", [bG["ktok"], b_gcs["ekd"]], [bs["kd"]], out=s["kd"], in0=ktok_n, in1=bj(gcs["ekd"][:, n, :]),
          op=ALU.mult)
        yield
        psd, b_psd = next_ps(k, True)
        O(k, "pe", "matmul", [k.b_c, bs["Gb"]], [b_psd], out=psd[0:64, :], lhsT=U64, rhs=fl(s["Gb"]), start=True, stop=False)
        O(k, "pe", "matmul", [k.b_c, bs["Gu"]], [b_psd], out=psd[0:64, :], lhsT=ONES64, rhs=fl(s["Gu"]), start=False, stop=True)
        yield
        O(k, "act", "activation", [b_psd], [bs["E"]], out=fl(s["E"]), in_=psd[0:64, :], func=AF.Abs)
        rel_ps(k, b_psd)
        O(k, "act", "activation", [bs["E"]], [bs["E"]], out=fl(s["E"]), in_=fl(s["E"]), func=AF.Exp, scale=-1.0)
        O(k, "pool", "tensor_tensor", [bs["E"], k.b_c], [bs["EU"]], out=s["EU"], in0=s["E"], in1=UIb, op=ALU.mult)
        O(k, "dve", "tensor_tensor", [bs["E"], k.b_c], [bs["EL"]], out=s["EL"], in0=s["E"], in1=SLb, op=ALU.mult)
        O(k, "dve", "tensor_tensor", [bs["EL"], b_gb], [bs["EL"]], out=s["EL"], in0=s["EL"], in1=bj(beta_n), op=ALU.mult)
        pkk, b_pkk = next_ps(k, True)
        mm8(pkk, b_pkk, G["kT"], bG["kT"], G["kT"], bG["kT"], cs_, cs_)
        pqk, b_pqk = next_ps(k, True)
        mm8(pqk, b_pqk, G["kT"], bG["kT"], G["qT"], bG["qT"], cs_, cs_)
        yield
        O(k, "dve", "scalar_tensor_tensor", [b_pkk, bs["EL"]], [bs["X0"]], out=fl(s["X0"]), in0=pkk[0:64, :], scalar=-1.0, in1=fl(s["EL"]),
          op0=ALU.mult, op1=ALU.mult)
        rel_ps(k, b_pkk)
        O(k, "dve", "tensor_tensor", [b_pqk, bs["EU"]], [bs["qkT"]], out=fl(s["qkT"]), in0=pqk[0:64, :], in1=fl(s["EU"]), op=ALU.mult)
        rel_ps(k, b_pqk)
        yield
        pt, b_pt = next_ps(k, True)
        ptb = pt[0:64, :].bitcast(BF16)
        for h in range(8):
            O(k, "pe", "transpose", [bs["X0"], k.b_cbf], [b_pt], out=ptb[:, h * 64:(h + 1) * 64], in_=s["X0"][:, h, :], identity=id64)
        yield
        v6 = "ab"
        if "a" in v6:
            O(k, "act", "activation", [b_pt], [bs["Y0"]], out=fl(s["Y0"]), in_=ptb[:, 0:512], func=AF.Copy)
        if "b" in v6:
            O(k, "dve", "tensor_tensor", [b_pt, k.b_cbf], [bs["Pm"]], out=s["Pm"], in0=ptb[:, 0:512].rearrange("p (h d) -> p h d", d=64), in1=Ib,
              op=ALU.add)
        if "c" in v6:
            O(k, "dve", "tensor_tensor", [bs["Y0"], k.b_cbf], [bs["Pm"]], out=s["Pm"], in0=s["Y0"], in1=Ib, op=ALU.add)
        rel_ps(k, b_pt)
        yield
        X, Y = "X0", "Y0"
        for lv in range(5):
            Xn = "X1" if X == "X0" else "X0"
            Yn = "Y1" if Y == "Y0" else "Y0"
            px, b_px = next_ps(k, True)
            mm8(px, b_px, s[Y], bs[Y], s[X], bs[X])
            if lv < 4:
                py, b_py = next_ps(k, True)
                mm8(py, b_py, s[X], bs[X], s[Y], bs[Y])
            yield
            O(k, "act", "activation", [b_px], [bs[Xn]], out=fl(s[Xn]), in_=px[0:64, :], func=AF.Copy)
            rel_ps(k, b_px)
            if lv < 4:
                O(k, "dve", "tensor_copy", [b_py], [bs[Yn]], out=fl(s[Yn]), in_=py[0:64, :])
                rel_ps(k, b_py)
            yield
            pp, b_pp = next_ps(k, True)
            mm8(pp, b_pp, s[Xn], bs[Xn], s["Pm"], bs["Pm"])
            yield
            O(k, "dve", "tensor_tensor", [b_pp, bs["Pm"]], [bs["Pm"]], out=fl(s["Pm"]), in0=pp[0:64, :], in1=fl(s["Pm"]), op=ALU.add)
            rel_ps(k, b_pp)
            yield
            X, Y = Xn, Yn
        pu, b_pu = next_ps(k, True)
        mm8(pu, b_pu, s["Pm"], bs["Pm"], s["vb"], bs["vb"])
        pw, b_pw = next_ps(k, True)
        mm8(pw, b_pw, s["kbe"], bs["kbe"], s["Pm"], bs["Pm"])
        yield
        O(k, "act", "activation", [b_pu], [bs["u"]], out=fl(s["u"]), in_=pu[0:64, :], func=AF.Copy)
        rel_ps(k, b_pu)
        O(k, "act", "activation", [b_pw], [bs["wT"]], out=fl(s["wT"]), in_=pw[0:64, :], func=AF.Copy)
        rel_ps(k, b_pw)
        yield

    def seq(n):
        g = n // 8
        ci = n % 8
        gi = g % 2
        G = grp[gi]
        bG = b_grp[gi]
        cs_ = slice(ci * 64, (ci + 1) * 64)
        s = sets[n % NSET]
        bs = b_sets[n % NSET]
        q = sq_[n % 2]
        bq = b_sq[n % 2]
        z = zt[n % 3]
        bz = b_zt[n % 3]
        DMA(k, "sp", [bd["z_tok"]], [bz], out=fl(z), in_=d["z_tok"][n * 64:(n + 1) * 64, :])
        pws, b_pws = next_ps(k, True)
        mm8(pws, b_pws, s["wT"], bs["wT"], Sb, b_Sb)
        po1, b_po1 = next_ps(k, True)
        mm8(po1, b_po1, G["qT"], bG["qT"], Sb, b_Sb, cs_, None)
        O(k, "pool", "tensor_tensor", [b_S, b_gcs["gtot"]], [bq["St"]], out=q["St"], in0=Sst, in1=bj(gcs["gtot"][:, n, :]), op=ALU.mult)
        yield
        O(k, "dve", "tensor_tensor", [bs["u"], b_pws], [bq["vnew"]], out=fl(q["vnew"]), in0=fl(s["u"]), in1=pws[0:64, :], op=ALU.subtract)
        rel_ps(k, b_pws)
        O(k, "dve", "tensor_tensor", [b_po1, b_gcs["egc"]], [bq["oa"]], out=q["oa"], in0=po1[0:64, :].rearrange("p (h d) -> p h d", d=64),
          in1=bj(gcs["egc"][:, n, :]), op=ALU.mult)
        rel_ps(k, b_po1)
        yield
        pkv, b_pkv = next_ps(k, True)
        mm8(pkv, b_pkv, s["kd"], bs["kd"], q["vnew"], bq["vnew"])
        po2, b_po2 = next_ps(k, True)
        mm8(po2, b_po2, s["qkT"], bs["qkT"], q["vnew"], bq["vnew"])
        yield
        O(k, "dve", "tensor_tensor", [bq["St"], b_pkv], [b_S], out=fl(Sst), in0=fl(q["St"]), in1=pkv[0:64, :], op=ALU.add)
        rel_ps(k, b_pkv)
        O(k, "act", "activation", [b_S], [b_Sb], out=fl(Sb), in_=fl(Sst), func=AF.Copy)
        O(k, "dve", "tensor_tensor", [bq["oa"], b_po2], [bq["o"]], out=fl(q["o"]), in0=fl(q["oa"]), in1=po2[0:64, :], op=ALU.add)
        rel_ps(k, b_po2)
        yield
        O(k, "pool", "tensor_tensor", [bq["o"]], [bq["sq"]], out=q["sq"], in0=q["o"], in1=q["o"], op=ALU.mult)
        O(k, "dve", "tensor_reduce", [bq["sq"]], [bq["ss"]], out=q["ss"], in_=q["sq"], axis=AX.X, op=ALU.add)
        O(k, "act", "activation", [bq["ss"], k.b_eps], [bq["ss"]], out=q["ss"], in_=q["ss"], func=AF.Sqrt, bias=k.epsb[0:64, :], scale=1.0 / 64.0)
        O(k, "dve", "reciprocal", [bq["ss"]], [bq["ss"]], out=q["ss"], in_=q["ss"])
        yield
        O(k, "dve", "tensor_tensor", [bq["o"], bq["ss"]], [bq["on"]], out=q["on"], in0=q["o"], in1=bj(q["ss"]), op=ALU.mult)
        O(k, "pool", "tensor_tensor", [bq["on"], k.b_rows], [bq["on"]], out=q["on"], in0=q["on"], in1=dngb, op=ALU.mult)
        O(k, "pool", "tensor_tensor", [bq["on"], bz], [bq["onb"]], out=q["onb"], in0=q["on"], in1=z, op=ALU.mult)
        yield
        pt, b_pt = next_ps(k, True)
        ptb = pt[:, :].bitcast(BF16)
        onf = fl(q["onb"])
        for c4 in range(4):
            O(k, "pe", "transpose", [bq["onb"], k.b_cbf], [b_pt], out=ptb[:, c4 * 64:(c4 + 1) * 64], in_=onf[:, c4 * 128:(c4 + 1) * 128],
              identity=id64)
        yield
        O(k, "act", "activation", [b_pt], [b_oast[gi]], out=oaT_st[gi][:, :, cs_], in_=ptb[:, 0:256].rearrange("p (c t) -> p c t", t=64),
          func=AF.Copy)
        rel_ps(k, b_pt)
        if ci == 7:
            DMA(k, "sp", [b_oast[gi]], [bd["oaT"]], out=d["oaT"][:, g * 512:(g + 1) * 512].rearrange("(c p) t -> p c t", p=128),
                in_=oaT_st[gi])
        yield

    if k.gdn_stages is not None:
        def pre_lim(n):
            for i, _ in enumerate(pre(n)):
                if i + 1 >= k.gdn_stages:
                    k.ps_held.clear()
                    return
                yield

        def seq_none(n):
            return
            yield
        pipeline(k.gdn_n, pre_lim, seq_none, NSET)
    else:
        pipeline(k.gdn_n, pre, seq, NSET)


NIT = 20
TOPK = 256


def dsa_pass(k):
    P = k.P
    B = P.buf
    d = k.d
    bd = k.bd
    cv = Carver(k.big, k.PH0, k.PHLIM)
    kiT = cv.t([64, S], BF16)
    kbT = cv.t([65, 8, S], BF16)
    vb = cv.t([128, 32, 520], BF16)
    cbias = cv.t([128, 8], F32)
    km8 = cv.t([8, 1], F32)
    dg = cv.t([8, 8], F32)
    sc = [cv.t([128, S], F32) for _ in range(2)]
    m01 = cv.t([128, S], BF16)
    junk = m01
    junk2 = cv.t([128, S], BF16)
    obu = [cv.t([128, 8, 65], F32) for _ in range(2)]
    rc8 = cv.t([128, 8, 1], F32)
    maskT = [cv.t([128, 32, 128], BF16) for _ in range(2)]
    qiq = [cv.t([64, 8, 128], BF16) for _ in range(2)]
    qbq = [cv.t([65, 8, 128], BF16) for _ in range(2)]
    wiq = [cv.t([128, 8], F32) for _ in range(2)]
    rl = [cv.t([128, 512], F32) for _ in range(2)]
    ex = [cv.t([128, 512], BF16) for _ in range(3)]
    pm = [cv.t([128, 512], BF16) for _ in range(3)]
    bis = [{nm: cv.t([128, 1], F32) for nm in ("hi", "lo", "rng", "c1", "s2", "s3", "b1", "nb2", "nb")} for _ in range(2)]
    t3v = [cv.t([128, 3], F32) for _ in range(2)]
    b_lo = [B("a_lo%d" % i) for i in range(2)]
    b_t3 = [B("a_t3%d" % i) for i in range(2)]
    b_s2 = [B("a_s2%d" % i) for i in range(2)]
    b_s3 = [B("a_s3%d" % i) for i in range(2)]
    Wt = [cv.t([128, 32], F32) for _ in range(2)]
    ob = [cv.t([128, 512], BF16) for _ in range(2)]
    rc = [cv.t([128, 1], F32) for _ in range(2)]
    obst = [cv.t([128, 4, 128], BF16) for _ in range(2)]

    b_kiT = B("a_kiT", True)
    b_kbT = [B("a_kbT%d" % h, True) for h in range(8)]
    b_vb = B("a_vb", True)
    b_cbias = B("a_cbias")
    b_km8 = B("a_km8")
    b_dg = B("a_dg")
    b_sc = [B("a_sc%d" % i) for i in range(2)]
    b_m01 = B("a_m01")
    b_junk = b_m01
    b_junk2 = B("a_junk2")
    b_obu = [B("a_obu%d" % i) for i in range(2)]
    b_rc8 = B("a_rc8")
    b_mid = [B("a_mid%d" % i) for i in range(2)]
    b_ssg = [B("a_ssg%d" % i) for i in range(2)]
    b_maskT = [B("a_maskT%d" % i) for i in range(2)]
    b_qiq = [B("a_qiq%d" % i, True) for i in range(2)]
    b_qbq = [B("a_qbq%d" % i, True) for i in range(2)]
    b_wiq = [B("a_wiq%d" % i, True) for i in range(2)]
    b_rl = [B("a_rl%d" % i) for i in range(2)]
    b_ex = [B("a_ex%d" % i) for i in range(3)]
    b_pm = [B("a_pm%d" % i) for i in range(3)]
    b_bis = [B("a_bis%d" % i) for i in range(2)]
    b_W = [B("a_W%d" % i) for i in range(2)]
    b_ob = [B("a_ob%d" % i) for i in range(2)]
    b_rc = [B("a_rc%d" % i) for i in range(2)]
    b_obst = [B("a_obst%d" % i) for i in range(2)]

    ident_bf = k.cbf[:, C_IDENT:C_IDENT + 128]
    ident_f = k.c32[:, C_IDENT:C_IDENT + 128]

    DMA(k, "sp", [bd["kiT"]], [b_kiT], out=kiT, in_=d["kiT"])
    for h in range(8):
        DMA(k, "sp" if h % 2 == 0 else "act", [bd["kbT"]], [b_kbT[h]], out=kbT[:, h, :], in_=d["kbT"][h])
    for q4 in range(4):
        DMA(k, "sp", [bd["vb_tok"]], [b_vb], out=vb[:, q4 * 8:(q4 + 1) * 8, :],
            in_=d["vb_tok"][q4 * 1024:(q4 + 1) * 1024, :].rearrange("(kt p) f -> p kt f", p=128))
    ps, b_ps = next_ps(k)
    O(k, "pe", "transpose", [k.b_kmax, k.b_c], [b_ps], out=ps[0:8, 0:128], in_=k.kmax, identity=ident_f)
    O(k, "dve", "tensor_reduce", [b_ps], [b_km8], out=km8, in_=ps[0:8, 0:128], axis=AX.X, op=ALU.max)
    O(k, "dve", "tensor_scalar", [k.b_c, b_km8], [b_dg], out=dg, in0=k.c32[0:8, C_IDENT:C_IDENT + 8], scalar1=km8[:, 0:1], scalar2=None,
      op0=ALU.mult)
    ps, b_ps = next_ps(k)
    O(k, "pe", "matmul", [k.b_c, b_dg], [b_ps], out=ps[:, 0:8], lhsT=k.c32[0:8, C_ONESD:C_ONESD + 128], rhs=dg, start=True, stop=True)
    O(k, "dve", "tensor_scalar", [b_ps], [b_cbias], out=cbias, in0=ps[:, 0:8], scalar1=-64.0, scalar2=None, op0=ALU.mult)

    k.ps_pool = [0, 1, 2, 3, 4, 5]
    k.ps_i = 0
    NQB = S // 128

    def idx(qb):
        par = qb % 2
        t0 = qb * 128
        nk = qb + 1
        N = nk * 128
        scp = sc[par]
        bsc = b_sc[par]
        bb = bis[par]
        bbis = b_bis[par]
        W = Wt[par]
        DMA(k, "sp", [bd["qiT"]], [b_qiq[par]], out=qiq[par], in_=d["qiT"][:, :, t0:t0 + 128].rearrange("h r t -> r h t"))
        DMA(k, "sp", [bd["wi_tok"]], [b_wiq[par]], out=wiq[par], in_=d["wi_tok"][t0:t0 + 128, :])
        for kg in range((N + 511) // 512):
            n = min(512, N - kg * 512)
            ksl = slice(kg * 512, kg * 512 + n)
            for h in range(8):
                ps, b_ps = next_ps(k)
                O(k, "pe", "matmul", [b_qiq[par], b_kiT], [b_ps], out=ps[:, 0:n], lhsT=qiq[par][:, h, :], rhs=kiT[:, ksl], start=True, stop=True)
                r = rl[h % 2]
                br = b_rl[h % 2]
                O(k, "act", "activation", [b_ps], [br], out=r[:, 0:n], in_=ps[:, 0:n], func=AF.Relu)
                if h == 0:
                    O(k, "dve", "tensor_scalar", [br, b_wiq[par]], [bsc], out=scp[:, ksl], in0=r[:, 0:n], scalar1=wiq[par][:, 0:1], scalar2=None,
                      op0=ALU.mult)
                else:
                    O(k, "dve", "scalar_tensor_tensor", [br, b_wiq[par], bsc], [bsc], out=scp[:, ksl], in0=r[:, 0:n], scalar=wiq[par][:, h:h + 1],
                      in1=scp[:, ksl], op0=ALU.mult, op1=ALU.add)
            yield
        dsl = slice(qb * 128, (qb + 1) * 128)
        O(k, "dve", "tensor_tensor", [bsc, k.b_c], [bsc], out=scp[:, dsl], in0=scp[:, dsl], in1=k.c32[:, C_NEGC:C_NEGC + 128], op=ALU.add)
        yield

    def idxB(qb):
        par = qb % 2
        t0 = qb * 128
        nk = qb + 1
        N = nk * 128
        scp = sc[par]
        bsc = b_sc[par]
        bb = bis[par]
        bbis = b_bis[par]
        W = Wt[par]
        DMA(k, "sp", [bd["qbT"]], [b_qbq[par]], out=qbq[par], in_=d["qbT"][:, :, t0:t0 + 128].rearrange("h r t -> r h t"))
        if qb >= 2:
            O(k, "dve", "tensor_reduce", [bsc], [bbis], out=bb["hi"], in_=scp[:, 0:N], axis=AX.X, op=ALU.max)
            O(k, "dve", "tensor_reduce", [bsc], [b_lo[par]], out=bb["lo"], in_=scp[:, 0:qb * 128], axis=AX.X, op=ALU.min)
            O(k, "dve", "tensor_tensor", [bbis, b_lo[par]], [bbis], out=bb["rng"], in0=bb["hi"], in1=bb["lo"], op=ALU.subtract)
            O(k, "dve", "tensor_scalar", [k.b_c, bbis], [b_W[par]], out=W[:, 0:16], in0=k.c32[:, C_POW4:C_POW4 + 16], scalar1=bb["rng"][:, 0:1],
              scalar2=None, op0=ALU.mult)
            yield
            cK = float(N - 2 * TOPK)
            def emit_t3(i):
                O(k, "dve", "scalar_tensor_tensor", [k.b_c, b_W[par], b_lo[par]], [b_t3[par]], out=t3v[par], in0=k.c32[:, C_123:C_123 + 3],
                  scalar=W[:, i:i + 1], in1=bb["lo"].to_broadcast([128, 3]), op0=ALU.mult, op1=ALU.add)

            emit_t3(0)
            yield
            for i in range(NIT // 2):
                O(k, "dve", "tensor_scalar", [bsc, b_t3[par]], [b_junk, bbis], out=junk[:, 0:N], in0=scp[:, 0:N], scalar1=t3v[par][:, 0:1],
                  scalar2=None, op0=ALU.is_ge, op1=ALU.add, accum_out=bb["c1"])
                O(k, "act", "activation", [bsc, b_t3[par]], [b_s2[par]], out=junk2[:, 0:N], in_=scp[:, 0:N], func=AF.Sign,
                  bias=t3v[par][:, 1:2], scale=-1.0, accum_out=bb["s2"])
                O(k, "act", "activation", [bsc, b_t3[par]], [b_s3[par]], out=junk2[:, 0:N], in_=scp[:, 0:N], func=AF.Sign,
                  bias=t3v[par][:, 2:3], scale=-1.0, accum_out=bb["s3"])
                yield
                O(k, "dve", "tensor_scalar", [bbis], [bbis], out=bb["b1"], in0=bb["c1"], scalar1=float(TOPK), scalar2=None, op0=ALU.is_ge)
                O(k, "dve", "scalar_tensor_tensor", [b_s2[par], bbis], [bbis], out=bb["nb2"], in0=bb["s2"], scalar=cK, in1=bb["b1"],
                  op0=ALU.is_le, op1=ALU.add)
                O(k, "dve", "scalar_tensor_tensor", [b_s3[par], bbis], [bbis], out=bb["nb"], in0=bb["s3"], scalar=cK, in1=bb["nb2"],
                  op0=ALU.is_le, op1=ALU.add)
                O(k, "dve", "scalar_tensor_tensor", [bbis, b_W[par], b_lo[par]], [b_lo[par]], out=bb["lo"], in0=bb["nb"], scalar=W[:, i:i + 1],
                  in1=bb["lo"], op0=ALU.mult, op1=ALU.add)
                if i + 1 < NIT // 2:
                    emit_t3(i + 1)
                yield
            O(k, "dve", "tensor_scalar", [bsc, b_lo[par]], [b_m01], out=m01[:, 0:N], in0=scp[:, 0:N], scalar1=bb["lo"][:, 0:1], scalar2=None,
              op0=ALU.is_ge)
        else:
            O(k, "dve", "tensor_scalar", [bsc], [b_m01], out=m01[:, 0:N], in0=scp[:, 0:N], scalar1=-1e29, scalar2=None, op0=ALU.is_ge)
        yield
        for k8 in range((nk + 7) // 8):
            kts = list(range(k8 * 8, min(nk, k8 * 8 + 8)))
            ps, b_ps = next_ps(k)
            psb = ps[:, :].bitcast(BF16)
            for j, kt in enumerate(kts):
                O(k, "pe", "transpose", [b_m01, k.b_cbf], [b_ps], out=psb[:, j * 128:(j + 1) * 128], in_=m01[:, kt * 128:(kt + 1) * 128],
                  identity=ident_bf)
            O(k, "act", "activation", [b_ps], [b_maskT[par]], out=maskT[par][:, kts[0]:kts[-1] + 1, :],
              in_=psb[:, 0:len(kts) * 128].rearrange("p (a t) -> p a t", t=128), func=AF.Copy)
            yield

    ctr = [0]

    def att(qb):
        par = qb % 2
        t0 = qb * 128
        nk = qb + 1
        ngr = (nk + 3) // 4
        groups = [(h, kg) for h in range(8) for kg in range(ngr)]

        def emit_qk(h, kg):
            kts = list(range(kg * 4, min(nk, kg * 4 + 4)))
            ps, b_ps = next_ps(k, True)
            for j, kt in enumerate(kts):
                O(k, "pe", "matmul", [b_kbT[h], b_qbq[par]], [b_ps], out=ps[:, j * 128:(j + 1) * 128], lhsT=kbT[:, h, kt * 128:(kt + 1) * 128],
                  rhs=qbq[par][:, h, :], start=True, stop=True)
            return ps, b_ps, kts

        cur = emit_qk(*groups[0])
        for gi, (h, kg) in enumerate(groups):
            nxt = emit_qk(*groups[gi + 1]) if gi + 1 < len(groups) else None
            ps, b_ps, kts = cur
            n = len(kts) * 128
            po = k.ps[6 + h % 2]
            b_po = k.b_ps[6 + h % 2]
            i3 = ctr[0] % 3
            ctr[0] += 1
            O(k, "act", "activation", [b_ps, b_cbias], [b_ex[i3]], out=ex[i3][:, 0:n], in_=ps[:, 0:n], func=AF.Exp, bias=cbias[:, h:h + 1],
              scale=1.0)
            rel_ps(k, b_ps)
            O(k, "pool", "tensor_tensor", [b_ex[i3], b_maskT[par]], [b_pm[i3]], out=pm[i3][:, 0:n], in0=ex[i3][:, 0:n],
              in1=maskT[par][:, kts[0]:kts[-1] + 1, :].rearrange("p a t -> p (a t)"), op=ALU.mult)
            for j, kt in enumerate(kts):
                O(k, "pe", "matmul", [b_pm[i3], b_vb], [b_po], out=po[:, 0:65], lhsT=pm[i3][:, j * 128:(j + 1) * 128],
                  rhs=vb[:, kt, h * 65:(h + 1) * 65], start=(kt == 0), stop=(kt == nk - 1))
            if kg == ngr - 1:
                O(k, "act", "activation", [b_po], [b_obu[par]], out=obu[par][:, h, :], in_=po[:, 0:65], func=AF.Copy)
            cur = nxt
            yield
        O(k, "dve", "reciprocal", [b_obu[par]], [b_rc8], out=rc8, in_=obu[par][:, :, 64:65])
        O(k, "dve", "tensor_tensor", [b_obu[par], b_rc8], [b_ob[par]], out=ob[par].rearrange("p (h d) -> p h d", d=64), in0=obu[par][:, :, 0:64],
          in1=rc8.to_broadcast([128, 8, 64]), op=ALU.mult)
        ps, b_ps = next_ps(k)
        psb = ps[:, :].bitcast(BF16)
        for c4 in range(4):
            O(k, "pe", "transpose", [b_ob[par], k.b_cbf], [b_ps], out=psb[:, c4 * 128:(c4 + 1) * 128], in_=ob[par][:, c4 * 128:(c4 + 1) * 128],
              identity=ident_bf)
        O(k, "act", "activation", [b_ps], [b_obst[par]], out=obst[par], in_=psb[:, 0:512].rearrange("p (c t) -> p c t", t=128), func=AF.Copy)
        DMA(k, "sp", [b_obst[par]], [bd["obT"]], out=d["obT"][:, t0:t0 + 128].rearrange("(c p) t -> p c t", p=128), in_=obst[par])
        yield

    pipeline3(NQB, idx, idxB, att, 2)
    k.ps_pool = list(range(8))
    k.ps_i = 0


def merge_pass(k):
    P = k.P
    B = P.buf
    d = k.d
    bd = k.bd
    T = 512
    cv = Carver(k.big, k.PH0, k.PHLIM)
    w2 = cv.t([128, 8, 2048], BF16)
    wa = cv.t([128, 4, D], BF16)
    wb = cv.t([128, 4, D], BF16)
    wo = cv.t([128, 8, D], BF16)
    hT = cv.t([128, 8, T], F32)
    su = cv.t([128, 8, T], BF16)
    rstd = cv.t([128, T], F32)
    oa = cv.t([128, 4, T], BF16)
    obt = cv.t([128, 4, T], BF16)
    sga = [cv.t([128, T], F32) for _ in range(2)]
    sgb = [cv.t([128, T], F32) for _ in range(2)]
    mg = cv.t([128, 8, T], BF16)
    y = cv.t([128, 8, T], F32)
    b_w2 = [B("m_w2_%d" % c, True) for c in range(8)]
    b_wa = B("m_wa", True)
    b_wb = B("m_wb", True)
    b_wo = B("m_wo", True)
    b_h = B("m_h", True)
    b_su = B("m_su")
    b_rstd = B("m_rstd")
    b_oa = B("m_oa", True)
    b_ob = B("m_ob", True)
    b_sga = [B("m_sga%d" % i) for i in range(2)]
    b_sgb = [B("m_sgb%d" % i) for i in range(2)]
    b_mg = [B("m_mg%d" % i) for i in range(8)]
    b_y = B("m_y")
    g_pre = k.gsb[:, 16:24]
    g_post = k.gsb[:, 24:32]
    for c in range(8):
        DMA(k, "pool", [], [b_w2[c]], out=w2[:, c, :], in_=k.mix_w2[:, c, :])
    DMA(k, "pool", [], [b_wa], out=wa, in_=k.w_br_a)
    DMA(k, "pool", [], [b_wb], out=wb, in_=k.w_br_b)
    DMA(k, "pool", [], [b_wo], out=wo, in_=k.mix_w_out)
    for it in range(S // T):
        t0 = it * T
        DMA(k, "sp", [], [b_h], out=hT, in_=k.h1T[:, t0:t0 + T].rearrange("(c p) t -> p c t", p=128))
        DMA(k, "sp", [bd["oaT"]], [b_oa], out=oa, in_=d["oaT"][:, t0:t0 + T].rearrange("(c p) t -> p c t", p=128))
        DMA(k, "sp", [bd["obT"]], [b_ob], out=obt, in_=d["obT"][:, t0:t0 + T].rearrange("(c p) t -> p c t", p=128))
        for c in range(8):
            O(k, "act", "activation", [b_h], [b_su], out=su[:, c, :], in_=hT[:, c, :], func=AF.Square)
        rms_stats(k, su, b_su, rstd, b_rstd, T)
        for c in range(8):
            O(k, "dve", "scalar_tensor_tensor", [b_h, k.b_g, b_rstd], [b_su], out=su[:, c, :], in0=hT[:, c, :], scalar=g_pre[:, c:c + 1],
              in1=rstd, op0=ALU.mult, op1=ALU.mult)
        for dc in range(8):
            j = dc % 2
            dsl = slice(dc * 128, (dc + 1) * 128)
            pga, b_pga = next_ps(k)
            for c in range(8):
                O(k, "pe", "matmul", [b_w2[c], b_su], [b_pga], out=pga[:, 0:T], lhsT=w2[:, c, dc * 128:(dc + 1) * 128], rhs=su[:, c, :],
                  start=(c == 0), stop=(c == 7))
            pgb, b_pgb = next_ps(k)
            for c in range(8):
                O(k, "pe", "matmul", [b_w2[c], b_su], [b_pgb], out=pgb[:, 0:T], lhsT=w2[:, c, D + dc * 128:D + (dc + 1) * 128], rhs=su[:, c, :],
                  start=(c == 0), stop=(c == 7))
            pya, b_pya = next_ps(k)
            for c in range(4):
                O(k, "pe", "matmul", [b_wa, b_oa], [b_pya], out=pya[:, 0:T], lhsT=wa[:, c, dsl], rhs=oa[:, c, :], start=(c == 0), stop=(c == 3))
            pyb, b_pyb = next_ps(k)
            for c in range(4):
                O(k, "pe", "matmul", [b_wb, b_ob], [b_pyb], out=pyb[:, 0:T], lhsT=wb[:, c, dsl], rhs=obt[:, c, :], start=(c == 0), stop=(c == 3))
            O(k, "act", "activation", [b_pga], [b_sga[j]], out=sga[j], in_=pga[:, 0:T], func=AF.Sigmoid)
            O(k, "act", "activation", [b_pgb], [b_sgb[j]], out=sgb[j], in_=pgb[:, 0:T], func=AF.Sigmoid)
            O(k, "dve", "tensor_tensor", [b_sga[j], b_pya], [b_sga[j]], out=sga[j], in0=sga[j], in1=pya[:, 0:T], op=ALU.mult)
            O(k, "dve", "tensor_tensor", [b_sgb[j], b_pyb], [b_sgb[j]], out=sgb[j], in0=sgb[j], in1=pyb[:, 0:T], op=ALU.mult)
            O(k, "pool", "tensor_tensor", [b_sga[j], b_sgb[j]], [b_mg[dc]], out=mg[:, dc, :], in0=sga[j], in1=sgb[j], op=ALU.add)
        for dc in range(8):
            psy, b_psy = next_ps(k)
            for c in range(8):
                O(k, "pe", "matmul", [b_wo, b_mg[c]], [b_psy], out=psy[:, 0:T], lhsT=wo[:, c, dc * 128:(dc + 1) * 128], rhs=mg[:, c, :],
                  start=(c == 0), stop=(c == 7))
            O(k, "act", "activation", [b_psy], [b_y], out=y[:, dc, :], in_=psy[:, 0:T], func=AF.Copy)
            O(k, "act", "activation", [b_psy], [b_su], out=su[:, dc, :], in_=psy[:, 0:T], func=AF.Square)
        rms_stats(k, su, b_su, rstd, b_rstd, T)
        for c in range(8):
            O(k, "dve", "scalar_tensor_tensor", [b_y, k.b_g, b_rstd], [b_y], out=y[:, c, :], in0=y[:, c, :], scalar=g_post[:, c:c + 1],
              in1=rstd, op0=ALU.mult, op1=ALU.mult)
        for c in range(8):
            O(k, "pool", "tensor_tensor", [b_h, b_y], [b_h], out=hT[:, c, :], in0=hT[:, c, :], in1=y[:, c, :], op=ALU.add)
        DMA(k, "sp", [b_h], [bd["h2T"]], out=d["h2T"][:, t0:t0 + T].rearrange("(c p) t -> p c t", p=128), in_=hT)
```
